# Optimizing a Trainium2 kernel written in Bass

```python
import math, functools
import jax, jax.numpy as jnp
from jax import lax
import numpy as np

D_MODEL = 1024
BATCH = 8
SEQ = 4096
DEPTH = 2
DEC_BATCH = 32
DEC_SEQ = 4
PAST_LEN = 16384
PAGE_SIZE = 128

GROUP_W = D_MODEL // 4
CONV_A_W = 3
N_HEADS_B = 4
HEAD_DIM_B = GROUP_W // N_HEADS_B
CONV_B_W = 4
N_HEADS_C = 4
HEAD_DIM_C = GROUP_W // N_HEADS_C
N_HEADS_D = 4
HEAD_DIM_DV = GROUP_W // N_HEADS_D
HEAD_DIM_DQK = HEAD_DIM_DV // 2
CHUNK = 64
Q_BLOCK = 128
N_BUCKETS = 32
MAX_EXACT = N_BUCKETS // 2
MAX_DISTANCE = 128
D_FF = 2816
N_EXPERTS = 8
TOP_K = 2
D_FF_EXPERT = 1408
N_DENSE = (DEPTH + 1) // 2
N_MOE = DEPTH // 2
ALPHA = (2 * DEPTH) ** 0.25
BETA_INIT = (8 * DEPTH) ** -0.25
LN_EPS = 1e-5
RMS_EPS = 1e-6
SPLITS = (GROUP_W, GROUP_W, GROUP_W,
          3 * GROUP_W, N_HEADS_B, N_HEADS_B, GROUP_W,
          GROUP_W, GROUP_W, GROUP_W, GROUP_W,
          GROUP_W, GROUP_W, GROUP_W)
D_IN = sum(SPLITS)

kernel_name = 'hybrid_parallel_groups_decode_step'


def _split(u, sizes):
    out, o = [], 0
    for s in sizes:
        out.append(u[..., o:o + s])
        o += s
    return out


def _layer_norm(x, g, b):
    xf = x.astype(jnp.float32)
    mu = jnp.mean(xf, -1, keepdims=True)
    var = jnp.mean(jnp.square(xf - mu), -1, keepdims=True)
    return ((xf - mu) * lax.rsqrt(var + LN_EPS) * g + b).astype(x.dtype)


def _rms_norm(x, w):
    xf = x.astype(jnp.float32)
    r = xf * lax.rsqrt(jnp.mean(jnp.square(xf), -1, keepdims=True) + RMS_EPS)
    return (r * w).astype(x.dtype)


def _l2norm(x):
    xf = x.astype(jnp.float32)
    return (xf * lax.rsqrt(jnp.sum(jnp.square(xf), -1, keepdims=True) + RMS_EPS)).astype(x.dtype)


def _causal_dwconv(x, buf, w):
    width, L = w.shape[0], x.shape[1]
    xp = jnp.concatenate([buf.astype(x.dtype), x], axis=1)
    y = sum(xp[:, j:j + L] * w[j] for j in range(width))
    return y, xp[:, xp.shape[1] - (width - 1):]


def _to_chunks(t, n):
    t = t.reshape((t.shape[0], n, CHUNK) + t.shape[2:])
    return jnp.swapaxes(jnp.moveaxis(t, 1, 0), 2, 3)


def _from_chunks(t, L):
    t = jnp.moveaxis(jnp.swapaxes(t, 2, 3), 0, 1)
    return t.reshape((t.shape[0], t.shape[1] * t.shape[2]) + t.shape[3:])[:, :L]


def _pad_f32(t, pad):
    return jnp.pad(t.astype(jnp.float32), [(0, 0), (0, pad)] + [(0, 0)] * (t.ndim - 2))


def _gated_delta(q, k, v, beta, g, s0):
    L, dv = q.shape[1], v.shape[-1]
    n = -(-L // CHUNK)
    pad = n * CHUNK - L
    qc, kc, vc, bc, gc = [_to_chunks(_pad_f32(t, pad), n) for t in (q, k, v, beta, g)]
    incl = jnp.tril(jnp.ones((CHUNK, CHUNK), bool))
    strict = jnp.tril(jnp.ones((CHUNK, CHUNK), bool), -1)

    def step(S, inp):
        qi, ki, vi, bi, gi = inp
        gcum = jnp.cumsum(gi, axis=-1)
        decay = jnp.exp(jnp.where(incl, gcum[..., :, None] - gcum[..., None, :], -jnp.inf))
        a = jnp.where(strict, jnp.einsum('bhik,bhjk->bhij', ki, ki) * decay * bi[..., None], 0.0)
        rhs = jnp.concatenate([vi * bi[..., None], ki * (bi * jnp.exp(gcum))[..., None]], axis=-1)
        sol = lax.linalg.triangular_solve(a, rhs, left_side=True, lower=True, unit_diagonal=True)
        u, w = sol[..., :dv], sol[..., dv:]
        v_new = u - jnp.einsum('bhck,bhkv->bhcv', w, S)
        o = (jnp.einsum('bhck,bhkv->bhcv', qi * jnp.exp(gcum)[..., None], S)
             + jnp.einsum('bhij,bhjv->bhiv', jnp.einsum('bhik,bhjk->bhij', qi, ki) * decay, v_new))
        glast = gcum[..., -1:]
        S = S * jnp.exp(glast)[..., None] + jnp.einsum('bhck,bhcv->bhkv', ki * jnp.exp(glast - gcum)[..., None], v_new)
        return S, o

    S, o = lax.scan(step, s0.astype(jnp.float32), (qc, kc, vc, bc, gc))
    return _from_chunks(o, L).astype(q.dtype), S.astype(s0.dtype)


def _gla(q, k, v, log_f, s0):
    L = q.shape[1]
    n = -(-L // CHUNK)
    pad = n * CHUNK - L
    qc, kc, vc, lc = [_to_chunks(_pad_f32(t, pad), n) for t in (q, k, v, log_f)]
    incl = jnp.tril(jnp.ones((CHUNK, CHUNK), bool))[:, :, None]

    def step(S, inp):
        qi, ki, vi, li = inp
        b = jnp.cumsum(li, axis=2)
        dec = jnp.exp(jnp.where(incl, b[:, :, :, None, :] - b[:, :, None, :, :], -jnp.inf))
        att = jnp.einsum('bhik,bhjk,bhijk->bhij', qi, ki, dec)
        o = jnp.einsum('bhik,bhkv->bhiv', qi * jnp.exp(b), S) + jnp.einsum('bhij,bhjv->bhiv', att, vi)
        blast = b[:, :, -1:, :]
        S = jnp.exp(blast[:, :, 0, :])[..., None] * S + jnp.einsum('bhjk,bhjv->bhkv', ki * jnp.exp(blast - b), vi)
        return S, o

    S, o = lax.scan(step, s0.astype(jnp.float32), (qc, kc, vc, lc))
    return _from_chunks(o, L).astype(q.dtype), S.astype(s0.dtype)


def _t5_bucket(q_pos, k_pos):
    n = jnp.maximum(q_pos[:, None] - k_pos[None, :], 0)
    nf = jnp.maximum(n, MAX_EXACT).astype(jnp.float32)
    large = MAX_EXACT + (jnp.log(nf / MAX_EXACT) / math.log(MAX_DISTANCE / MAX_EXACT)
                         * (N_BUCKETS - MAX_EXACT)).astype(jnp.int32)
    large = jnp.minimum(large, N_BUCKETS - 1)
    return jnp.where(n < MAX_EXACT, n, large)


def _diff_attn_core(q, k, v, q_pos, k_pos, lam, rel_bias):
    s = jnp.einsum('bqhmd,bkhmd->bmhqk', q, k).astype(jnp.float32) * (HEAD_DIM_DQK ** -0.5)
    bias = jnp.transpose(rel_bias[_t5_bucket(q_pos, k_pos)], (2, 0, 1)).astype(jnp.float32)
    s = jnp.where(k_pos[None, :] <= q_pos[:, None], s + bias, -jnp.inf)
    p = jax.nn.softmax(s, axis=-1)
    w = p[:, 0] - lam * p[:, 1]
    return jnp.einsum('bhqk,bkhv->bqhv', w.astype(v.dtype), v)


def _attend_prompt(q, k, v, lam, rel_bias):
    bsz, S = q.shape[:2]
    k_pos = jnp.arange(S)

    def one(i):
        start = i * Q_BLOCK
        qb = lax.dynamic_slice_in_dim(q, start, Q_BLOCK, axis=1)
        return _diff_attn_core(qb, k, v, start + jnp.arange(Q_BLOCK), k_pos, lam, rel_bias)

    o = lax.map(one, jnp.arange(S // Q_BLOCK))
    return jnp.moveaxis(o, 0, 1).reshape(bsz, S, N_HEADS_D, HEAD_DIM_DV)


def _attend_sample(q, k, v, lam, rel_bias, k_pages, v_pages, page_table):
    bsz, L = q.shape[:2]
    past = page_table.shape[1] * k_pages.shape[1]
    kp = k_pages[page_table].reshape(bsz, past, N_HEADS_D, 2, HEAD_DIM_DQK)
    vp = v_pages[page_table].reshape(bsz, past, N_HEADS_D, HEAD_DIM_DV)
    k_all = jnp.concatenate([kp.astype(k.dtype), k], axis=1)
    v_all = jnp.concatenate([vp.astype(v.dtype), v], axis=1)
    return _diff_attn_core(q, k_all, v_all, past + jnp.arange(L), jnp.arange(past + L), lam, rel_bias)


def _swiglu(x, wg, wu, wd):
    return (jax.nn.silu(x @ wg) * (x @ wu)) @ wd


def _moe(x, w_r, w_g, w_u, w_d):
    shp = x.shape
    xt = x.reshape(-1, shp[-1])
    logits = (xt @ w_r).astype(jnp.float32)
    top_v, top_i = lax.top_k(logits, TOP_K)
    gates = jax.nn.softmax(top_v, axis=-1)
    comb = jnp.sum(jax.nn.one_hot(top_i, N_EXPERTS, dtype=jnp.float32) * gates[..., None], axis=1)
    out = jnp.zeros_like(xt)
    for e in range(N_EXPERTS):
        out = out + (comb[:, e:e + 1] * _swiglu(xt, w_g[e], w_u[e], w_d[e])).astype(xt.dtype)
    return out.reshape(shp)


def _layer(l, x, conv_a0, conv_b0, s_gdn0, s_hgrn0, attend, p):
    f32 = jnp.float32
    bsz, L, _ = x.shape
    u = x @ p['w_in'][l]
    (a_in, a_gb, a_gc, b_qkv, b_a, b_b, b_z,
     c_q, c_f, c_i, c_g, d_q, d_k, d_v) = _split(u, SPLITS)
    a_conv, conv_a1 = _causal_dwconv(a_gc * a_in, conv_a0, p['conv_a'][l])
    y_a = a_gb * a_conv
    b_conv, conv_b1 = _causal_dwconv(b_qkv, conv_b0, p['conv_b'][l])
    b_conv = jax.nn.silu(b_conv)
    hb = lambda t: t.reshape(bsz, L, N_HEADS_B, HEAD_DIM_B)
    qb = _l2norm(hb(b_conv[..., :GROUP_W])) * (HEAD_DIM_B ** -0.5)
    kb = _l2norm(hb(b_conv[..., GROUP_W:2 * GROUP_W]))
    vb = hb(b_conv[..., 2 * GROUP_W:])
    beta = jax.nn.sigmoid(b_b.astype(f32))
    g = -jnp.exp(p['gdn_a_log'][l].astype(f32)) * jax.nn.softplus(b_a.astype(f32) + p['gdn_dt_bias'][l].astype(f32))
    ob, s_gdn1 = _gated_delta(qb, kb, vb, beta, g, s_gdn0)
    y_b = (_rms_norm(ob, p['norm_b'][l]) * jax.nn.silu(hb(b_z))).reshape(bsz, L, GROUP_W)
    lbs = jax.nn.softmax(p['lower_bounds'].astype(f32), axis=0)
    lb = (jnp.cumsum(lbs, axis=0) - lbs[0])[l]
    cf = c_f.astype(f32)
    sig_f = jax.nn.sigmoid(cf)
    log_f = jnp.log(lb + (1.0 - lb) * sig_f)
    k_c = (1.0 - lb) * (1.0 - sig_f)
    hc = lambda t: t.reshape(bsz, L, N_HEADS_C, HEAD_DIM_C)
    oc, s_hgrn1 = _gla(hc(jax.nn.silu(c_q)), hc(k_c), hc(c_i), hc(log_f), s_hgrn0)
    y_c = (_rms_norm(oc, p['norm_c'][l]) * jax.nn.silu(hc(c_g))).reshape(bsz, L, GROUP_W)
    qd = d_q.reshape(bsz, L, N_HEADS_D, 2, HEAD_DIM_DQK)
    kd = d_k.reshape(bsz, L, N_HEADS_D, 2, HEAD_DIM_DQK)
    vd = d_v.reshape(bsz, L, N_HEADS_D, HEAD_DIM_DV)
    lam_init = 0.8 - 0.6 * math.exp(-0.3 * l)
    lam = (jnp.exp(jnp.sum(p['lambda_q1'][l].astype(f32) * p['lambda_k1'][l].astype(f32)))
           - jnp.exp(jnp.sum(p['lambda_q2'][l].astype(f32) * p['lambda_k2'][l].astype(f32))) + lam_init)
    od = attend(qd, kd, vd, lam)
    y_d = (_rms_norm(od, p['norm_d'][l]) * (1.0 - lam_init)).reshape(bsz, L, GROUP_W)
    mix = jnp.concatenate([y_a, y_b, y_c, y_d], axis=-1) @ p['w_o'][l]
    x = _layer_norm(ALPHA * x + mix, p['ln1_g'][l], p['ln1_b'][l])
    j = l // 2
    if l % 2 == 0:
        f = _swiglu(x, p['ffn_w_gate'][j], p['ffn_w_up'][j], p['ffn_w_down'][j])
    else:
        f = _moe(x, p['router_w'][j], p['moe_w_gate'][j], p['moe_w_up'][j], p['moe_w_down'][j])
    x = _layer_norm(ALPHA * x + f, p['ln2_g'][l], p['ln2_b'][l])
    k_rows = kd.reshape(bsz, L, N_HEADS_D, 2 * HEAD_DIM_DQK)
    return x, (k_rows, vd, conv_a1, conv_b1, s_gdn1, s_hgrn1)


def _run(x, init, attend, p):
    states = []
    for l in range(DEPTH):
        x, st = _layer(l, x, *init[l], functools.partial(attend, l), p)
        states.append(st)
    return x, [jnp.stack([s[i] for s in states]) for i in range(6)]


def setup_inputs(seed: int = 0) -> dict:
    key = jax.random.key(seed)
    keys = jax.random.split(key, 48)
    ctr = [0]

    def nk():
        ctr[0] += 1
        return keys[ctr[0] - 1]

    f32 = jnp.float32

    def nrm(shape, scale):
        return jax.random.normal(nk(), shape, f32) * scale

    n_pages = PAST_LEN // PAGE_SIZE
    n_pool = (DEC_BATCH * n_pages * 5) // 4
    page_table = jax.random.permutation(nk(), n_pool)[:DEC_BATCH * n_pages].reshape(DEC_BATCH, n_pages).astype(jnp.int32)
    dt = jnp.exp(jax.random.uniform(nk(), (DEPTH, N_HEADS_B), f32, math.log(1e-3), math.log(1e-1)))
    return {
        'x_prompt': nrm((BATCH, SEQ, D_MODEL), 1.0),
        'x_sample': nrm((DEC_BATCH, DEC_SEQ, D_MODEL), 1.0),
        'cache_k': nrm((DEPTH, n_pool, PAGE_SIZE, N_HEADS_D, 2 * HEAD_DIM_DQK), 1.0),
        'cache_v': nrm((DEPTH, n_pool, PAGE_SIZE, N_HEADS_D, HEAD_DIM_DV), 1.0),
        'state_conv_a': nrm((DEPTH, DEC_BATCH, CONV_A_W - 1, GROUP_W), 1.0),
        'state_conv_b': nrm((DEPTH, DEC_BATCH, CONV_B_W - 1, 3 * GROUP_W), 1.0),
        'state_gdn': nrm((DEPTH, DEC_BATCH, N_HEADS_B, HEAD_DIM_B, HEAD_DIM_B), 0.5),
        'state_hgrn': nrm((DEPTH, DEC_BATCH, N_HEADS_C, HEAD_DIM_C, HEAD_DIM_C), 1.0),
        'page_table': page_table,
        'w_in': nrm((DEPTH, D_MODEL, D_IN), D_MODEL ** -0.5),
        'conv_a': nrm((DEPTH, CONV_A_W, GROUP_W), CONV_A_W ** -0.5),
        'conv_b': nrm((DEPTH, CONV_B_W, 3 * GROUP_W), CONV_B_W ** -0.5),
        'gdn_a_log': jnp.log(jax.random.uniform(nk(), (DEPTH, N_HEADS_B), f32, 1.0, 16.0)),
        'gdn_dt_bias': dt + jnp.log(-jnp.expm1(-dt)),
        'norm_b': 1.0 + nrm((DEPTH, HEAD_DIM_B), 0.02),
        'lower_bounds': nrm((DEPTH, GROUP_W), 0.1),
        'norm_c': 1.0 + nrm((DEPTH, HEAD_DIM_C), 0.02),
        'lambda_q1': nrm((DEPTH, HEAD_DIM_DQK), 0.1),
        'lambda_k1': nrm((DEPTH, HEAD_DIM_DQK), 0.1),
        'lambda_q2': nrm((DEPTH, HEAD_DIM_DQK), 0.1),
        'lambda_k2': nrm((DEPTH, HEAD_DIM_DQK), 0.1),
        'norm_d': 1.0 + nrm((DEPTH, HEAD_DIM_DV), 0.02),
        'rel_bias': nrm((N_BUCKETS, N_HEADS_D), 0.5),
        'w_o': nrm((DEPTH, D_MODEL, D_MODEL), D_MODEL ** -0.5 * BETA_INIT),
        'ln1_g': 1.0 + nrm((DEPTH, D_MODEL), 0.02),
        'ln1_b': nrm((DEPTH, D_MODEL), 0.02),
        'ffn_w_gate': nrm((N_DENSE, D_MODEL, D_FF), D_MODEL ** -0.5),
        'ffn_w_up': nrm((N_DENSE, D_MODEL, D_FF), D_MODEL ** -0.5),
        'ffn_w_down': nrm((N_DENSE, D_FF, D_MODEL), D_FF ** -0.5 * BETA_INIT),
        'router_w': nrm((N_MOE, D_MODEL, N_EXPERTS), D_MODEL ** -0.5),
        'moe_w_gate': nrm((N_MOE, N_EXPERTS, D_MODEL, D_FF_EXPERT), D_MODEL ** -0.5),
        'moe_w_up': nrm((N_MOE, N_EXPERTS, D_MODEL, D_FF_EXPERT), D_MODEL ** -0.5),
        'moe_w_down': nrm((N_MOE, N_EXPERTS, D_FF_EXPERT, D_MODEL), D_FF_EXPERT ** -0.5 * BETA_INIT),
        'ln2_g': 1.0 + nrm((DEPTH, D_MODEL), 0.02),
        'ln2_b': nrm((DEPTH, D_MODEL), 0.02),
    }


def reference(x_prompt, x_sample, cache_k, cache_v, state_conv_a, state_conv_b, state_gdn, state_hgrn,
              page_table, w_in, conv_a, conv_b, gdn_a_log, gdn_dt_bias, norm_b, lower_bounds, norm_c,
              lambda_q1, lambda_k1, lambda_q2, lambda_k2, norm_d, rel_bias, w_o, ln1_g, ln1_b,
              ffn_w_gate, ffn_w_up, ffn_w_down, router_w, moe_w_gate, moe_w_up, moe_w_down, ln2_g, ln2_b):
    p = dict(w_in=w_in, conv_a=conv_a, conv_b=conv_b, gdn_a_log=gdn_a_log, gdn_dt_bias=gdn_dt_bias,
             norm_b=norm_b, lower_bounds=lower_bounds, norm_c=norm_c, lambda_q1=lambda_q1,
             lambda_k1=lambda_k1, lambda_q2=lambda_q2, lambda_k2=lambda_k2, norm_d=norm_d, w_o=w_o,
             ln1_g=ln1_g, ln1_b=ln1_b, ffn_w_gate=ffn_w_gate, ffn_w_up=ffn_w_up, ffn_w_down=ffn_w_down,
             router_w=router_w, moe_w_gate=moe_w_gate, moe_w_up=moe_w_up, moe_w_down=moe_w_down,
             ln2_g=ln2_g, ln2_b=ln2_b)
    bp = x_prompt.shape[0]
    dtp = x_prompt.dtype
    init_p = [(jnp.zeros((bp, CONV_A_W - 1, GROUP_W), dtp),
               jnp.zeros((bp, CONV_B_W - 1, 3 * GROUP_W), dtp),
               jnp.zeros((bp, N_HEADS_B, HEAD_DIM_B, HEAD_DIM_B), dtp),
               jnp.zeros((bp, N_HEADS_C, HEAD_DIM_C, HEAD_DIM_C), dtp)) for _ in range(DEPTH)]
    init_s = [(state_conv_a[l], state_conv_b[l], state_gdn[l], state_hgrn[l]) for l in range(DEPTH)]
    attend_p = lambda l, q, k, v, lam: _attend_prompt(q, k, v, lam, rel_bias)
    attend_s = lambda l, q, k, v, lam: _attend_sample(q, k, v, lam, rel_bias, cache_k[l], cache_v[l], page_table)
    y_prompt, (k_p, v_p, ca_p, cb_p, sg_p, sh_p) = _run(x_prompt, init_p, attend_p, p)
    y_sample, (k_s, v_s, ca_s, cb_s, sg_s, sh_s) = _run(x_sample, init_s, attend_s, p)
    return (y_prompt, y_sample, k_p, v_p, k_s, v_s, ca_p, ca_s, cb_p, cb_s, sg_p, sg_s, sh_p, sh_s)
```

```python
import math
from contextlib import ExitStack
import numpy as np
import concourse.bass as bass
import concourse.mybir as mybir
from concourse.bass_utils import run_bass_kernel_spmd

F32 = mybir.dt.float32
BF16 = mybir.dt.bfloat16
I32 = mybir.dt.int32
AF = mybir.ActivationFunctionType
OP = mybir.AluOpType
AX = mybir.AxisListType

D_MODEL = 1024
GW = 256
D_IN = 3592
D_FF = 2816
D_FFE = 1408
N_EXP = 8
ALPHA = 4 ** 0.25
LN_EPS = 1e-5
RMS_EPS = 1e-6
O_AIN, O_AGB, O_AGC, O_BQKV, O_BA, O_BB, O_BZ = 0, 256, 512, 768, 1536, 1540, 1544
O_CQ, O_CF, O_CI, O_CG, O_DQ, O_DK, O_DV = 1800, 2056, 2312, 2568, 2824, 3080, 3336
FM_COLS = [(O_AIN, 768), (O_CQ, 512), (O_DQ, 512)]
FM_N = 1792
R_AIN, R_AGB, R_AGC, R_CQ, R_CF, R_DQ, R_DK = 0, 256, 512, 768, 1024, 1280, 1536
TM_COLS = [(O_BQKV, 1032), (O_CI, 512), (O_DK, 512)]
TM_N = 2056
C_BQKV, C_BA, C_BB, C_BZ, C_CI, C_CG, C_DK, C_DV = 0, 768, 772, 776, 1032, 1288, 1544, 1800


class Res:
    __slots__ = ("w", "r")

    def __init__(self):
        self.w = None
        self.r = []


class Sched:
    NDMA = 24

    def __init__(self, nc, es):
        self.nc = nc
        self.ops = []
        self.out_dmas = []
        self.engs = {"pe": nc.tensor, "act": nc.scalar, "pool": nc.gpsimd, "dve": nc.vector, "sp": nc.sync}
        self.esem = {e: es.enter_context(nc.semaphore("s_" + e)) for e in self.engs}
        self.dsem = [es.enter_context(nc.semaphore("d%d" % i)) for i in range(self.NDMA)]
        self.tok = []
        self.ecount = {e: 0 for e in self.engs}
        self.dval = [0] * self.NDMA
        self.clock = {e: {} for e in self.engs}
        self.dcount = 0
        self.last_on_sem = [None] * self.NDMA
        self.flushed = 0
        self.n_instr = {e: 0 for e in self.engs}

    def op(self, eng, fn, reads=(), writes=(), dma=False):
        idx = len(self.ops)
        deps = set()
        for r in reads:
            if r.w is not None:
                deps.add(r.w)
        for w in writes:
            if w.w is not None:
                deps.add(w.w)
            deps.update(w.r)
        for r in reads:
            r.r.append(idx)
        for w in writes:
            w.w = idx
            w.r = []
        self.ops.append([eng, fn, deps, dma])
        return idx

    def dma(self, eng, out, in_, reads=(), writes=(), output=False, **kw):
        idx = self.op(eng, lambda e: e.dma_start(out=out, in_=in_, **kw), reads, writes, dma=True)
        if output:
            self.out_dmas.append(idx)
        return idx

    def flush(self):
        nc = self.nc
        ops = self.ops
        start = self.flushed
        ops.append(["sp", None, set(i for i in range(start, len(ops)) if ops[i][3]), False])
        n = len(ops)
        self.tok.extend([None] * (n - len(self.tok)))
        tok = self.tok
        needed = {}
        dma_slot = {}
        for i in range(start, n):
            o = ops[i]
            if o[3]:
                k = self.dcount % self.NDMA
                self.dcount += 1
                if self.last_on_sem[k] is not None:
                    o[2].add(self.last_on_sem[k])
                self.last_on_sem[k] = i
                dma_slot[i] = k
        for i in range(start, n):
            o = ops[i]
            for d in o[2]:
                if d < start:
                    continue
                od = ops[d]
                if od[3] or o[3] or od[0] != o[0] or o[0] != "pe":
                    needed[d] = True
        streams = {e: [] for e in self.engs}
        for i in range(start, n):
            eng, fn, deps, is_dma = ops[i]
            waits = {}
            clk = self.clock[eng]
            for d in deps:
                if d < start:
                    continue
                od = ops[d]
                if not (od[3] or is_dma or od[0] != eng or eng != "pe"):
                    continue
                sem, val = tok[d]
                key = id(sem)
                if clk.get(key, 0) >= val:
                    continue
                if key not in waits or waits[key][1] < val:
                    waits[key] = (sem, val)
            for key, (sem, val) in waits.items():
                clk[key] = val
            wl = list(waits.values())
            if is_dma:
                k = dma_slot[i]
                self.dval[k] += 16
                tok[i] = (self.dsem[k], self.dval[k])
                inc = (self.dsem[k], 16)
            elif needed.get(i, False):
                self.ecount[eng] += 1
                tok[i] = (self.esem[eng], self.ecount[eng])
                inc = (self.esem[eng], 1)
            else:
                inc = None
            streams[eng].append((wl, fn, inc))
        for e, v in streams.items():
            self.n_instr[e] += len(v)
        self.flushed = n

        def run(e, lst):
            for wl, fn, inc in lst:
                for sem, val in wl:
                    e.wait_ge(sem, val)
                if fn is None:
                    continue
                ins = fn(e)
                if inc is not None:
                    ins.then_inc(inc[0], inc[1])

        with nc.Block() as block:
            @block.sync
            def _(e):
                run(e, streams["sp"])

            @block.scalar
            def _(e):
                run(e, streams["act"])

            @block.vector
            def _(e):
                run(e, streams["dve"])

            @block.gpsimd
            def _(e):
                run(e, streams["pool"])

            @block.tensor
            def _(e):
                run(e, streams["pe"])


class Buf:
    def __init__(self, t):
        self.t = t
        self.res = Res()

    def __getitem__(self, k):
        return self.t[k]


class View:
    def __init__(self, ap, res):
        self.t = ap
        self.res = res

    def __getitem__(self, k):
        return self.t[k]


class Ring:
    def __init__(self, bufs):
        self.bufs = bufs
        self.i = 0

    def next(self):
        b = self.bufs[self.i % len(self.bufs)]
        self.i += 1
        return b


class Builder:
    def __init__(self, NPT, n_cores, n_local_pages, n_pages_seq):
        self.NPT = NPT
        self.NT = (NPT + 1) * 128
        self.TP = NPT * 128
        self.n_cores = n_cores
        self.NLP = n_local_pages
        self.NPS = n_pages_seq
        self.nc = bass.Bass("TRN2", target_bir_lowering=False)
        self.es = ExitStack()
        self.S = Sched(self.nc, self.es)
        self.sb_bytes = 0
        self.ph = None
        self.ph_bytes = 0
        self.ph_max = 0
        self.uid = 0

    def sb(self, name, shape, dt=F32, glob=False):
        stack = self.es if (glob or self.ph is None) else self.ph
        self.uid += 1
        t = stack.enter_context(self.nc.sbuf_tensor("%s_%d" % (name, self.uid), list(shape), dt))
        nb = int(np.prod(shape[1:])) * (2 if dt == BF16 else 4)
        if stack is self.es:
            self.sb_bytes += nb
        else:
            self.ph_bytes += nb
            self.ph_max = max(self.ph_max, self.ph_bytes)
        return Buf(t)

    def ring(self, name, shape, dt=F32, n=2):
        return Ring([self.sb("%s%d" % (name, i), shape, dt) for i in range(n)])

    def phase_begin(self):
        assert self.ph is None
        self.ph = ExitStack()
        self.ph_bytes = 0

    def phase_end(self):
        self.S.flush()
        self.ph.close()
        self.ph = None

    def ps(self, name, shape, dt=F32):
        t = self.es.enter_context(self.nc.psum_tensor(name, list(shape), dt))
        return Buf(t)

    def dram(self, name, shape, dt=F32, kind="Internal"):
        if kind == "Internal" and getattr(self, "debug", False):
            kind = "ExternalOutput"
        t = self.nc.dram_tensor(name, list(shape), dt, kind=kind)
        b = Buf(t.ap())
        return b

    def dmaq(self, out, in_, reads=(), writes=(), eng="sp", output=False, **kw):
        return self.S.dma(eng, out, in_, reads, writes, output=output, **kw)

    def op(self, eng, fn, reads=(), writes=()):
        return self.S.op(eng, fn, reads, writes)

    def mm(self, out, lhsT, rhs, start, stop, reads, writes):
        return self.S.op("pe", lambda e: e.matmul(out, lhsT, rhs, start=start, stop=stop), reads, writes)

    def tr(self, out, in_, ident, reads, writes):
        return self.S.op("pe", lambda e: e.transpose(out, in_, ident), reads, writes)


def _setup(B):
    nc = B.nc
    NT, TP = B.NT, B.TP
    d = {}

    def inp(name, shape, dt=F32):
        d[name] = B.dram(name, shape, dt, kind="ExternalInput")

    def outp(name, shape):
        d[name] = B.dram(name, shape, F32, kind="ExternalOutput")

    inp("x_prompt", [TP, D_MODEL])
    inp("x_sample", [128, D_MODEL])
    for l_ in range(2):
        inp("cache_k%d" % l_, [B.NLP * 128, 256])
        inp("cache_v%d" % l_, [B.NLP * 128, 256])
    inp("state_conv_a", [2, 32, 2, 256])
    inp("state_conv_b", [2, 32, 3, 768])
    inp("state_gdn", [2, 128, 4096])
    inp("state_hgrn", [2, 128, 4096])
    inp("page_table", [32, B.NPS], I32)
    inp("w_in", [2, D_MODEL, D_IN])
    inp("conv_a", [2, 3, 256])
    inp("conv_b", [2, 4, 768])
    inp("gdn_a_log", [2, 4])
    inp("gdn_dt_bias", [2, 4])
    inp("norm_b", [2, 64])
    inp("lower_bounds", [2, 256])
    inp("norm_c", [2, 64])
    for nm in ("lambda_q1", "lambda_k1", "lambda_q2", "lambda_k2"):
        inp(nm, [2, 32])
    inp("norm_d", [2, 64])
    inp("rel_bias", [32, 4])
    inp("w_o", [2, D_MODEL, D_MODEL])
    inp("ln1_g", [2, D_MODEL])
    inp("ln1_b", [2, D_MODEL])
    inp("ffn_w_gate", [1, D_MODEL, D_FF])
    inp("ffn_w_up", [1, D_MODEL, D_FF])
    inp("ffn_w_down", [1, D_FF, D_MODEL])
    inp("router_w", [1, D_MODEL, N_EXP])
    inp("moe_w_gate", [1, N_EXP, D_MODEL, D_FFE])
    inp("moe_w_up", [1, N_EXP, D_MODEL, D_FFE])
    inp("moe_w_down", [1, N_EXP, D_FFE, D_MODEL])
    inp("ln2_g", [2, D_MODEL])
    inp("ln2_b", [2, D_MODEL])
    inp("c_ident", [128, 128])
    inp("c_misc", [128, 8])
    inp("c_bkt_diag", [128, 33, 128])
    inp("c_bkt_prev", [128, 32, 128])
    inp("c_masks", [128, 8, 128])
    inp("c_scan", [128, 1024])
    inp("c_new", [128, 5, 128])
    inp("c_rep", [32, 128])
    inp("c_pid", [32, B.NLP])
    inp("c_lastb", [128, 4, 32])
    inp("c_ind", [16, 512])
    inp("pt_own", [4 * B.NPS], I32)
    inp("c_ownL", [128, 4 * B.NPS])
    inp("c_isl", [16, 4 * B.NPS])

    outp("y_prompt", [TP, D_MODEL])
    outp("y_sample", [128, D_MODEL])
    outp("k_prompt", [2, TP, 256])
    outp("v_prompt", [2, TP, 256])
    outp("k_sample", [2, 128, 256])
    outp("v_sample", [2, 128, 256])
    outp("conv_a_prompt", [2, 2, 256])
    outp("conv_a_sample", [2, 32, 2, 256])
    outp("conv_b_prompt", [2, 3, 768])
    outp("conv_b_sample", [2, 32, 3, 768])
    outp("gdn_prompt", [2, 256, 64])
    outp("gdn_sample", [2, 128, 4096])
    outp("hgrn_prompt", [2, 256, 64])
    outp("hgrn_sample", [2, 128, 4096])

    d["uT"] = B.dram("uT", [FM_N, NT])
    d["uM"] = B.dram("uM", [NT, TM_N])
    d["yT"] = B.dram("yT", [D_MODEL, NT], BF16)
    d["x1"] = B.dram("x1", [NT, D_MODEL])
    d["x1T"] = B.dram("x1T", [D_MODEL, NT], BF16)
    d["facc"] = B.dram("facc", [NT, D_MODEL])
    d["xs"] = B.dram("xs", [NT, D_MODEL])
    d["gate"] = B.dram("gate", [NT, N_EXP])
    d["uMs"] = B.dram("uMs", [128, 512])
    d["agin"] = B.dram("agin", [128, 520])
    d["agout"] = B.dram("agout", [B.n_cores * 128, 520])
    d["sqd"] = B.dram("sqd", [128, 768])
    d["sod"] = B.dram("sod", [2, 128, 256])
    B.d = d

    B.ident = B.sb("ident", [128, 128])
    B.dmaq(B.ident[:], d["c_ident"][:], writes=[B.ident.res])
    B.masks = B.sb("masks", [128, 8, 128])
    B.dmaq(B.masks[:, :, :], d["c_masks"][:, :, :], writes=[B.masks.res])
    B.psum = [B.ps("ps%d" % i, [128, 512]) for i in range(8)]
    B.ps_i = 0


def _psum2(B):
    p = B.ps_i % 4
    B.ps_i += 1
    return B.psum[2 * p], B.psum[2 * p + 1]


def _psum1(B):
    p = B.ps_i2 % 8 if hasattr(B, "ps_i2") else 0
    B.ps_i2 = p + 1
    return B.psum[p]


def _load_w_bf16(B, dst, src_ap, nk, ncols, stage_ring, eng_cast="pool"):
    src = src_ap.rearrange("(k p) n -> p k n", p=128)
    for k in range(nk):
        st = stage_ring.next()
        B.dmaq(st[:, :ncols], src[:, k, :], writes=[st.res])
        B.op(eng_cast, (lambda e, st=st, k=k: e.tensor_copy(dst[:, k, :], st[:, :ncols])),
             reads=[st.res], writes=[dst.res])


def _phase_inproj(B, l, xsrc_prompt, xsrc_sample):
    d = B.d
    NT, TP = B.NT, B.TP
    B.phase_begin()
    w = B.sb("w_in", [128, 8, D_IN], BF16)
    B.stage = B.ring("stage", [128, D_IN], F32, 2)
    B.xT_ring = B.ring("xT", [128, 8, 512], BF16, 2)
    B.x_ring = B.ring("xt", [128, 1024], F32, 3)
    B.ev_ring = B.ring("ev", [128, 512], F32, 4)
    _load_w_bf16(B, w, d["w_in"][l], 8, D_IN, B.stage)
    ntiles = NT // 128
    groups = []
    t = 0
    while t < ntiles:
        g = min(4, ntiles - t)
        groups.append((t, g))
        t += g
    def grp(t0, g):
        ntok = g * 128
        xT = B.xT_ring.next()
        for j in range(g):
            ti = t0 + j
            xt = B.x_ring.next()
            if ti < B.NPT:
                B.dmaq(xt[:], xsrc_prompt[ti * 128:(ti + 1) * 128, :], writes=[xt.res],
                       reads=[B.xsrc_res])
            else:
                B.dmaq(xt[:], xsrc_sample[:, :], writes=[xt.res], reads=[B.xsrc_res])
            pa, pb = _psum2(B)
            for k in range(8):
                pp = pa if k < 4 else pb
                B.tr(pp[:, (k % 4) * 128:(k % 4 + 1) * 128], xt[:, k * 128:(k + 1) * 128], B.ident[:],
                     reads=[xt.res, B.ident.res], writes=[pp.res])
            B.op("act", (lambda e, xT=xT, pa=pa, j=j: e.copy(
                xT[:, 0:4, j * 128:(j + 1) * 128], pa[:, :].rearrange("p (k t) -> p k t", k=4))),
                reads=[pa.res], writes=[xT.res])
            B.op("dve", (lambda e, xT=xT, pb=pb, j=j: e.tensor_copy(
                xT[:, 4:8, j * 128:(j + 1) * 128], pb[:, :].rearrange("p (k t) -> p k t", k=4))),
                reads=[pb.res], writes=[xT.res])
        row = 0
        ci = 0
        for (c0, cn) in FM_COLS:
            for cc in range(cn // 128):
                col = c0 + cc * 128
                pp = _psum1(B)
                for k in range(8):
                    B.mm(pp[:, :ntok], w[:, k, col:col + 128], xT[:, k, :ntok], k == 0, k == 7,
                         reads=[w.res, xT.res], writes=[pp.res])
                ev = B.ev_ring.next()
                eng = "act" if ci % 2 == 0 else "dve"
                if eng == "act":
                    B.op("act", (lambda e, ev=ev, pp=pp: e.copy(ev[:, :ntok], pp[:, :ntok])),
                         reads=[pp.res], writes=[ev.res])
                else:
                    B.op("dve", (lambda e, ev=ev, pp=pp: e.tensor_copy(ev[:, :ntok], pp[:, :ntok])),
                         reads=[pp.res], writes=[ev.res])
                B.dmaq(d["uT"][row:row + 128, t0 * 128:t0 * 128 + ntok], ev[:, :ntok],
                       reads=[ev.res], writes=[B.uT_res], eng="pool")
                row += 128
                ci += 1
        for j in range(g):
            ti = t0 + j
            colo = 0
            for (c0, cn) in TM_COLS:
                off = 0
                while off < cn:
                    n = min(512, cn - off)
                    pp = _psum1(B)
                    for k in range(8):
                        B.mm(pp[:, :n], xT[:, k, j * 128:(j + 1) * 128], w[:, k, c0 + off:c0 + off + n],
                             k == 0, k == 7, reads=[w.res, xT.res], writes=[pp.res])
                    ev = B.ev_ring.next()
                    eng = "act" if ci % 2 == 0 else "dve"
                    ci += 1
                    if eng == "act":
                        B.op("act", (lambda e, ev=ev, pp=pp, n=n: e.copy(ev[:, :n], pp[:, :n])),
                             reads=[pp.res], writes=[ev.res])
                    else:
                        B.op("dve", (lambda e, ev=ev, pp=pp, n=n: e.tensor_copy(ev[:, :n], pp[:, :n])),
                             reads=[pp.res], writes=[ev.res])
                    B.dmaq(d["uM"][ti * 128:(ti + 1) * 128, colo + off:colo + off + n], ev[:, :n],
                           reads=[ev.res], writes=[B.uM_res], eng="pool")
                    if c0 == O_DK:
                        if ti < B.NPT:
                            ko, vo = d["k_prompt"][l, ti * 128:(ti + 1) * 128, :], d["v_prompt"][l, ti * 128:(ti + 1) * 128, :]
                        else:
                            ko, vo = d["k_sample"][l, :, :], d["v_sample"][l, :, :]
                        B.dmaq(ko, ev[:, 0:256], reads=[ev.res], eng="pool", output=True)
                        B.dmaq(vo, ev[:, 256:512], reads=[ev.res], eng="pool", output=True)
                    off += n
                colo += cn
            if ti >= B.NPT:
                pp = _psum1(B)
                for k in range(8):
                    B.mm(pp[:, :512], xT[:, k, j * 128:(j + 1) * 128], w[:, k, O_CQ:O_CQ + 512],
                         k == 0, k == 7, reads=[w.res, xT.res], writes=[pp.res])
                ev = B.ev_ring.next()
                B.op("act", (lambda e, ev=ev, pp=pp: e.copy(ev[:, :512], pp[:, :512])), reads=[pp.res], writes=[ev.res])
                B.dmaq(d["uMs"][:, :], ev[:, :512], reads=[ev.res], writes=[B.uM_res], eng="pool")

    for (t0, g) in groups:
        grp(t0, g)
    B.phase_end()


def build(NPT=32, n_cores=8, n_local_pages=640, n_pages_seq=128, debug=False, nlayers=2, mixers="abcdsp"):
    B = Builder(NPT, n_cores, n_local_pages, n_pages_seq)
    B.debug = debug
    _setup(B)
    d = B.d
    for nm in ("uT_res", "uM_res", "xsrc_res", "yT_res", "x1_res", "x1T_res", "gate_res", "facc_res"):
        setattr(B, nm, Res())
    B.sm_ring = Ring([B.sb("sm%d" % i, [128, 16], F32, glob=True) for i in range(12)])
    B.rbb = B.sb("rbb", [128, 128], glob=True)
    B.Bd = B.sb("Bd", [128, 4, 128], glob=True)
    B.Bp = B.sb("Bp", [128, 4, 128], glob=True)
    B.lam = B.sb("lam", [128, 4], glob=True)
    B.nrmd = B.sb("nrmd", [128, 128], glob=True)
    B.phase_begin()
    B.G = B.ring("G", [128, 1026], F32, 4)
    B.ybf = B.ring("ybf", [128, 1024], BF16, 1)
    zt = B.ybf.next()
    B.op("pool", lambda e: e.memset(zt[:, :], 0.0), writes=[zt.res])
    for r in range(8):
        for c0 in range(0, B.NT, 1024):
            n = min(1024, B.NT - c0)
            B.dmaq(d["yT"][r * 128:(r + 1) * 128, c0:c0 + n], zt[:, :n], reads=[zt.res], writes=[B.yT_res])
    B.dmaq(B.nrmd[:, :], d["norm_d"].t.rearrange("l d -> (l d)").partition_broadcast(128), writes=[B.nrmd.res])
    _attn_consts(B)
    lam_inits = [_lambda(B, l) for l in range(2)]
    B.phase_end()
    if "p" in mixers or "q" in mixers:
        _sample_attn_setup(B)
    xp, xsm = d["x_prompt"], d["x_sample"]
    for l in range(nlayers):
        lam_init = lam_inits[l]
        _phase_inproj(B, l, xp, xsm)
        if "a" in mixers:
            _mixer_a(B, l)
        if "d" in mixers:
            _mixer_d_prompt(B, l, lam_init)
        if "c" in mixers:
            _mixer_c_prompt(B, l)
        if "b" in mixers:
            _mixer_b_prompt(B, l)
        if "s" in mixers:
            _sample_bc(B, l)
        if "p" in mixers:
            _sample_attn(B, l, lam_init)
        _phase_outproj(B, l, xp, xsm)
        _phase_ffn(B, l)
        if l == 0:
            _phase_ln2(B, l, d["xs"][0:B.TP, :], d["xs"][B.TP:B.NT, :], False)
            xp, xsm = d["xs"][0:B.TP, :], d["xs"][B.TP:B.NT, :]
        else:
            _phase_ln2(B, l, d["y_prompt"], d["y_sample"], True)
    B.phase_begin()
    B.phase_end()
    B.es.close()
    return B


def _own_consts(core, nps):
    nl = 4 * nps
    qseq = np.arange(128)[:, None] // 4
    lseq = 4 * core + np.arange(nl)[None, :] // nps
    ownl = (qseq == lseq).astype(np.float32)
    isl = np.broadcast_to(((np.arange(nl) % nps) == nps - 1).astype(np.float32)[None, :], (16, nl)).copy()
    return ownl, isl


def _host_consts(core, n_local_pages):
    ident = np.eye(128, dtype=np.float32)
    misc = np.zeros((128, 8), np.float32)
    misc[:, 0] = core * n_local_pages
    def bucket(n):
        n = np.maximum(n, 0)
        nf = np.maximum(n, 16).astype(np.float32)
        large = 16 + (np.log(nf / 16) / np.float32(math.log(128 / 16)) * 16).astype(np.int32)
        large = np.minimum(large, 31)
        return np.where(n < 16, n, large)
    kk = np.arange(128)[:, None]
    qq = np.arange(128)[None, :]
    bd = bucket(qq - kk)
    diag = np.zeros((128, 33, 128), np.float32)
    prev = np.zeros((128, 32, 128), np.float32)
    for b in range(32):
        diag[:, b, :] = ((bd == b) & (qq >= kk))
    diag[:, 32, :] = (qq < kk)
    bp = bucket(qq + 128 - kk)
    for b in range(32):
        prev[:, b, :] = (bp == b)
    masks = np.zeros((128, 8, 128), np.float32)
    jj = np.arange(128)[:, None]
    ii = np.arange(128)[None, :]
    masks[:, 0, :] = (jj <= ii)
    masks[:, 1, :] = (jj <= ii) & (jj // 64 == ii // 64)
    masks[:, 2, :] = (jj < ii)
    masks[:, 3, :] = np.where(jj < ii, 0.0, -30000.0)
    masks[:, 4, :] = 1.0
    masks[:, 6, :] = (jj // 64 == ii // 64)
    scan = np.ones((128, 1024), np.float32)
    scan[:, ::64] = 0.0
    misc[:, 1] = np.arange(128)
    kb_, kt_ = np.arange(128)[:, None] // 4, np.arange(128)[:, None] % 4
    qb_, qt_ = np.arange(128)[None, :] // 4, np.arange(128)[None, :] % 4
    cnew = np.zeros((128, 5, 128), np.float32)
    for dist in range(4):
        cnew[:, dist, :] = (kb_ == qb_) & (qt_ - kt_ == dist)
    cnew[:, 4, :] = ~((kb_ == qb_) & (kt_ <= qt_))
    rep = (np.arange(32)[:, None] == (np.arange(128)[None, :] // 4)).astype(np.float32)
    pid = np.broadcast_to((core * n_local_pages + np.arange(n_local_pages, dtype=np.float32))[None, :], (32, n_local_pages)).copy()
    lastb = np.zeros((128, 4, 32), np.float32)
    rr = np.arange(128)[:, None]
    tt = np.arange(4)[None, :]
    bl = bucket(128 + tt - rr)
    for b in range(32):
        lastb[:, :, b] = (bl == b)
    ind = np.zeros((16, 4, 32, 4), np.float32)
    for h in range(4):
        for t in range(4):
            ind[h * 4 + t, h, :, t] = 1.0
    ind = ind.reshape(16, 512)
    return ident, misc, diag, prev, masks, scan, cnew, rep, pid, lastb, ind


def _bcast_row(B, dst, src_row_ap, n):
    B.dmaq(dst[:, :n], src_row_ap.partition_broadcast(128), writes=[dst.res])


def _layernorm(B, tt, g, b, out):
    st = B.sm_ring.next()
    B.op("dve", lambda e: e.bn_stats(st[:, 0:6], tt[:, 0:512]), reads=[tt.res], writes=[st.res])
    B.op("dve", lambda e: e.bn_stats(st[:, 6:12], tt[:, 512:1024]), reads=[tt.res], writes=[st.res])
    B.op("dve", lambda e: e.bn_aggr(st[:, 12:14], st[:, 0:12]), reads=[st.res], writes=[st.res])
    B.op("dve", lambda e: e.tensor_scalar(st[:, 14:15], st[:, 13:14], LN_EPS, None, OP.add), reads=[st.res], writes=[st.res])
    B.op("act", lambda e: e.activation(st[:, 14:15], st[:, 14:15], AF.Ln), reads=[st.res], writes=[st.res])
    B.op("act", lambda e: e.activation(st[:, 14:15], st[:, 14:15], AF.Exp, scale=-0.5), reads=[st.res], writes=[st.res])
    B.op("dve", lambda e: e.scalar_tensor_tensor(st[:, 15:16], st[:, 12:13], -1.0, st[:, 14:15], OP.mult, OP.mult),
         reads=[st.res], writes=[st.res])
    B.op("act", lambda e: e.activation(tt[:, :], tt[:, :], AF.Identity, bias=st[:, 15:16], scale=st[:, 14:15]),
         reads=[st.res, tt.res], writes=[tt.res])
    B.op("pool", lambda e: e.tensor_tensor(out[:, :], tt[:, :], g[:, :], OP.mult),
         reads=[tt.res, g.res], writes=[out.res])
    B.op("pool", lambda e: e.tensor_tensor(out[:, :], out[:, :], b[:, :], OP.add),
         reads=[out.res, b.res], writes=[out.res])


def _phase_outproj(B, l, xsrc_prompt, xsrc_sample):
    d = B.d
    B.phase_begin()
    wo = B.sb("wo", [128, 8, D_MODEL], BF16)
    B.stage = B.ring("stage", [128, D_MODEL], F32, 2)
    B.lng = B.sb("lng", [128, D_MODEL])
    B.lnb = B.sb("lnb", [128, D_MODEL])
    B.t_ring = B.ring("tt", [128, 1024], F32, 6)
    B.x_ring = B.t_ring
    B.yT_ring = B.ring("yTt", [128, 8, 128], BF16, 4)
    B.xTf = B.sb("xTf", [128, 8, 128])
    B.wr = B.sb("wr", [128, 8, 8])
    _load_w_bf16(B, wo, d["w_o"][l], 8, D_MODEL, B.stage)
    _bcast_row(B, B.lng, d["ln1_g"][l], D_MODEL)
    _bcast_row(B, B.lnb, d["ln1_b"][l], D_MODEL)
    moe = (l % 2 == 1)
    if moe:
        B.dmaq(B.wr[:, :, :], d["router_w"][0].rearrange("(k p) n -> p k n", p=128), writes=[B.wr.res])
    yTv = d["yT"].t.rearrange("(k p) t -> p k t", p=128)
    x1Tv = d["x1T"].t.rearrange("(k p) t -> p k t", p=128)
    def tile(ti):
        tok = slice(ti * 128, (ti + 1) * 128)
        xt = B.x_ring.next()
        if ti < B.NPT:
            B.dmaq(xt[:], xsrc_prompt[tok, :], writes=[xt.res], reads=[B.xsrc_res])
        else:
            B.dmaq(xt[:], xsrc_sample[:, :], writes=[xt.res], reads=[B.xsrc_res])
        yt = B.yT_ring.next()
        B.dmaq(yt[:, :, :], yTv[:, :, tok], writes=[yt.res], reads=[B.yT_res])
        pa, pb = _psum2(B)
        for h, pp in enumerate((pa, pb)):
            for k in range(8):
                B.mm(pp[:, :], yt[:, k, :], wo[:, k, h * 512:(h + 1) * 512], k == 0, k == 7,
                     reads=[yt.res, wo.res], writes=[pp.res])
        tt = B.t_ring.next()
        for h, pp in enumerate((pa, pb)):
            B.op("dve", (lambda e, pp=pp, h=h: e.scalar_tensor_tensor(
                tt[:, h * 512:(h + 1) * 512], xt[:, h * 512:(h + 1) * 512], ALPHA, pp[:, :], OP.mult, OP.add)),
                reads=[xt.res, pp.res], writes=[tt.res])
        x1 = B.t_ring.next()
        _layernorm(B, tt, B.lng, B.lnb, x1)
        B.dmaq(d["x1"][tok, :], x1[:, :], reads=[x1.res], writes=[B.x1_res], eng="pool")
        pa, pb = _psum2(B)
        for k in range(8):
            pp = pa if k < 4 else pb
            B.tr(pp[:, (k % 4) * 128:(k % 4 + 1) * 128], x1[:, k * 128:(k + 1) * 128], B.ident[:],
                 reads=[x1.res, B.ident.res], writes=[pp.res])
        xT = B.yT_ring.next()
        B.op("act", lambda e: e.copy(xT[:, 0:4, :], pa[:, :].rearrange("p (k t) -> p k t", k=4)),
             reads=[pa.res], writes=[xT.res])
        B.op("dve", lambda e: e.tensor_copy(xT[:, 4:8, :], pb[:, :].rearrange("p (k t) -> p k t", k=4)),
             reads=[pb.res], writes=[xT.res])
        B.dmaq(x1Tv[:, :, tok], xT[:, :, :], reads=[xT.res], writes=[B.x1T_res], eng="pool")
        gt = B.sm_ring.next()
        if moe:
            xTf = B.xTf
            B.op("act", lambda e: e.copy(xTf[:, 0:4, :], pa[:, :].rearrange("p (k t) -> p k t", k=4)),
                 reads=[pa.res], writes=[xTf.res])
            B.op("dve", lambda e: e.tensor_copy(xTf[:, 4:8, :], pb[:, :].rearrange("p (k t) -> p k t", k=4)),
                 reads=[pb.res], writes=[xTf.res])
            pr = _psum1(B)
            for k in range(8):
                B.mm(pr[:, 0:8], xTf[:, k, :], B.wr[:, k, :], k == 0, k == 7,
                     reads=[xTf.res, B.wr.res], writes=[pr.res])
            lg = B.sm_ring.next()
            B.op("dve", lambda e: e.tensor_copy(lg[:, 0:8], pr[:, 0:8]), reads=[pr.res], writes=[lg.res])
            B.op("dve", lambda e: e.tensor_reduce(lg[:, 8:9], lg[:, 0:8], AX.X, OP.max), reads=[lg.res], writes=[lg.res])
            mk = B.sm_ring.next()
            B.op("dve", lambda e: e.tensor_scalar(mk[:, 0:8], lg[:, 0:8], lg[:, 8:9], None, OP.is_equal),
                 reads=[lg.res], writes=[mk.res])
            B.op("dve", lambda e: e.scalar_tensor_tensor(mk[:, 8:16], mk[:, 0:8], -1e30, lg[:, 0:8], OP.mult, OP.add),
                 reads=[lg.res, mk.res], writes=[mk.res])
            B.op("dve", lambda e: e.tensor_reduce(lg[:, 9:10], mk[:, 8:16], AX.X, OP.max), reads=[mk.res], writes=[lg.res])
            B.op("dve", lambda e: e.tensor_scalar(mk[:, 8:16], mk[:, 8:16], lg[:, 9:10], None, OP.is_equal),
                 reads=[lg.res, mk.res], writes=[mk.res])
            B.op("dve", lambda e: e.tensor_tensor(lg[:, 10:11], lg[:, 8:9], lg[:, 9:10], OP.subtract),
                 reads=[lg.res], writes=[lg.res])
            B.op("act", lambda e: e.activation(lg[:, 10:11], lg[:, 10:11], AF.Sigmoid), reads=[lg.res], writes=[lg.res])
            B.op("dve", lambda e: e.tensor_scalar(lg[:, 11:12], lg[:, 10:11], -1.0, 1.0, OP.mult, OP.add),
                 reads=[lg.res], writes=[lg.res])
            B.op("dve", lambda e: e.tensor_scalar(gt[:, 0:8], mk[:, 0:8], lg[:, 10:11], None, OP.mult),
                 reads=[lg.res, mk.res], writes=[gt.res])
            B.op("dve", lambda e: e.scalar_tensor_tensor(gt[:, 0:8], mk[:, 8:16], lg[:, 11:12], gt[:, 0:8], OP.mult, OP.add),
                 reads=[lg.res, mk.res, gt.res], writes=[gt.res])
        else:
            B.op("pool", lambda e: e.memset(gt[:, 0:8], 1.0), writes=[gt.res])
        B.dmaq(d["gate"][tok, :], gt[:, 0:8], reads=[gt.res], writes=[B.gate_res], eng="pool")

    for ti in range(B.NT // 128):
        tile(ti)
    B.phase_end()


def _phase_ffn(B, l):
    d = B.d
    moe = (l % 2 == 1)
    B.phase_begin()
    W = B.sb("arena", [128, 33792], BF16)
    B.stage = B.ring("stage", [128, D_FFE], F32, 3)
    B.xT_ring = B.ring("xT", [128, 8, 512], BF16, 2)
    B.hT_ring = B.ring("hT", [128, 11, 512], BF16, 2)
    B.ev_ring = B.ring("ev", [128, 512], F32, 3)
    B.t_ring = B.ring("tt", [128, 1024], F32, 3)
    B.sm4_ring = B.ring("sm4", [128, 4, 8], F32, 2)
    wflat = W.t
    wg = wflat[:, 0:8 * D_FFE].rearrange("p (k n) -> p k n", k=8)
    wu = wflat[:, 8 * D_FFE:16 * D_FFE].rearrange("p (k n) -> p k n", k=8)
    wd = wflat[:, 16 * D_FFE:16 * D_FFE + 11 * D_MODEL].rearrange("p (k n) -> p k n", k=11)
    x1Tv = d["x1T"].t.rearrange("(k p) t -> p k t", p=128)
    n_exp = N_EXP if moe else 2
    ntiles = B.NT // 128
    groups = []
    t = 0
    while t < ntiles:
        g = min(4, ntiles - t)
        groups.append((t, g))
        t += g
    for ex in range(n_exp):
        if moe:
            sg, su, sd = d["moe_w_gate"][0, ex], d["moe_w_up"][0, ex], d["moe_w_down"][0, ex]
        else:
            sg = d["ffn_w_gate"][0][:, ex * D_FFE:(ex + 1) * D_FFE]
            su = d["ffn_w_up"][0][:, ex * D_FFE:(ex + 1) * D_FFE]
            sd = d["ffn_w_down"][0][ex * D_FFE:(ex + 1) * D_FFE, :]
        for (dstv, src, nk, ncol) in ((wg, sg, 8, D_FFE), (wu, su, 8, D_FFE), (wd, sd, 11, D_MODEL)):
            srcv = src.rearrange("(k p) n -> p k n", p=128)
            for k in range(nk):
                st = B.stage.next()
                B.dmaq(st[:, :ncol], srcv[:, k, :], writes=[st.res])
                B.op("pool", (lambda e, st=st, k=k, dstv=dstv, ncol=ncol: e.tensor_copy(dstv[:, k, :], st[:, :ncol])),
                     reads=[st.res], writes=[W.res])
        def group(t0, g, ex=ex):
            ntok = g * 128
            xT = B.xT_ring.next()
            B.dmaq(xT[:, :, :ntok], x1Tv[:, :, t0 * 128:t0 * 128 + ntok], writes=[xT.res], reads=[B.x1T_res])
            gsb = B.sm4_ring.next()
            B.dmaq(gsb[:, :g, :], d["gate"][t0 * 128:t0 * 128 + ntok, :].rearrange("(j p) n -> p j n", p=128),
                   writes=[gsb.res], reads=[B.gate_res])
            hT = B.hT_ring.next()
            for fc in range(11):
                pg, pu = _psum2(B)
                for k in range(8):
                    B.mm(pg[:, :ntok], wg[:, k, fc * 128:(fc + 1) * 128], xT[:, k, :ntok], k == 0, k == 7,
                         reads=[W.res, xT.res], writes=[pg.res])
                for k in range(8):
                    B.mm(pu[:, :ntok], wu[:, k, fc * 128:(fc + 1) * 128], xT[:, k, :ntok], k == 0, k == 7,
                         reads=[W.res, xT.res], writes=[pu.res])
                sg_ = B.ev_ring.next()
                B.op("act", (lambda e, sg_=sg_, pg=pg: e.activation(sg_[:, :ntok], pg[:, :ntok], AF.Silu)),
                     reads=[pg.res], writes=[sg_.res])
                B.op("dve", (lambda e, sg_=sg_, pu=pu, fc=fc, hT=hT: e.tensor_tensor(hT[:, fc, :ntok], sg_[:, :ntok], pu[:, :ntok], OP.mult)),
                     reads=[sg_.res, pu.res], writes=[hT.res])
            for j in range(g):
                ti = t0 + j
                pa, pb = _psum2(B)
                for h, pp in enumerate((pa, pb)):
                    for k in range(11):
                        B.mm(pp[:, :], hT[:, k, j * 128:(j + 1) * 128], wd[:, k, h * 512:(h + 1) * 512], k == 0, k == 10,
                             reads=[hT.res, W.res], writes=[pp.res])
                fo = B.t_ring.next()
                ge = ex if moe else 0
                B.op("act", (lambda e, fo=fo, pa=pa, gsb=gsb, j=j, ge=ge: e.activation(
                    fo[:, 0:512], pa[:, :], AF.Copy, scale=gsb[:, j, ge:ge + 1])),
                    reads=[pa.res, gsb.res], writes=[fo.res])
                B.op("dve", (lambda e, fo=fo, pb=pb, gsb=gsb, j=j, ge=ge: e.tensor_scalar(
                    fo[:, 512:1024], pb[:, :], gsb[:, j, ge:ge + 1], None, OP.mult)),
                    reads=[pb.res, gsb.res], writes=[fo.res])
                tok = slice(ti * 128, (ti + 1) * 128)
                if ex == 0:
                    B.dmaq(d["facc"][tok, :], fo[:, :], reads=[fo.res], writes=[B.facc_res], eng="pool")
                else:
                    B.dmaq(d["facc"][tok, :], fo[:, :], reads=[fo.res], writes=[B.facc_res], eng="pool",
                           accum_op=OP.add)

        for (t0, g) in groups:
            group(t0, g)
    B.phase_end()


def _phase_ln2(B, l, dst_prompt, dst_sample, final):
    d = B.d
    B.phase_begin()
    B.lng = B.sb("lng", [128, D_MODEL])
    B.lnb = B.sb("lnb", [128, D_MODEL])
    B.t_ring = B.ring("tt", [128, 1024], F32, 8)
    B.x_ring = B.t_ring
    _bcast_row(B, B.lng, d["ln2_g"][l], D_MODEL)
    _bcast_row(B, B.lnb, d["ln2_b"][l], D_MODEL)
    for ti in range(B.NT // 128):
        tok = slice(ti * 128, (ti + 1) * 128)
        xt = B.x_ring.next()
        B.dmaq(xt[:], d["x1"][tok, :], writes=[xt.res], reads=[B.x1_res])
        ft = B.x_ring.next()
        B.dmaq(ft[:], d["facc"][tok, :], writes=[ft.res], reads=[B.facc_res])
        tt = B.t_ring.next()
        B.op("dve", (lambda e, tt=tt, xt=xt, ft=ft: e.scalar_tensor_tensor(tt[:, :], xt[:, :], ALPHA, ft[:, :], OP.mult, OP.add)),
             reads=[xt.res, ft.res], writes=[tt.res])
        x2 = B.t_ring.next()
        _layernorm(B, tt, B.lng, B.lnb, x2)
        if ti < B.NPT:
            B.dmaq(dst_prompt[tok, :], x2[:, :], reads=[x2.res], writes=[B.xsrc_res], eng="pool", output=final)
        else:
            B.dmaq(dst_sample[:, :], x2[:, :], reads=[x2.res], writes=[B.xsrc_res], eng="pool", output=final)
    B.phase_end()


def _mixer_a(B, l):
    d = B.d
    TP = B.TP
    SEG = min(1024, TP)
    B.phase_begin()
    B.G = B.ring("G", [128, 1026], F32, 8)
    B.ybf = B.ring("ybf", [128, 1024], BF16, 2)
    B.zprev = B.sb("zprev", [128, 2])
    cw = B.sb("cwa", [128, 2, 3])
    for c in range(2):
        B.dmaq(cw[:, c, :], d["conv_a"][l].rearrange("j (c p) -> p c j", p=128)[:, c, :], writes=[cw.res],
               allow_slow_non_contiguous=True)
    for c in range(2):
        rows = lambda r0: slice(r0 + c * 128, r0 + (c + 1) * 128)

        def seg_fn(s0, n, first, last, c=c, rows=rows):
            ain, agc, agb = B.G.next(), B.G.next(), B.G.next()
            B.dmaq(ain[:, :n], d["uT"][rows(R_AIN), s0:s0 + n], writes=[ain.res], reads=[B.uT_res])
            B.dmaq(agc[:, :n], d["uT"][rows(R_AGC), s0:s0 + n], writes=[agc.res], reads=[B.uT_res])
            B.dmaq(agb[:, :n], d["uT"][rows(R_AGB), s0:s0 + n], writes=[agb.res], reads=[B.uT_res])
            z = B.G.next()
            zp = B.zprev
            if first:
                B.op("pool", lambda e: e.memset(z[:, 0:2], 0.0), writes=[z.res])
            else:
                B.op("pool", lambda e: e.tensor_copy(z[:, 0:2], zp[:, 0:2]), reads=[zp.res], writes=[z.res])
            B.op("pool", lambda e: e.tensor_tensor(z[:, 2:2 + n], agc[:, :n], ain[:, :n], OP.mult),
                 reads=[agc.res, ain.res], writes=[z.res])
            B.op("pool", lambda e: e.tensor_copy(zp[:, 0:2], z[:, n:n + 2]), reads=[z.res], writes=[zp.res])
            y = agc
            B.op("dve", lambda e: e.tensor_scalar(y[:, :n], z[:, 0:n], cw[:, c, 0:1], None, OP.mult),
                 reads=[z.res, cw.res], writes=[y.res])
            B.op("dve", lambda e: e.scalar_tensor_tensor(y[:, :n], z[:, 1:n + 1], cw[:, c, 1:2], y[:, :n], OP.mult, OP.add),
                 reads=[z.res, cw.res, y.res], writes=[y.res])
            B.op("dve", lambda e: e.scalar_tensor_tensor(y[:, :n], z[:, 2:n + 2], cw[:, c, 2:3], y[:, :n], OP.mult, OP.add),
                 reads=[z.res, cw.res, y.res], writes=[y.res])
            yb = B.ybf.next()
            B.op("dve", lambda e: e.tensor_tensor(yb[:, :n], y[:, :n], agb[:, :n], OP.mult),
                 reads=[y.res, agb.res], writes=[yb.res])
            B.dmaq(d["yT"][c * 128:(c + 1) * 128, s0:s0 + n], yb[:, :n], reads=[yb.res], writes=[B.yT_res], eng="pool")
            if last:
                B.dmaq(d["conv_a_prompt"][l].rearrange("j (c p) -> p c j", p=128)[:, c, :], zp[:, 0:2],
                       reads=[zp.res], eng="pool", output=True, allow_slow_non_contiguous=True)

        nseg = TP // SEG
        for s in range(nseg):
            seg_fn(s * SEG, SEG, s == 0, s == nseg - 1)

        def samp(c=c, rows=rows):
            ain, agc, agb, z = B.G.next(), B.G.next(), B.G.next(), B.G.next()
            B.dmaq(ain[:, :128], d["uT"][rows(R_AIN), TP:TP + 128], writes=[ain.res], reads=[B.uT_res])
            B.dmaq(agc[:, :128], d["uT"][rows(R_AGC), TP:TP + 128], writes=[agc.res], reads=[B.uT_res])
            B.dmaq(agb[:, :128], d["uT"][rows(R_AGB), TP:TP + 128], writes=[agb.res], reads=[B.uT_res])
            zv = z[:, 0:192].rearrange("p (b j) -> p b j", j=6)
            for jj in range(2):
                B.dmaq(zv[:, :, jj],
                       d["state_conv_a"][l].rearrange("b j (c p) -> p c j b", p=128)[:, c, jj, :],
                       writes=[z.res], allow_slow_non_contiguous=True)
            v3 = lambda t, n=4: t[:, 0:128].rearrange("p (b j) -> p b j", j=4)
            B.op("pool", lambda e: e.tensor_tensor(zv[:, :, 2:6], v3(agc), v3(ain), OP.mult),
                 reads=[agc.res, ain.res, z.res], writes=[z.res])
            y = agc
            B.op("dve", lambda e: e.tensor_scalar(v3(y), zv[:, :, 0:4], cw[:, c, 0:1], None, OP.mult),
                 reads=[z.res, cw.res], writes=[y.res])
            B.op("dve", lambda e: e.scalar_tensor_tensor(v3(y), zv[:, :, 1:5], cw[:, c, 1:2], v3(y), OP.mult, OP.add),
                 reads=[z.res, cw.res, y.res], writes=[y.res])
            B.op("dve", lambda e: e.scalar_tensor_tensor(v3(y), zv[:, :, 2:6], cw[:, c, 2:3], v3(y), OP.mult, OP.add),
                 reads=[z.res, cw.res, y.res], writes=[y.res])
            yb = B.ybf.next()
            B.op("dve", lambda e: e.tensor_tensor(yb[:, :128], y[:, :128], agb[:, :128], OP.mult),
                 reads=[y.res, agb.res], writes=[yb.res])
            B.dmaq(d["yT"][c * 128:(c + 1) * 128, TP:TP + 128], yb[:, :128], reads=[yb.res], writes=[B.yT_res], eng="pool")
            for jj in range(2):
                B.dmaq(d["conv_a_sample"][l].rearrange("b j (c p) -> p c j b", p=128)[:, c, jj, :],
                       zv[:, :, 4 + jj], reads=[z.res], eng="pool", output=True,
                       allow_slow_non_contiguous=True)
        samp()
    B.phase_end()


def _attn_consts(B):
    d = B.d
    rb = B.rbb
    B.dmaq(rb[:, :], d["rel_bias"].t.rearrange("b h -> (b h)").partition_broadcast(128), writes=[rb.res])
    Bd, Bp = B.Bd, B.Bp
    for (dst, src, nb) in ((Bd, d["c_bkt_diag"], 33), (Bp, d["c_bkt_prev"], 32)):
        def one(bk, dst=dst, src=src):
            oh = B.G.next()
            B.dmaq(oh[:, 0:128], src[:, bk, :], writes=[oh.res])
            for h in range(4):
                if bk == 32:
                    B.op("dve", (lambda e, h=h: e.scalar_tensor_tensor(dst[:, h, :], oh[:, 0:128], -30000.0, dst[:, h, :], OP.mult, OP.add)),
                         reads=[oh.res, dst.res], writes=[dst.res])
                elif bk == 0:
                    B.op("dve", (lambda e, h=h: e.tensor_scalar(dst[:, h, :], oh[:, 0:128], rb[:, bk * 4 + h:bk * 4 + h + 1], None, OP.mult)),
                         reads=[oh.res, rb.res], writes=[dst.res])
                else:
                    B.op("dve", (lambda e, h=h: e.scalar_tensor_tensor(dst[:, h, :], oh[:, 0:128], rb[:, bk * 4 + h:bk * 4 + h + 1], dst[:, h, :], OP.mult, OP.add)),
                         reads=[oh.res, rb.res, dst.res], writes=[dst.res])
        for bk in range(nb):
            one(bk)


def _lambda(B, l):
    d = B.d
    lam_init = 0.8 - 0.6 * math.exp(-0.3 * l)
    t = B.sm_ring.next()
    lv = B.G.next()
    for i, nm in enumerate(("lambda_q1", "lambda_k1", "lambda_q2", "lambda_k2")):
        B.dmaq(lv[:, i * 32:(i + 1) * 32], d[nm][l].partition_broadcast(128), writes=[lv.res])
    B.op("dve", lambda e: e.tensor_tensor(lv[:, 128:192].rearrange("p (a c) -> p a c", a=2), lv[:, 0:128].rearrange("p (a b c) -> p a b c", a=2, b=2)[:, :, 0, :],
                                          lv[:, 0:128].rearrange("p (a b c) -> p a b c", a=2, b=2)[:, :, 1, :], OP.mult),
         reads=[lv.res], writes=[lv.res])
    B.op("dve", lambda e: e.tensor_reduce(t[:, 0:2], lv[:, 128:192].rearrange("p (a c) -> p a c", a=2), AX.X, OP.add),
         reads=[lv.res], writes=[t.res])
    B.op("act", lambda e: e.activation(t[:, 0:2], t[:, 0:2], AF.Exp), reads=[t.res], writes=[t.res])
    lam = B.lam
    B.op("dve", lambda e: e.tensor_tensor(lam[:, l:l + 1], t[:, 0:1], t[:, 1:2], OP.subtract), reads=[t.res], writes=[lam.res])
    B.op("dve", lambda e: e.tensor_scalar(lam[:, l:l + 1], lam[:, l:l + 1], lam_init, None, OP.add), reads=[lam.res], writes=[lam.res])
    B.op("dve", lambda e: e.tensor_scalar(lam[:, 2 + l:3 + l], lam[:, l:l + 1], -1.0, None, OP.mult), reads=[lam.res], writes=[lam.res])
    return lam_init


def _diff_combine(B, l, lam_init, O1, O2, ncols_tok, nw, yrow0, tok0, h):
    d = B.d
    w = B.sm_ring.next()
    o = B.ev_ring.next()
    lam = B.lam
    B.op("dve", lambda e: e.reciprocal(w[:, 0:1], O1[:, 64:65]), reads=nw, writes=[w.res])
    B.op("dve", lambda e: e.reciprocal(w[:, 1:2], O2[:, 64:65]), reads=nw, writes=[w.res])
    B.op("dve", lambda e: e.tensor_tensor(w[:, 1:2], w[:, 1:2], lam[:, 2 + l:3 + l], OP.mult), reads=[w.res, lam.res], writes=[w.res])
    B.op("dve", lambda e: e.tensor_scalar(o[:, 0:64], O1[:, 0:64], w[:, 0:1], None, OP.mult), reads=nw + [w.res], writes=[o.res])
    B.op("dve", lambda e: e.scalar_tensor_tensor(o[:, 0:64], O2[:, 0:64], w[:, 1:2], o[:, 0:64], OP.mult, OP.add),
         reads=nw + [w.res, o.res], writes=[o.res])
    B.op("dve", lambda e: e.tensor_tensor(o[:, 64:128], o[:, 0:64], o[:, 0:64], OP.mult), reads=[o.res], writes=[o.res])
    B.op("dve", lambda e: e.tensor_reduce(w[:, 2:3], o[:, 64:128], AX.X, OP.add), reads=[o.res], writes=[w.res])
    B.op("dve", lambda e: e.tensor_scalar(w[:, 4:5], w[:, 2:3], 64 * RMS_EPS, None, OP.add), reads=[w.res], writes=[w.res])
    B.op("act", lambda e: e.activation(w[:, 4:5], w[:, 4:5], AF.Ln), reads=[w.res], writes=[w.res])
    B.op("act", lambda e: e.activation(w[:, 4:5], w[:, 4:5], AF.Exp, scale=-0.5), reads=[w.res], writes=[w.res])
    B.op("dve", lambda e: e.tensor_scalar(w[:, 3:4], w[:, 4:5], 8.0 * (1.0 - lam_init), None, OP.mult), reads=[w.res], writes=[w.res])
    B.op("dve", lambda e: e.scalar_tensor_tensor(o[:, 128:192], o[:, 0:64], w[:, 3:4], B.nrmd[:, l * 64:(l + 1) * 64], OP.mult, OP.mult),
         reads=[o.res, w.res, B.nrmd.res], writes=[o.res])
    pt = B.psum[7]
    B.tr(pt[0:64, 0:128], o[:, 128:192], B.ident[:], reads=[o.res, B.ident.res], writes=[pt.res])
    yb = B.ybf.next()
    B.op("act", lambda e: e.copy(yb[0:64, 0:128], pt[0:64, 0:128]), reads=[pt.res], writes=[yb.res])
    B.dmaq(d["yT"][yrow0:yrow0 + 64, tok0:tok0 + 128], yb[0:64, 0:128], reads=[yb.res], writes=[B.yT_res], eng="pool")


def _mixer_d_prompt(B, l, lam_init):
    d = B.d
    TP, NPT = B.TP, B.NPT
    SC = 32 ** -0.5
    B.phase_begin()
    qTb = B.sb("qTb", [64, TP], BF16)
    kTb = B.sb("kTb", [64, TP], BF16)
    Vaug = B.sb("Vaug", [128, NPT, 65], BF16)
    B.E_ring = B.ring("E", [128, 512], BF16, 4)
    zl = B.sb("zl", [128, 128], BF16)
    zr = B.sb("zr", [128, 260], BF16)
    B.op("pool", lambda e: e.memset(zl[:, :], 0.0), writes=[zl.res])
    B.op("pool", lambda e: e.memset(zr[:, :], 0.0), writes=[zr.res])
    B.ev_ring = B.ring("ev", [128, 512], F32, 4)
    B.G = B.ring("G", [128, 1026], F32, 4)
    B.ybf = B.ring("ybf", [128, 1024], BF16, 2)
    rb = B.rbb
    for h in range(4):
        def load_head(h=h):
            for (dst, r0) in ((qTb, R_DQ), (kTb, R_DK)):
                for m in range(2):
                    row = r0 + h * 64 + m * 32
                    for s0 in range(0, TP, 1024):
                        n = min(1024, TP - s0)
                        st = B.G.next()
                        B.dmaq(st[m * 32:(m + 1) * 32, :n], d["uT"][row:row + 32, s0:s0 + n], writes=[st.res], reads=[B.uT_res])
                        B.op("pool", (lambda e, st=st, dst=dst, m=m, s0=s0, n=n: e.tensor_copy(dst[m * 32:(m + 1) * 32, s0:s0 + n], st[m * 32:(m + 1) * 32, :n])),
                             reads=[st.res], writes=[dst.res])
            vs = B.G.next()
            for j0 in range(0, NPT, 16):
                nj = min(16, NPT - j0)
                B.dmaq(vs[:, :nj * 64].rearrange("p (j d) -> p j d", d=64),
                       d["uM"][j0 * 128:(j0 + nj) * 128, C_DV + h * 64:C_DV + (h + 1) * 64].rearrange("(j p) d -> p j d", p=128),
                       writes=[vs.res], reads=[B.uM_res])
                B.op("pool", (lambda e, j0=j0, nj=nj: e.tensor_copy(Vaug[:, j0:j0 + nj, 0:64], vs[:, :nj * 64].rearrange("p (j d) -> p j d", d=64))),
                     reads=[vs.res], writes=[Vaug.res])
            B.op("pool", lambda e: e.memset(Vaug[:, :, 64:65], 1.0), writes=[Vaug.res])
        load_head()
        nqt = (NPT + 3) // 4
        for qt in range(nqt):
            def qtile(qt=qt, h=h):
                ns = min(4, NPT - qt * 4)
                nq = ns * 128
                OA, OB = B.psum[2 * (qt % 2)], B.psum[2 * (qt % 2) + 1]
                jmax = qt * 4 + ns - 1
                for OO in (OA, OB):
                    B.mm(OO[:, 0:ns * 65], zl[:, :], zr[:, 0:ns * 65], True, False, reads=[zl.res, zr.res], writes=[OO.res])
                for j in range(jmax + 1):
                    for m, OO in ((0, OA), (1, OB)):
                        sp = _psum1s(B)
                        B.mm(sp[:, :nq], kTb[m * 32:(m + 1) * 32, j * 128:(j + 1) * 128], qTb[m * 32:(m + 1) * 32, qt * 512:qt * 512 + nq], True, True,
                             reads=[kTb.res, qTb.res], writes=[sp.res])
                        E = B.E_ring.next()
                        far_all = (j <= qt * 4 - 2)
                        if far_all:
                            B.op("act", (lambda e, sp=sp, E=E: e.activation(E[:, :nq], sp[:, :nq], AF.Exp,
                                                                           bias=rb[:, 31 * 4 + h:31 * 4 + h + 1], scale=SC)),
                                 reads=[sp.res, rb.res], writes=[E.res])
                            subs = list(range(ns))
                        else:
                            subs = []
                            for s in range(ns):
                                i = qt * 4 + s
                                cs = slice(s * 128, (s + 1) * 128)
                                if j > i:
                                    continue
                                subs.append(s)
                                if j <= i - 2:
                                    B.op("act", (lambda e, sp=sp, E=E, cs=cs: e.activation(E[:, cs], sp[:, cs], AF.Exp,
                                                                                          bias=rb[:, 31 * 4 + h:31 * 4 + h + 1], scale=SC)),
                                         reads=[sp.res, rb.res], writes=[E.res])
                                else:
                                    Bm = B.Bd if j == i else B.Bp
                                    tmp = B.ev_ring.next()
                                    B.op("dve", (lambda e, sp=sp, tmp=tmp, cs=cs, Bm=Bm: e.scalar_tensor_tensor(
                                        tmp[:, 0:128], sp[:, cs], SC, Bm[:, h, :], OP.mult, OP.add)),
                                        reads=[sp.res, Bm.res], writes=[tmp.res])
                                    B.op("act", (lambda e, tmp=tmp, E=E, cs=cs: e.activation(E[:, cs], tmp[:, 0:128], AF.Exp)),
                                         reads=[tmp.res], writes=[E.res])
                        for s in subs:
                            i = qt * 4 + s
                            B.mm(OO[:, s * 65:(s + 1) * 65], E[:, s * 128:(s + 1) * 128], Vaug[:, j, :], False, (j == jmax and s == ns - 1),
                                 reads=[E.res, Vaug.res], writes=[OO.res])
                for s in range(ns):
                    i = qt * 4 + s
                    _diff_combine(B, l, lam_init, OA[:, s * 65:(s + 1) * 65], OB[:, s * 65:(s + 1) * 65], 128,
                                  [OA.res, OB.res], 768 + h * 64, i * 128, h)
            qtile()
    B.phase_end()


def _psum1s(B):
    p = getattr(B, "ps_s", 0)
    B.ps_s = p + 1
    return B.psum[4 + (p % 3)]


_CACHE = {}


def kernel(**inputs):
    NC = 8
    NLP = 5120
    if "B" not in _CACHE:
        _CACHE["B"] = build(NPT=32, n_cores=1, n_local_pages=NLP, n_pages_seq=128)
    B = _CACHE["B"]
    f = lambda a: np.ascontiguousarray(np.asarray(a))
    shared = {}
    for nm in ("w_in", "conv_a", "conv_b", "gdn_a_log", "gdn_dt_bias", "norm_b", "lower_bounds", "norm_c",
               "lambda_q1", "lambda_k1", "lambda_q2", "lambda_k2", "norm_d", "rel_bias", "w_o", "ln1_g", "ln1_b",
               "ffn_w_gate", "ffn_w_up", "ffn_w_down", "router_w", "moe_w_gate", "moe_w_up", "moe_w_down",
               "ln2_g", "ln2_b", "state_conv_a", "state_conv_b", "page_table"):
        shared[nm] = f(inputs[nm])
    shared["x_sample"] = f(inputs["x_sample"]).reshape(128, D_MODEL)
    shared["state_gdn"] = f(inputs["state_gdn"]).reshape(2, 128, 4096)
    shared["state_hgrn"] = f(inputs["state_hgrn"]).reshape(2, 128, 4096)
    ck = np.asarray(inputs["cache_k"]).reshape(2, NLP * 128, 256)
    cv = np.asarray(inputs["cache_v"]).reshape(2, NLP * 128, 256)
    for l_ in range(2):
        shared["cache_k%d" % l_] = f(ck[l_])
        shared["cache_v%d" % l_] = f(cv[l_])
    consts = _host_consts(0, NLP)
    xp = np.asarray(inputs["x_prompt"])
    in_maps = []
    for c in range(NC):
        m = dict(shared)
        m["x_prompt"] = f(xp[c])
        ident, misc, diag, prev, masks, scan, cnew, rep, pid, lastb, ind = consts
        m["pt_own"] = f(np.asarray(inputs["page_table"])[4 * c:4 * c + 4].reshape(-1).astype(np.int32))
        m["c_ownL"], m["c_isl"] = _own_consts(c, 128)
        m["c_ind"] = ind
        m["c_ident"], m["c_misc"], m["c_bkt_diag"], m["c_bkt_prev"] = ident, misc, diag, prev
        m["c_masks"], m["c_scan"] = masks, scan
        m["c_new"], m["c_rep"], m["c_pid"], m["c_lastb"] = cnew, rep, pid, lastb
        in_maps.append(m)
    res = run_bass_kernel_spmd(B.nc, in_maps, core_ids=list(range(NC))).results
    st = lambda nm: np.stack([res[c][nm] for c in range(NC)], 0)
    y_prompt = st("y_prompt")
    def samp(nm, lead):
        outs_ = []
        for c in range(NC):
            a = res[c][nm].reshape(lead + (32, -1))
            outs_.append(a[..., 4 * c:4 * c + 4, :])
        return np.concatenate(outs_, axis=len(lead))
    y_sample = samp("y_sample", ()).reshape(32, 4, D_MODEL)
    k_p = st("k_prompt").transpose(1, 0, 2, 3).reshape(2, NC, 4096, 4, 64)
    v_p = st("v_prompt").transpose(1, 0, 2, 3).reshape(2, NC, 4096, 4, 64)
    k_s = samp("k_sample", (2,)).reshape(2, 32, 4, 4, 64)
    v_s = samp("v_sample", (2,)).reshape(2, 32, 4, 4, 64)
    ca_p = st("conv_a_prompt").transpose(1, 0, 2, 3)
    ca_s = samp("conv_a_sample", (2,)).reshape(2, 32, 2, 256)
    cb_p = st("conv_b_prompt").transpose(1, 0, 2, 3)
    cb_s = samp("conv_b_sample", (2,)).reshape(2, 32, 3, 768)
    g_p = st("gdn_prompt").transpose(1, 0, 2, 3).reshape(2, NC, 4, 64, 64)
    g_s = samp("gdn_sample", (2,)).reshape(2, 32, 4, 64, 64)
    h_p = st("hgrn_prompt").transpose(1, 0, 2, 3).reshape(2, NC, 4, 64, 64)
    h_s = samp("hgrn_sample", (2,)).reshape(2, 32, 4, 64, 64)
    outs = (y_prompt, y_sample, k_p, v_p, k_s, v_s, ca_p, ca_s, cb_p, cb_s, g_p, g_s, h_p, h_s)
    return tuple(np.ascontiguousarray(o, dtype=np.float32) for o in outs)


def _norm_gate_out(B, o_all, z, nw, yrow0, tok0):
    d = B.d
    sq = B.W256.next()
    st = B.sm_ring.next()
    B.op("pool", lambda e: e.tensor_tensor(sq[:, :], o_all[:, :], o_all[:, :], OP.mult), reads=[o_all.res], writes=[sq.res])
    B.op("dve", lambda e: e.tensor_reduce(st[:, 0:4], sq[:, :].rearrange("p (h d) -> p h d", h=4), AX.X, OP.add),
         reads=[sq.res], writes=[st.res])
    B.op("dve", lambda e: e.tensor_scalar(st[:, 4:8], st[:, 0:4], 1.0 / 64, RMS_EPS, OP.mult, OP.add), reads=[st.res], writes=[st.res])
    B.op("act", lambda e: e.activation(st[:, 4:8], st[:, 4:8], AF.Ln), reads=[st.res], writes=[st.res])
    B.op("act", lambda e: e.activation(st[:, 4:8], st[:, 4:8], AF.Exp, scale=-0.5), reads=[st.res], writes=[st.res])
    sz = B.W256.next()
    B.op("act", lambda e: e.activation(sz[:, :], z, AF.Silu), reads=[B.zres], writes=[sz.res])
    on = sq
    B.op("dve", lambda e: e.tensor_tensor(on[:, :].rearrange("p (h d) -> p h d", h=4), o_all[:, :].rearrange("p (h d) -> p h d", h=4),
                                          st[:, 4:8].unsqueeze(2).broadcast_to([128, 4, 64]), OP.mult),
         reads=[o_all.res, st.res], writes=[on.res])
    B.op("pool", lambda e: e.tensor_tensor(on[:, :], on[:, :], nw[:, :], OP.mult), reads=[on.res, nw.res], writes=[on.res])
    B.op("pool", lambda e: e.tensor_tensor(on[:, :], on[:, :], sz[:, :], OP.mult), reads=[on.res, sz.res], writes=[on.res])
    pt = _psr(B)
    for c in range(2):
        B.tr(pt[:, c * 128:(c + 1) * 128], on[:, c * 128:(c + 1) * 128], B.ident[:], reads=[on.res, B.ident.res], writes=[pt.res])
    yb = B.ybf.next()
    B.op("act", lambda e: e.copy(yb[:, 0:256], pt[:, 0:256]), reads=[pt.res], writes=[yb.res])
    for c in range(2):
        B.dmaq(d["yT"][yrow0 + c * 128:yrow0 + (c + 1) * 128, tok0:tok0 + 128], yb[:, c * 128:(c + 1) * 128],
               reads=[yb.res], writes=[B.yT_res], eng="pool")


def _psr(B):
    p = getattr(B, "ps_r", 0)
    B.ps_r = p + 1
    return B.psum[p % 6]


def _mixer_c_prompt(B, l):
    d = B.d
    TP, NPT = B.TP, B.NPT
    SEG = min(1024, TP)
    M = B.masks
    B.phase_begin()
    B.G = B.ring("G", [128, 1024], F32, 14)
    B.W128 = B.ring("W128", [128, 128], F32, 12)
    B.W256 = B.ring("W256", [128, 256], F32, 8)
    B.ybf = B.ring("ybf", [128, 256], BF16, 3)
    scanm = B.sb("scanm", [128, 1024])
    B.dmaq(scanm[:, :], d["c_scan"][:, :], writes=[scanm.res])
    nw = B.sb("nwc", [128, 256])
    for h in range(4):
        B.dmaq(nw[:, h * 64:(h + 1) * 64], d["norm_c"][l].partition_broadcast(128), writes=[nw.res])
    lbc = B.sb("lbc", [128, 2, 4])
    for pc in range(2):
        if l == 0:
            B.op("pool", (lambda e, pc=pc: e.memset(lbc[:, pc, 0:1], 0.0)), writes=[lbc.res])
        else:
            lbt = B.sm_ring.next()
            B.dmaq(lbt[:, 0:2], d["lower_bounds"].t.rearrange("l (c p) -> p c l", p=128)[:, pc, :], writes=[lbt.res],
                   allow_slow_non_contiguous=True)
            B.op("dve", (lambda e, pc=pc, lbt=lbt: e.tensor_tensor(lbc[:, pc, 3:4], lbt[:, 1:2], lbt[:, 0:1], OP.subtract)),
                 reads=[lbt.res], writes=[lbc.res])
            B.op("act", (lambda e, pc=pc: e.activation(lbc[:, pc, 0:1], lbc[:, pc, 3:4], AF.Sigmoid)), reads=[lbc.res], writes=[lbc.res])
        B.op("dve", (lambda e, pc=pc: e.tensor_scalar(lbc[:, pc, 1:2], lbc[:, pc, 0:1], -1.0, 1.0, OP.mult, OP.add)), reads=[lbc.res], writes=[lbc.res])
        B.op("dve", (lambda e, pc=pc: e.tensor_scalar(lbc[:, pc, 2:3], lbc[:, pc, 1:2], -1.0, None, OP.mult)), reads=[lbc.res], writes=[lbc.res])
    kz = [[B.sb("kz%d_%d" % (c, i), [128, 128]) for i in range(3)] for c in range(2)]
    qz = [[B.sb("qz%d_%d" % (c, i), [128, 128]) for i in range(3)] for c in range(2)]
    for c in range(2):
        for i in range(3):
            for t_ in (kz[c][i], qz[c][i]):
                B.op("pool", (lambda e, t_=t_: e.memset(t_[:, :], 0.0)), writes=[t_.res])
    Sst = [[B.sb("S%d_%d" % (pc, i), [128, 64]) for i in range(3)] for pc in range(2)]
    for pc in range(2):
        B.op("pool", (lambda e, pc=pc: e.memset(Sst[pc][0][:, :], 0.0)), writes=[Sst[pc][0].res])
    sidx = [0, 0]
    kzi = [0]
    scb = [[B.sb("scb%d_%d" % (pc, i), [128, 64]) for i in range(2)] for pc in range(2)]
    nseg = TP // SEG
    for sg in range(nseg):
        def seg(sg=sg):
            s0 = sg * SEG
            nch = SEG // 64
            feat = []
            for pc in range(2):
                cq, cf = B.G.next(), B.G.next()
                B.dmaq(cq[:, :SEG], d["uT"][R_CQ + pc * 128:R_CQ + (pc + 1) * 128, s0:s0 + SEG], writes=[cq.res], reads=[B.uT_res])
                B.dmaq(cf[:, :SEG], d["uT"][R_CF + pc * 128:R_CF + (pc + 1) * 128, s0:s0 + SEG], writes=[cf.res], reads=[B.uT_res])
                sig, lf, kc, bb = B.G.next(), B.G.next(), B.G.next(), B.G.next()
                B.op("act", (lambda e, sig=sig, cf=cf: e.activation(sig[:, :SEG], cf[:, :SEG], AF.Sigmoid)), reads=[cf.res], writes=[sig.res])
                B.op("dve", (lambda e, lf=lf, sig=sig, pc=pc: e.tensor_scalar(lf[:, :SEG], sig[:, :SEG], lbc[:, pc, 1:2], lbc[:, pc, 0:1], OP.mult, OP.add)),
                     reads=[sig.res, lbc.res], writes=[lf.res])
                B.op("act", (lambda e, lf=lf: e.activation(lf[:, :SEG], lf[:, :SEG], AF.Ln)), reads=[lf.res], writes=[lf.res])
                B.op("dve", (lambda e, kc=kc, sig=sig, pc=pc: e.tensor_scalar(kc[:, :SEG], sig[:, :SEG], lbc[:, pc, 2:3], lbc[:, pc, 1:2], OP.mult, OP.add)),
                     reads=[sig.res, lbc.res], writes=[kc.res])
                B.op("dve", (lambda e, bb=bb, lf=lf: e.tensor_tensor_scan(bb[:, :SEG], scanm[:, :SEG], lf[:, :SEG], 0.0, OP.mult, OP.add)),
                     reads=[lf.res, scanm.res], writes=[bb.res])
                sq_ = cf
                B.op("act", (lambda e, sq_=sq_, cq=cq: e.activation(sq_[:, :SEG], cq[:, :SEG], AF.Silu)), reads=[cq.res], writes=[sq_.res])
                sc = scb[pc][sg % 2]
                b3 = lambda t_: t_[:, :SEG].rearrange("p (c j) -> p c j", j=64)
                B.op("pool", (lambda e, sc=sc, bb=bb: e.tensor_copy(sc[:, 0:nch], b3(bb)[:, :, 31])), reads=[bb.res], writes=[sc.res])
                B.op("pool", (lambda e, sc=sc, bb=bb: e.tensor_copy(sc[:, 16:16 + nch], b3(bb)[:, :, 63])), reads=[bb.res], writes=[sc.res])
                B.op("dve", (lambda e, sc=sc: e.tensor_tensor(sc[:, 32:32 + nch], sc[:, 16:16 + nch], sc[:, 0:nch], OP.subtract)), reads=[sc.res], writes=[sc.res])
                B.op("act", (lambda e, sc=sc: e.activation(sc[:, 32:32 + nch], sc[:, 32:32 + nch], AF.Exp)), reads=[sc.res], writes=[sc.res])
                B.op("act", (lambda e, sc=sc: e.activation(sc[:, 48:48 + nch], sc[:, 16:16 + nch], AF.Exp)), reads=[sc.res], writes=[sc.res])
                dl, qt_, kt_, qe = lf, B.G.next(), sig, cq
                B.op("dve", (lambda e, dl=dl, bb=bb, sc=sc: e.tensor_tensor(b3(dl), b3(bb), sc[:, 0:nch].unsqueeze(2).broadcast_to([128, nch, 64]), OP.subtract)),
                     reads=[bb.res, sc.res], writes=[dl.res])
                B.op("act", (lambda e, qt_=qt_, dl=dl: e.activation(qt_[:, :SEG], dl[:, :SEG], AF.Exp)), reads=[dl.res], writes=[qt_.res])
                B.op("act", (lambda e, kt_=kt_, dl=dl: e.activation(kt_[:, :SEG], dl[:, :SEG], AF.Exp, scale=-1.0)), reads=[dl.res], writes=[kt_.res])
                B.op("act", (lambda e, qe=qe, bb=bb: e.activation(qe[:, :SEG], bb[:, :SEG], AF.Exp)), reads=[bb.res], writes=[qe.res])
                B.op("pool", (lambda e, qt_=qt_, sq_=sq_: e.tensor_tensor(qt_[:, :SEG], qt_[:, :SEG], sq_[:, :SEG], OP.mult)), reads=[qt_.res, sq_.res], writes=[qt_.res])
                B.op("pool", (lambda e, kt_=kt_, kc=kc: e.tensor_tensor(kt_[:, :SEG], kt_[:, :SEG], kc[:, :SEG], OP.mult)), reads=[kt_.res, kc.res], writes=[kt_.res])
                B.op("pool", (lambda e, qe=qe, sq_=sq_: e.tensor_tensor(qe[:, :SEG], qe[:, :SEG], sq_[:, :SEG], OP.mult)), reads=[qe.res, sq_.res], writes=[qe.res])
                feat.append((qt_, kt_, qe, sc))
                if B.debug and l == 0 and sg == 0 and pc == 0:
                    for nm, t_ in (("dbgc_b", bb), ("dbgc_q", qt_), ("dbgc_k", kt_), ("dbgc_qe", qe), ("dbgc_lf", kc)):
                        dd = B.dram(nm, [128, SEG])
                        B.dmaq(dd[:, :], t_[:, :SEG], reads=[t_.res], output=True)
                    dd = B.dram("dbgc_sc", [128, 64])
                    B.dmaq(dd[:, :], sc[:, :], reads=[sc.res], output=True)
            for tl in range(SEG // 128):
                def tile(tl=tl):
                    ti = s0 // 128 + tl
                    tok0 = ti * 128
                    c0 = tl * 128
                    vg = B.W256.next()
                    zg = B.W256.next()
                    B.dmaq(vg[:, :], d["uM"][tok0:tok0 + 128, C_CI:C_CI + 256], writes=[vg.res], reads=[B.uM_res])
                    B.dmaq(zg[:, :], d["uM"][tok0:tok0 + 128, C_CG:C_CG + 256], writes=[zg.res], reads=[B.uM_res])
                    po = B.psum[6 + (ti % 2)]
                    for pc in range(2):
                        qt_, kt_, qe, sc = feat[pc]
                        ki = kzi[0] % 3
                        kzi[0] += 1
                        kz0, kz1, qz0, qz1 = kz[0][ki], kz[1][ki], qz[0][ki], qz[1][ki]
                        B.op("pool", (lambda e, kz0=kz0, kt_=kt_: e.tensor_copy(kz0[:, 0:64], kt_[:, c0:c0 + 64])), reads=[kt_.res], writes=[kz0.res])
                        B.op("pool", (lambda e, kz1=kz1, kt_=kt_: e.tensor_copy(kz1[:, 64:128], kt_[:, c0 + 64:c0 + 128])), reads=[kt_.res], writes=[kz1.res])
                        B.op("pool", (lambda e, qz0=qz0, qe=qe: e.tensor_copy(qz0[:, 0:64], qe[:, c0:c0 + 64])), reads=[qe.res], writes=[qz0.res])
                        B.op("pool", (lambda e, qz1=qz1, qe=qe: e.tensor_copy(qz1[:, 64:128], qe[:, c0 + 64:c0 + 128])), reads=[qe.res], writes=[qz1.res])
                        ptk = _psr(B)
                        B.tr(ptk[:, 0:128], kz0[:, :], B.ident[:], reads=[kz0.res, B.ident.res], writes=[ptk.res])
                        B.tr(ptk[:, 128:256], kz1[:, :], B.ident[:], reads=[kz1.res, B.ident.res], writes=[ptk.res])
                        kzT = B.W256.next()
                        B.op("act", (lambda e, kzT=kzT, ptk=ptk: e.copy(kzT[:, :], ptk[:, 0:256])), reads=[ptk.res], writes=[kzT.res])
                        S0 = Sst[pc][sidx[pc] % 3]
                        S1 = Sst[pc][(sidx[pc] + 1) % 3]
                        S2 = Sst[pc][(sidx[pc] + 2) % 3]
                        sidx[pc] += 2
                        ch = (c0 // 64)
                        for (cc, Sa, Sb_) in ((0, S0, S1), (1, S1, S2)):
                            pS = _psr(B)
                            B.mm(pS[:, 0:128], kzT[:, cc * 128:(cc + 1) * 128], vg[:, pc * 128:(pc + 1) * 128], True, True,
                                 reads=[kzT.res, vg.res], writes=[pS.res])
                            for hh in range(2):
                                rows = slice(hh * 64, (hh + 1) * 64)
                                tmp = B.W128.next()
                                B.op("dve", (lambda e, tmp=tmp, pS=pS, rows=rows, hh=hh, sc=sc, cc=cc: e.tensor_scalar(
                                    tmp[rows, 0:64], pS[rows, hh * 64:(hh + 1) * 64], sc[rows, 32 + ch + cc:33 + ch + cc], None, OP.mult)),
                                    reads=[pS.res, sc.res], writes=[tmp.res])
                                B.op("dve", (lambda e, tmp=tmp, Sa=Sa, Sb_=Sb_, rows=rows, sc=sc, cc=cc: e.scalar_tensor_tensor(
                                    Sb_[rows, :], Sa[rows, :], sc[rows, 48 + ch + cc:49 + ch + cc], tmp[rows, 0:64], OP.mult, OP.add)),
                                    reads=[Sa.res, tmp.res, sc.res], writes=[Sb_.res])
                        for hh in range(2):
                            h = pc * 2 + hh
                            rows = slice(hh * 64, (hh + 1) * 64)
                            pa = _psr(B)
                            B.mm(pa[:, 0:64], kz0[rows, :], qt_[rows, c0:c0 + 64], True, True, reads=[kz0.res, qt_.res], writes=[pa.res])
                            B.mm(pa[:, 64:128], kz1[rows, :], qt_[rows, c0 + 64:c0 + 128], True, True, reads=[kz1.res, qt_.res], writes=[pa.res])
                            aT = B.W128.next()
                            B.op("dve", (lambda e, aT=aT, pa=pa: e.tensor_tensor(aT[:, :], pa[:, 0:128], M[:, 1, :], OP.mult)),
                                 reads=[pa.res, M.res], writes=[aT.res])
                            oc = slice(h * 64, (h + 1) * 64)
                            B.mm(po[:, oc], aT[:, :], vg[:, oc], True, False, reads=[aT.res, vg.res], writes=[po.res])
                            B.mm(po[:, oc], qz0[rows, :], S0[rows, :], False, False, reads=[qz0.res, S0.res], writes=[po.res])
                            B.mm(po[:, oc], qz1[rows, :], S1[rows, :], False, True, reads=[qz1.res, S1.res], writes=[po.res])
                    o_all = B.W256.next()
                    B.op("act", (lambda e, o_all=o_all, po=po: e.copy(o_all[:, :], po[:, 0:256])), reads=[po.res], writes=[o_all.res])
                    if B.debug and l == 0 and ti == 0:
                        dd = B.dram("dbgc_o", [128, 256])
                        B.dmaq(dd[:, :], o_all[:, :], reads=[o_all.res], output=True)
                    B.zres = zg.res
                    _norm_gate_out(B, o_all, zg[:, :], nw, 512, tok0)
                tile()
        seg()
    if B.debug and l == 0:
        for i in range(3):
            dd = B.dram("dbgc_S%d" % i, [128, 64])
            B.dmaq(dd[:, :], Sst[0][i][:, :], reads=[Sst[0][i].res], output=True)
    for pc in range(2):
        Sf = Sst[pc][sidx[pc] % 3]
        B.dmaq(d["hgrn_prompt"][l, pc * 128:(pc + 1) * 128, :], Sf[:, :], reads=[Sf.res], output=True)
    B.phase_end()


def _mixer_b_prompt(B, l):
    d = B.d
    TP, NPT = B.TP, B.NPT
    M = B.masks
    ident = B.ident
    B.phase_begin()
    B.W256 = B.ring("W256", [128, 256], F32, 6)
    B.ybf = B.ring("ybf", [128, 256], BF16, 3)
    cw = B.sb("cwb", [128, 4, 768])
    B.dmaq(cw[:, :, :].rearrange("p j c -> p (j c)"), d["conv_b"][l].rearrange("j c -> (j c)").partition_broadcast(128), writes=[cw.res])
    nAb = B.sb("nAb", [128, 8])
    B.dmaq(nAb[:, 0:4], d["gdn_a_log"][l].partition_broadcast(128), writes=[nAb.res])
    B.dmaq(nAb[:, 4:8], d["gdn_dt_bias"][l].partition_broadcast(128), writes=[nAb.res])
    B.op("act", lambda e: e.activation(nAb[:, 0:4], nAb[:, 0:4], AF.Exp), reads=[nAb.res], writes=[nAb.res])
    B.op("dve", lambda e: e.tensor_scalar(nAb[:, 0:4], nAb[:, 0:4], -1.0, None, OP.mult), reads=[nAb.res], writes=[nAb.res])
    nwb = B.sb("nwb", [128, 256])
    for h in range(4):
        B.dmaq(nwb[:, h * 64:(h + 1) * 64], d["norm_b"][l].partition_broadcast(128), writes=[nwb.res])
    Xs = [B.sb("X%d" % j, [128, 768]) for j in range(4)]
    acc = B.sb("acc", [128, 768])
    tmpc = B.sb("tmpc", [128, 768])
    qkv = B.sb("qkv", [128, 768])
    sqb = B.sb("sqb", [128, 512])
    smx = B.sb("smx", [128, 64])
    kn, qn, kb, vb, qg = [B.sb(nm, [128, 256]) for nm in ("kn", "qn", "kb", "vb", "qg")]
    bab = B.sb("bab", [128, 8])
    zt = B.sb("ztb", [128, 256])
    kbgz = B.sb("kbgz", [128, 4, 128])
    kdecz = B.sb("kdecz", [128, 4, 128])
    B.op("pool", lambda e: e.memset(kbgz[:, :, :], 0.0), writes=[kbgz.res])
    B.op("pool", lambda e: e.memset(kdecz[:, :, :], 0.0), writes=[kdecz.res])
    knT, kbT, qnT, qgT = [B.sb(nm, [128, 2, 128]) for nm in ("knT", "kbT", "qnT", "qgT")]
    o_all = B.sb("o_allb", [128, 256])
    H = []
    for h in range(4):
        hb = {}
        for nm in ("diag", "arg", "Dst", "PT", "Xa", "Xb", "Ya", "Yb", "TT", "wT"):
            hb[nm] = B.sb("%s%d" % (nm, h), [128, 128])
        for nm in ("u", "vnew"):
            hb[nm] = B.sb("%s%d" % (nm, h), [128, 64])
        H.append(hb)
    Sp = [[B.sb("Sb%d_%d" % (blk, i), [128, 64]) for i in range(2)] for blk in range(2)]
    for blk in range(2):
        B.op("pool", (lambda e, blk=blk: e.memset(Sp[blk][0][:, :], 0.0)), writes=[Sp[blk][0].res])
    bc3 = lambda col: col.unsqueeze(2).broadcast_to([128, 4, 64])
    v3 = lambda ap: ap.rearrange("p (h d) -> p h d", h=4)

    for ti in range(NPT):
        def tile(ti=ti):
            tok0 = ti * 128
            Sin = [Sp[blk][ti % 2] for blk in range(2)]
            Sout = [Sp[blk][(ti + 1) % 2] for blk in range(2)]
            for j in range(4):
                sh = 3 - j
                if ti == 0 and sh > 0:
                    B.op("pool", (lambda e, j=j, sh=sh: e.memset(Xs[j][0:sh, :], 0.0)), writes=[Xs[j].res])
                    B.dmaq(Xs[j][sh:128, :], d["uM"][0:128 - sh, C_BQKV:C_BQKV + 768], writes=[Xs[j].res], reads=[B.uM_res])
                else:
                    B.dmaq(Xs[j][:, :], d["uM"][tok0 - sh:tok0 - sh + 128, C_BQKV:C_BQKV + 768], writes=[Xs[j].res], reads=[B.uM_res])
            B.dmaq(bab[:, :], d["uM"][tok0:tok0 + 128, C_BA:C_BA + 8], writes=[bab.res], reads=[B.uM_res])
            B.dmaq(zt[:, :], d["uM"][tok0:tok0 + 128, C_BZ:C_BZ + 256], writes=[zt.res], reads=[B.uM_res])
            B.op("dve", lambda e: e.tensor_tensor(acc[:, :], Xs[0][:, :], cw[:, 0, :], OP.mult), reads=[Xs[0].res, cw.res], writes=[acc.res])
            for j in range(1, 4):
                B.op("pool", (lambda e, j=j: e.tensor_tensor(tmpc[:, :], Xs[j][:, :], cw[:, j, :], OP.mult)), reads=[Xs[j].res, cw.res], writes=[tmpc.res])
                B.op("dve", lambda e: e.tensor_tensor(acc[:, :], acc[:, :], tmpc[:, :], OP.add), reads=[acc.res, tmpc.res], writes=[acc.res])
            B.op("act", lambda e: e.activation(qkv[:, :], acc[:, :], AF.Silu), reads=[acc.res], writes=[qkv.res])
            B.op("pool", lambda e: e.tensor_tensor(sqb[:, :], qkv[:, 0:512], qkv[:, 0:512], OP.mult), reads=[qkv.res], writes=[sqb.res])
            B.op("dve", lambda e: e.tensor_reduce(smx[:, 28:36], sqb[:, :].rearrange("p (h d) -> p h d", h=8), AX.X, OP.add), reads=[sqb.res], writes=[smx.res])
            B.op("dve", lambda e: e.tensor_scalar(smx[:, 28:36], smx[:, 28:36], RMS_EPS, None, OP.add), reads=[smx.res], writes=[smx.res])
            B.op("act", lambda e: e.activation(smx[:, 28:36], smx[:, 28:36], AF.Ln), reads=[smx.res], writes=[smx.res])
            B.op("act", lambda e: e.activation(smx[:, 28:36], smx[:, 28:36], AF.Exp, scale=-0.5), reads=[smx.res], writes=[smx.res])
            B.op("dve", lambda e: e.tensor_scalar(smx[:, 28:32], smx[:, 28:32], 0.125, None, OP.mult), reads=[smx.res], writes=[smx.res])
            B.op("dve", lambda e: e.tensor_tensor(smx[:, 0:4], bab[:, 0:4], nAb[:, 4:8], OP.add), reads=[bab.res, nAb.res], writes=[smx.res])
            B.op("act", lambda e: e.activation(smx[:, 0:4], smx[:, 0:4], AF.Exp), reads=[smx.res], writes=[smx.res])
            B.op("act", lambda e: e.activation(smx[:, 4:8], bab[:, 4:8], AF.Exp, scale=-1.0), reads=[bab.res, smx.res], writes=[smx.res])
            B.op("dve", lambda e: e.tensor_scalar(smx[:, 0:8], smx[:, 0:8], 1.0, None, OP.add), reads=[smx.res], writes=[smx.res])
            B.op("act", lambda e: e.activation(smx[:, 0:4], smx[:, 0:4], AF.Ln), reads=[smx.res], writes=[smx.res])
            B.op("dve", lambda e: e.reciprocal(smx[:, 4:8], smx[:, 4:8]), reads=[smx.res], writes=[smx.res])
            B.op("dve", lambda e: e.tensor_tensor(smx[:, 0:4], smx[:, 0:4], nAb[:, 0:4], OP.mult), reads=[smx.res, nAb.res], writes=[smx.res])
            pg = _psr(B)
            B.mm(pg[:, 0:4], M[:, 0, :], smx[:, 0:4], True, True, reads=[M.res, smx.res], writes=[pg.res])
            B.mm(pg[:, 4:8], M[:, 4, :], smx[:, 0:4], True, True, reads=[M.res, smx.res], writes=[pg.res])
            B.op("dve", lambda e: e.tensor_copy(smx[:, 8:16], pg[:, 0:8]), reads=[pg.res, smx.res], writes=[smx.res])
            B.op("act", lambda e: e.activation(smx[:, 16:20], smx[:, 8:12], AF.Exp), reads=[smx.res], writes=[smx.res])
            B.op("dve", lambda e: e.tensor_tensor(smx[:, 20:24], smx[:, 12:16], smx[:, 8:12], OP.subtract), reads=[smx.res], writes=[smx.res])
            B.op("act", lambda e: e.activation(smx[:, 20:24], smx[:, 20:24], AF.Exp), reads=[smx.res], writes=[smx.res])
            B.op("act", lambda e: e.activation(smx[:, 24:28], smx[:, 12:16], AF.Exp), reads=[smx.res], writes=[smx.res])
            B.op("dve", lambda e: e.tensor_tensor(smx[:, 36:40], smx[:, 4:8], smx[:, 16:20], OP.mult), reads=[smx.res], writes=[smx.res])
            qv, kv_, vv = qkv[:, 0:256], qkv[:, 256:512], qkv[:, 512:768]
            B.op("dve", lambda e: e.tensor_tensor(v3(kn[:, :]), v3(kv_), bc3(smx[:, 32:36]), OP.mult), reads=[qkv.res, smx.res], writes=[kn.res])
            B.op("pool", lambda e: e.tensor_tensor(v3(qn[:, :]), v3(qv), bc3(smx[:, 28:32]), OP.mult), reads=[qkv.res, smx.res], writes=[qn.res])
            B.op("pool", lambda e: e.tensor_tensor(v3(vb[:, :]), v3(vv), bc3(smx[:, 4:8]), OP.mult), reads=[qkv.res, smx.res], writes=[vb.res])
            B.op("dve", lambda e: e.tensor_tensor(v3(kb[:, :]), v3(kn[:, :]), bc3(smx[:, 4:8]), OP.mult), reads=[kn.res, smx.res], writes=[kb.res])
            B.op("pool", lambda e: e.tensor_tensor(v3(qg[:, :]), v3(qn[:, :]), bc3(smx[:, 16:20]), OP.mult), reads=[qn.res, smx.res], writes=[qg.res])
            for h in range(4):
                cs = slice((h % 2) * 64, (h % 2) * 64 + 64)
                hs = slice(h * 64, (h + 1) * 64)
                B.op("dve", (lambda e, h=h, cs=cs, hs=hs: e.tensor_scalar(kbgz[:, h, cs], kn[:, hs], smx[:, 36 + h:37 + h], None, OP.mult)),
                     reads=[kn.res, smx.res], writes=[kbgz.res])
                B.op("pool", (lambda e, h=h, cs=cs, hs=hs: e.tensor_scalar(kdecz[:, h, cs], kn[:, hs], smx[:, 20 + h:21 + h], None, OP.mult)),
                     reads=[kn.res, smx.res], writes=[kdecz.res])
            for (src, dst) in ((kn, knT), (kb, kbT), (qn, qnT), (qg, qgT)):
                pt = _psr(B)
                for c in range(2):
                    B.tr(pt[:, c * 128:(c + 1) * 128], src[:, c * 128:(c + 1) * 128], ident[:], reads=[src.res, ident.res], writes=[pt.res])
                B.op("act", (lambda e, dst=dst, pt=pt: e.copy(dst[:, :, :].rearrange("p c t -> p (c t)"), pt[:, 0:256])),
                     reads=[pt.res], writes=[dst.res])
            for h in range(4):
                hb = H[h]
                rows = slice((h % 2) * 64, (h % 2) * 64 + 64)
                blk = h // 2
                B.op("pool", (lambda e, hb=hb, h=h: e.tensor_scalar(hb["diag"][:, :], ident[:, :], smx[:, 8 + h:9 + h], None, OP.mult)),
                     reads=[ident.res, smx.res], writes=[hb["diag"].res])
                pA = _psr(B)
                B.mm(pA[:, 0:128], M[:, 4, :], hb["diag"][:, :], True, True, reads=[M.res, hb["diag"].res], writes=[pA.res])
                B.op("dve", (lambda e, hb=hb, h=h, pA=pA: e.scalar_tensor_tensor(hb["arg"][:, :], pA[:, 0:128], smx[:, 8 + h:9 + h], M[:, 3, :], OP.subtract, OP.add)),
                     reads=[pA.res, smx.res, M.res], writes=[hb["arg"].res])
                B.op("act", (lambda e, hb=hb: e.activation(hb["Dst"][:, :], hb["arg"][:, :], AF.Exp)), reads=[hb["arg"].res], writes=[hb["Dst"].res])
                pB = _psr(B)
                B.mm(pB[:, 0:128], knT[rows, blk, :], kbT[rows, blk, :], True, True, reads=[knT.res, kbT.res], writes=[pB.res])
                B.op("dve", (lambda e, hb=hb, pB=pB: e.scalar_tensor_tensor(hb["Xa"][:, :], pB[:, 0:128], -1.0, hb["Dst"][:, :], OP.mult, OP.mult)),
                     reads=[pB.res, hb["Dst"].res], writes=[hb["Xa"].res])
                pC = _psr(B)
                B.mm(pC[:, 0:128], knT[rows, blk, :], qnT[rows, blk, :], True, True, reads=[knT.res, qnT.res], writes=[pC.res])
                B.op("pool", (lambda e, hb=hb: e.tensor_tensor(hb["arg"][:, :], hb["Dst"][:, :], ident[:, :], OP.add)),
                     reads=[hb["Dst"].res, ident.res, hb["arg"].res], writes=[hb["arg"].res])
                B.op("dve", (lambda e, hb=hb, pC=pC: e.tensor_tensor(hb["PT"][:, :], pC[:, 0:128], hb["arg"][:, :], OP.mult)),
                     reads=[pC.res, hb["arg"].res], writes=[hb["PT"].res])
                pD = _psr(B)
                B.tr(pD[:, 0:128], hb["Xa"][:, :], ident[:], reads=[hb["Xa"].res, ident.res], writes=[pD.res])
                B.op("act", (lambda e, hb=hb, pD=pD: e.copy(hb["Ya"][:, :], pD[:, 0:128])), reads=[pD.res], writes=[hb["Ya"].res])
                B.op("pool", (lambda e, hb=hb: e.tensor_tensor(hb["TT"][:, :], hb["Xa"][:, :], ident[:, :], OP.add)),
                     reads=[hb["Xa"].res, ident.res], writes=[hb["TT"].res])
            for k in range(6):
                for h in range(4):
                    hb = H[h]
                    X, Y = (hb["Xa"], hb["Ya"]) if k % 2 == 0 else (hb["Xb"], hb["Yb"])
                    Xn, Yn = (hb["Xb"], hb["Yb"]) if k % 2 == 0 else (hb["Xa"], hb["Ya"])
                    pY = _psr(B)
                    B.mm(pY[:, 0:128], X[:, :], Y[:, :], True, True, reads=[X.res, Y.res], writes=[pY.res])
                    if k < 5:
                        pX = _psr(B)
                        B.mm(pX[:, 0:128], Y[:, :], X[:, :], True, True, reads=[X.res, Y.res], writes=[pX.res])
                    B.op("act", (lambda e, Yn=Yn, pY=pY: e.copy(Yn[:, :], pY[:, 0:128])), reads=[pY.res], writes=[Yn.res])
                    if k < 5:
                        B.op("dve", (lambda e, Xn=Xn, pX=pX: e.tensor_copy(Xn[:, :], pX[:, 0:128])), reads=[pX.res], writes=[Xn.res])
                    pT = _psr(B)
                    B.mm(pT[:, 0:128], Yn[:, :], hb["TT"][:, :], True, True, reads=[Yn.res, hb["TT"].res], writes=[pT.res])
                    B.op("dve", (lambda e, hb=hb, pT=pT: e.tensor_tensor(hb["TT"][:, :], hb["TT"][:, :], pT[:, 0:128], OP.add)),
                         reads=[pT.res, hb["TT"].res], writes=[hb["TT"].res])
            for h in range(4):
                hb = H[h]
                hs = slice(h * 64, (h + 1) * 64)
                pU = _psr(B)
                B.mm(pU[:, 0:64], hb["TT"][:, :], vb[:, hs], True, True, reads=[hb["TT"].res, vb.res], writes=[pU.res])
                B.op("act", (lambda e, hb=hb, pU=pU: e.copy(hb["u"][:, :], pU[:, 0:64])), reads=[pU.res], writes=[hb["u"].res])
                pW = _psr(B)
                B.mm(pW[:, 0:128], kbgz[:, h, :], hb["TT"][:, :], True, True, reads=[kbgz.res, hb["TT"].res], writes=[pW.res])
                B.op("act", (lambda e, hb=hb, pW=pW: e.copy(hb["wT"][:, :], pW[:, 0:128])), reads=[pW.res], writes=[hb["wT"].res])
            po = B.psum[6 + (ti % 2)]
            for h in range(4):
                hb = H[h]
                rows = slice((h % 2) * 64, (h % 2) * 64 + 64)
                blk = h // 2
                hs = slice(h * 64, (h + 1) * 64)
                S0, S1 = Sin[blk], Sout[blk]
                pV = _psr(B)
                B.mm(pV[:, 0:64], hb["wT"][rows, :], S0[rows, :], True, True, reads=[hb["wT"].res, S0.res], writes=[pV.res])
                B.op("dve", (lambda e, hb=hb, pV=pV: e.tensor_tensor(hb["vnew"][:, :], hb["u"][:, :], pV[:, 0:64], OP.subtract)),
                     reads=[hb["u"].res, pV.res], writes=[hb["vnew"].res])
                B.mm(po[:, hs], qgT[rows, blk, :], S0[rows, :], True, False, reads=[qgT.res, S0.res], writes=[po.res])
                B.mm(po[:, hs], hb["PT"][:, :], hb["vnew"][:, :], False, True, reads=[hb["PT"].res, hb["vnew"].res], writes=[po.res])
                pS = _psr(B)
                B.mm(pS[:, 0:64], kdecz[:, h, :], hb["vnew"][:, :], True, True, reads=[kdecz.res, hb["vnew"].res], writes=[pS.res])
                B.op("dve", (lambda e, S0=S0, S1=S1, rows=rows, h=h, pS=pS: e.scalar_tensor_tensor(
                    S1[rows, :], S0[rows, :], smx[rows, 24 + h:25 + h], pS[rows, 0:64], OP.mult, OP.add)),
                    reads=[S0.res, smx.res, pS.res], writes=[S1.res])
            B.op("act", lambda e: e.copy(o_all[:, :], po[:, 0:256]), reads=[po.res], writes=[o_all.res])
            B.zres = zt.res
            _norm_gate_out(B, o_all, zt[:, :], nwb, 256, tok0)
        tile()
    for blk in range(2):
        Sf = Sp[blk][NPT % 2]
        B.dmaq(d["gdn_prompt"][l, blk * 128:(blk + 1) * 128, :], Sf[:, :], reads=[Sf.res], output=True)
    B.dmaq(d["conv_b_prompt"][l], d["uM"][TP - 3:TP, C_BQKV:C_BQKV + 768], reads=[B.uM_res], output=True)
    B.phase_end()


def _sample_bc(B, l):
    d = B.d
    TP = B.TP
    B.phase_begin()
    B.W256 = B.ring("W256", [128, 256], F32, 6)
    B.ybf = B.ring("ybf", [128, 256], BF16, 2)
    S = B.sb("S_s", [128, 4096])
    T1 = B.sb("T1_s", [128, 4096])
    T2 = B.sb("T2_s", [128, 4096])
    S3 = lambda t_: t_[:, :].rearrange("p (k v) -> p k v", k=64)
    S3T = lambda t_: t_[:, :].rearrange("p (k v) -> p v k", k=64)
    rows_bt = lambda ap: ap.rearrange("(b t) c -> b t c", t=4)
    bk = lambda col: col.unsqueeze(2).broadcast_to([128, 64, 64])
    bv = lambda col: col.unsqueeze(1).broadcast_to([128, 64, 64])
    b4 = lambda col: col.unsqueeze(2).broadcast_to([128, 4, 64])
    xp = B.sb("xp", [32, 7, 768])
    cw = B.sb("cw32", [32, 4, 768])
    acc = B.sb("acc_s", [32, 4, 768])
    tmpc = B.sb("tmpc_s", [32, 4, 768])
    B.dmaq(cw[:, :, :].rearrange("p j c -> p (j c)"), d["conv_b"][l].rearrange("j c -> (j c)").partition_broadcast(32), writes=[cw.res])
    B.dmaq(xp[:, 0:3, :], d["state_conv_b"][l], writes=[xp.res])
    B.dmaq(xp[:, 3:7, :], rows_bt(d["uM"][TP:TP + 128, C_BQKV:C_BQKV + 768]), writes=[xp.res], reads=[B.uM_res])
    B.dmaq(d["conv_b_sample"][l], xp[:, 4:7, :], reads=[xp.res], output=True)
    cwj = lambda j: cw[:, j, :].unsqueeze(1).broadcast_to([32, 4, 768])
    B.op("dve", lambda e: e.tensor_tensor(acc[:, :, :], xp[:, 0:4, :], cwj(0), OP.mult), reads=[xp.res, cw.res], writes=[acc.res])
    for j in range(1, 4):
        B.op("pool", (lambda e, j=j: e.tensor_tensor(tmpc[:, :, :], xp[:, j:j + 4, :], cwj(j), OP.mult)), reads=[xp.res, cw.res], writes=[tmpc.res])
        B.op("dve", lambda e: e.tensor_tensor(acc[:, :, :], acc[:, :, :], tmpc[:, :, :], OP.add), reads=[acc.res, tmpc.res], writes=[acc.res])
    B.op("act", lambda e: e.activation(acc[:, :, :], acc[:, :, :], AF.Silu), reads=[acc.res], writes=[acc.res])
    sqd_res = Res()
    B.dmaq(rows_bt(d["sqd"][:, :]), acc[:, :, :], reads=[acc.res], writes=[sqd_res])
    q, k, v = [B.sb(nm, [128, 4, 64]) for nm in ("q_s", "k_s", "v_s")]
    gab = B.sb("gab_s", [128, 4, 2])
    cst = B.sb("cst_s", [128, 4])
    for h in range(4):
        ps_ = slice(h * 32, (h + 1) * 32)
        for (dst, c0) in ((q, 0), (k, 256), (v, 512)):
            B.dmaq(dst[ps_, :, :], rows_bt(d["sqd"][:, c0 + h * 64:c0 + (h + 1) * 64]), reads=[sqd_res], writes=[dst.res])
        for i_, c0 in enumerate((C_BA, C_BB)):
            B.dmaq(gab[ps_, :, i_], rows_bt(d["uM"][TP:TP + 128, c0 + h:c0 + h + 1])[:, :, 0], reads=[B.uM_res], writes=[gab.res],
                   allow_slow_non_contiguous=True)
        B.dmaq(cst[ps_, 0:1], d["gdn_a_log"][l, h:h + 1].partition_broadcast(32), writes=[cst.res])
        B.dmaq(cst[ps_, 1:2], d["gdn_dt_bias"][l, h:h + 1].partition_broadcast(32), writes=[cst.res])
        B.dmaq(S[ps_, :], d["state_gdn"][l].rearrange("(b h) x -> h b x", h=4)[h], writes=[S.res])
    B.op("act", lambda e: e.activation(cst[:, 0:1], cst[:, 0:1], AF.Exp), reads=[cst.res], writes=[cst.res])
    B.op("dve", lambda e: e.tensor_scalar(cst[:, 0:1], cst[:, 0:1], -1.0, None, OP.mult), reads=[cst.res], writes=[cst.res])
    sm = B.sb("sm_s", [128, 32])
    sqt = B.sb("sqt_s", [128, 4, 64])
    for (src, c0) in ((q, 0), (k, 4)):
        B.op("pool", (lambda e, src=src: e.tensor_tensor(sqt[:, :, :], src[:, :, :], src[:, :, :], OP.mult)), reads=[src.res, sqt.res], writes=[sqt.res])
        B.op("dve", (lambda e, c0=c0: e.tensor_reduce(sm[:, c0:c0 + 4], sqt[:, :, :], AX.X, OP.add)), reads=[sqt.res, sm.res], writes=[sm.res])
    B.op("dve", lambda e: e.tensor_scalar(sm[:, 0:8], sm[:, 0:8], RMS_EPS, None, OP.add), reads=[sm.res], writes=[sm.res])
    B.op("act", lambda e: e.activation(sm[:, 0:8], sm[:, 0:8], AF.Ln), reads=[sm.res], writes=[sm.res])
    B.op("act", lambda e: e.activation(sm[:, 0:8], sm[:, 0:8], AF.Exp, scale=-0.5), reads=[sm.res], writes=[sm.res])
    B.op("dve", lambda e: e.tensor_scalar(sm[:, 0:4], sm[:, 0:4], 0.125, None, OP.mult), reads=[sm.res], writes=[sm.res])
    B.op("dve", lambda e: e.tensor_tensor(q[:, :, :], q[:, :, :], b4(sm[:, 0:4]), OP.mult), reads=[q.res, sm.res], writes=[q.res])
    B.op("dve", lambda e: e.tensor_tensor(k[:, :, :], k[:, :, :], b4(sm[:, 4:8]), OP.mult), reads=[k.res, sm.res], writes=[k.res])
    B.op("act", lambda e: e.activation(sm[:, 8:12], gab[:, :, 1], AF.Exp, scale=-1.0), reads=[gab.res, sm.res], writes=[sm.res])
    B.op("dve", lambda e: e.tensor_scalar(sm[:, 8:12], sm[:, 8:12], 1.0, None, OP.add), reads=[sm.res], writes=[sm.res])
    B.op("dve", lambda e: e.reciprocal(sm[:, 8:12], sm[:, 8:12]), reads=[sm.res], writes=[sm.res])
    B.op("dve", lambda e: e.tensor_scalar(sm[:, 12:16], gab[:, :, 0], cst[:, 1:2], None, OP.add), reads=[gab.res, cst.res, sm.res], writes=[sm.res])
    B.op("act", lambda e: e.activation(sm[:, 12:16], sm[:, 12:16], AF.Exp), reads=[sm.res], writes=[sm.res])
    B.op("dve", lambda e: e.tensor_scalar(sm[:, 12:16], sm[:, 12:16], 1.0, None, OP.add), reads=[sm.res], writes=[sm.res])
    B.op("act", lambda e: e.activation(sm[:, 12:16], sm[:, 12:16], AF.Ln), reads=[sm.res], writes=[sm.res])
    B.op("dve", lambda e: e.tensor_scalar(sm[:, 12:16], sm[:, 12:16], cst[:, 0:1], None, OP.mult), reads=[sm.res, cst.res], writes=[sm.res])
    B.op("act", lambda e: e.activation(sm[:, 16:20], sm[:, 12:16], AF.Exp), reads=[sm.res], writes=[sm.res])
    B.op("dve", lambda e: e.tensor_scalar(sm[:, 20:24], sm[:, 16:20], -1.0, None, OP.mult), reads=[sm.res], writes=[sm.res])
    o = B.sb("o_s", [128, 4, 64])
    kS = B.sb("kS_s", [128, 64])
    dlt = B.sb("dlt_s", [128, 64])
    for t in range(4):
        def step(t=t):
            B.op("dve", lambda e: e.tensor_tensor(S3(T1), S3(S), bk(k[:, t, :]), OP.mult), reads=[S.res, k.res], writes=[T1.res])
            B.op("dve", lambda e: e.tensor_reduce(kS[:, :], S3T(T1), AX.X, OP.add), reads=[T1.res], writes=[kS.res])
            B.op("dve", lambda e: e.scalar_tensor_tensor(dlt[:, :], kS[:, :], sm[:, 20 + t:21 + t], v[:, t, :], OP.mult, OP.add),
                 reads=[kS.res, sm.res, v.res], writes=[dlt.res])
            B.op("dve", lambda e: e.tensor_scalar(dlt[:, :], dlt[:, :], sm[:, 8 + t:9 + t], None, OP.mult), reads=[dlt.res, sm.res], writes=[dlt.res])
            B.op("pool", lambda e: e.tensor_tensor(S3(T2), bk(k[:, t, :]), bv(dlt[:, :]), OP.mult), reads=[k.res, dlt.res], writes=[T2.res])
            B.op("dve", lambda e: e.scalar_tensor_tensor(S[:, :], S[:, :], sm[:, 16 + t:17 + t], T2[:, :], OP.mult, OP.add),
                 reads=[S.res, sm.res, T2.res], writes=[S.res])
            B.op("pool", lambda e: e.tensor_tensor(S3(T1), S3(S), bk(q[:, t, :]), OP.mult), reads=[S.res, q.res], writes=[T1.res])
            B.op("dve", lambda e: e.tensor_reduce(o[:, t, :], S3T(T1), AX.X, OP.add), reads=[T1.res], writes=[o.res])
        step()
    sod_res = Res()
    for h in range(4):
        ps_ = slice(h * 32, (h + 1) * 32)
        B.dmaq(rows_bt(d["sod"][0][:, h * 64:(h + 1) * 64]), o[ps_, :, :], reads=[o.res], writes=[sod_res])
        B.dmaq(d["gdn_sample"][l].rearrange("(b h) x -> h b x", h=4)[h], S[ps_, :], reads=[S.res], output=True)
    nwb = B.sb("nwb_s", [128, 256])
    nwc = B.sb("nwc_s", [128, 256])
    for h in range(4):
        B.dmaq(nwb[:, h * 64:(h + 1) * 64], d["norm_b"][l].partition_broadcast(128), writes=[nwb.res])
        B.dmaq(nwc[:, h * 64:(h + 1) * 64], d["norm_c"][l].partition_broadcast(128), writes=[nwc.res])
    otm = B.sb("otm_s", [128, 256])
    zt = B.sb("zt_s", [128, 256])
    B.dmaq(otm[:, :], d["sod"][0], reads=[sod_res], writes=[otm.res])
    B.dmaq(zt[:, :], d["uM"][TP:TP + 128, C_BZ:C_BZ + 256], reads=[B.uM_res], writes=[zt.res])
    B.zres = zt.res
    _norm_gate_out(B, otm, zt[:, :], nwb, 256, TP)
    cq, cf, ci = q, k, v
    lbt = B.sb("lbt_s", [128, 3, 64])
    for h in range(4):
        ps_ = slice(h * 32, (h + 1) * 32)
        B.dmaq(cq[ps_, :, :], rows_bt(d["uMs"][:, h * 64:(h + 1) * 64]), reads=[B.uM_res, o.res], writes=[cq.res])
        B.dmaq(cf[ps_, :, :], rows_bt(d["uMs"][:, 256 + h * 64:256 + (h + 1) * 64]), reads=[B.uM_res, o.res], writes=[cf.res])
        B.dmaq(ci[ps_, :, :], rows_bt(d["uM"][TP:TP + 128, C_CI + h * 64:C_CI + (h + 1) * 64]), reads=[B.uM_res, o.res], writes=[ci.res])
        B.dmaq(S[ps_, :], d["state_hgrn"][l].rearrange("(b h) x -> h b x", h=4)[h], writes=[S.res])
        if l > 0:
            B.dmaq(lbt[ps_, 0, :], d["lower_bounds"][1, h * 64:(h + 1) * 64].partition_broadcast(32), writes=[lbt.res])
            B.dmaq(lbt[ps_, 2, :], d["lower_bounds"][0, h * 64:(h + 1) * 64].partition_broadcast(32), writes=[lbt.res])
    if l == 0:
        B.op("pool", lambda e: e.memset(lbt[:, 0, :], 0.0), writes=[lbt.res])
    else:
        B.op("dve", lambda e: e.tensor_tensor(lbt[:, 0, :], lbt[:, 0, :], lbt[:, 2, :], OP.subtract), reads=[lbt.res], writes=[lbt.res])
        B.op("act", lambda e: e.activation(lbt[:, 0, :], lbt[:, 0, :], AF.Sigmoid), reads=[lbt.res], writes=[lbt.res])
    B.op("dve", lambda e: e.tensor_scalar(lbt[:, 1, :], lbt[:, 0, :], -1.0, 1.0, OP.mult, OP.add), reads=[lbt.res], writes=[lbt.res])
    bt = lambda ap: ap.unsqueeze(1).broadcast_to([128, 4, 64])
    f_, kc = cf, sqt
    B.op("act", lambda e: e.activation(cf[:, :, :], cf[:, :, :], AF.Sigmoid), reads=[cf.res], writes=[cf.res])
    B.op("dve", lambda e: e.tensor_scalar(kc[:, :, :], cf[:, :, :], -1.0, 1.0, OP.mult, OP.add), reads=[cf.res, sqt.res], writes=[kc.res])
    B.op("dve", lambda e: e.tensor_tensor(kc[:, :, :], kc[:, :, :], bt(lbt[:, 1, :]), OP.mult), reads=[kc.res, lbt.res], writes=[kc.res])
    B.op("dve", lambda e: e.tensor_tensor(f_[:, :, :], cf[:, :, :], bt(lbt[:, 1, :]), OP.mult), reads=[cf.res, lbt.res], writes=[f_.res])
    B.op("dve", lambda e: e.tensor_tensor(f_[:, :, :], f_[:, :, :], bt(lbt[:, 0, :]), OP.add), reads=[f_.res, lbt.res], writes=[f_.res])
    B.op("act", lambda e: e.activation(cq[:, :, :], cq[:, :, :], AF.Silu), reads=[cq.res], writes=[cq.res])
    o2 = B.sb("o2_s", [128, 4, 64])
    for t in range(4):
        def step2(t=t):
            B.op("dve", lambda e: e.tensor_tensor(S3(S), S3(S), bk(f_[:, t, :]), OP.mult), reads=[S.res, f_.res], writes=[S.res])
            B.op("pool", lambda e: e.tensor_tensor(S3(T2), bk(kc[:, t, :]), bv(ci[:, t, :]), OP.mult), reads=[kc.res, ci.res], writes=[T2.res])
            B.op("dve", lambda e: e.tensor_tensor(S[:, :], S[:, :], T2[:, :], OP.add), reads=[S.res, T2.res], writes=[S.res])
            B.op("pool", lambda e: e.tensor_tensor(S3(T1), S3(S), bk(cq[:, t, :]), OP.mult), reads=[S.res, cq.res], writes=[T1.res])
            B.op("dve", lambda e: e.tensor_reduce(o2[:, t, :], S3T(T1), AX.X, OP.add), reads=[T1.res], writes=[o2.res])
        step2()
    sod2_res = Res()
    for h in range(4):
        ps_ = slice(h * 32, (h + 1) * 32)
        B.dmaq(rows_bt(d["sod"][1][:, h * 64:(h + 1) * 64]), o2[ps_, :, :], reads=[o2.res], writes=[sod2_res])
        B.dmaq(d["hgrn_sample"][l].rearrange("(b h) x -> h b x", h=4)[h], S[ps_, :], reads=[S.res], output=True)
    otm2 = B.sb("otm2_s", [128, 256])
    zt2 = B.sb("zt2_s", [128, 256])
    B.dmaq(otm2[:, :], d["sod"][1], reads=[sod2_res], writes=[otm2.res])
    B.dmaq(zt2[:, :], d["uM"][TP:TP + 128, C_CG:C_CG + 256], reads=[B.uM_res], writes=[zt2.res])
    B.zres = zt2.res
    _norm_gate_out(B, otm2, zt2[:, :], nwc, 512, TP)
    B.phase_end()


def _sample_attn_setup(B):
    d = B.d
    NLP, NPS = B.NLP, B.NPS
    B.ownT = B.sb("ownT", [128, 4 * NPS], glob=True)
    B.gidx = B.sb("gidx", [128, 4 * NPS], I32, glob=True)
    B.lastidx = B.sb("lastidx", [128, 32], I32, glob=True)
    B.locr = B.sb("locr", [128, 32], glob=True)
    B.NB = B.sb("NBn", [128, 4, 128], glob=True)
    B.BLg = B.sb("BLg", [128, 8, 4], glob=True)
    B.ec = B.sb("ec", [128, 4], glob=True)
    B.islast = B.sb("islast", [16, 4 * NPS], glob=True)
    B.BLT = B.sb("BLT", [16, 128], glob=True)
    B.Ind = B.sb("Ind", [16, 512], BF16, glob=True)
    B.phase_begin()
    rb = B.rbb
    misc = B.sb("misc", [128, 8])
    B.dmaq(misc[:, :], d["c_misc"][:, :], writes=[misc.res])
    import os as _os
    parts = _os.environ.get('SETUP_PARTS', '123')
    if '1' in parts:
      _setup_own(B, d, NLP, NPS)
    if '3' in parts:
      _setup_bias(B, d, rb)
    B.phase_end()


def _setup_own(B, d, NLP, NPS):
    NL = 4 * NPS
    B.dmaq(B.ownT[:, :], d["c_ownL"][:, :], writes=[B.ownT.res])
    B.dmaq(B.islast[:, :], d["c_isl"][:, :], writes=[B.islast.res])
    misc = B.sb("misc2", [128, 8])
    B.dmaq(misc[:, :], d["c_misc"][:, :], writes=[misc.res])
    pi = B.sb("pi", [128, NL], I32)
    pf = B.sb("pf", [128, NL])
    B.dmaq(pi[:, :], d["pt_own"].t.partition_broadcast(128), writes=[pi.res])
    B.op("dve", lambda e: e.tensor_copy(pf[:, :], pi[:, :]), reads=[pi.res], writes=[pf.res])
    B.op("dve", lambda e: e.tensor_scalar(pf[:, :], pf[:, :], 128.0, misc[:, 1:2], OP.mult, OP.add), reads=[pf.res, misc.res], writes=[pf.res])
    B.op("dve", lambda e: e.tensor_copy(B.gidx[:, :], pf[:, :]), reads=[pf.res], writes=[B.gidx.res])


def _setup_last(B, d, NLP, NPS, misc):
    li = B.sb("li", [128, 32 * NPS], I32)
    lf = B.sb("lf", [128, 32])
    t1 = B.sb("t1", [128, 32])
    B.dmaq(li[:, :], d["page_table"].t.rearrange("b j -> (b j)").partition_broadcast(128), writes=[li.res])
    B.op("dve", lambda e: e.tensor_copy(lf[:, :], li[:, :].rearrange("p (b j) -> p b j", j=NPS)[:, :, NPS - 1]), reads=[li.res], writes=[lf.res])
    B.op("dve", lambda e: e.tensor_scalar(lf[:, :], lf[:, :], misc[:, 0:1], None, OP.subtract), reads=[lf.res, misc.res], writes=[lf.res])
    B.op("dve", lambda e: e.tensor_scalar(B.locr[:, :], lf[:, :], 0.0, None, OP.is_ge), reads=[lf.res], writes=[B.locr.res])
    B.op("dve", lambda e: e.tensor_scalar(t1[:, :], lf[:, :], float(NLP - 1), None, OP.is_le), reads=[lf.res], writes=[t1.res])
    B.op("dve", lambda e: e.tensor_tensor(B.locr[:, :], B.locr[:, :], t1[:, :], OP.mult), reads=[B.locr.res, t1.res], writes=[B.locr.res])
    BIG = float(NLP * 128 + 4096)
    B.op("dve", lambda e: e.tensor_scalar(lf[:, :], lf[:, :], 128.0, misc[:, 1:2], OP.mult, OP.add), reads=[lf.res, misc.res], writes=[lf.res])
    B.op("dve", lambda e: e.tensor_scalar(lf[:, :], lf[:, :], -BIG, None, OP.add), reads=[lf.res], writes=[lf.res])
    B.op("dve", lambda e: e.tensor_tensor(lf[:, :], lf[:, :], B.locr[:, :], OP.mult), reads=[lf.res, B.locr.res], writes=[lf.res])
    B.op("dve", lambda e: e.tensor_scalar(lf[:, :], lf[:, :], BIG, None, OP.add), reads=[lf.res], writes=[lf.res])
    B.op("dve", lambda e: e.tensor_copy(B.lastidx[:, :], lf[:, :]), reads=[lf.res], writes=[B.lastidx.res])


def _setup_bias(B, d, rb):
    cn = B.sb("cn", [128, 5, 128])
    B.dmaq(cn[:, :, :], d["c_new"][:, :, :], writes=[cn.res])
    for h in range(4):
        B.op("dve", (lambda e, h=h: e.tensor_scalar(B.NB[:, h, :], cn[:, 4, :], -30000.0, None, OP.mult)), reads=[cn.res], writes=[B.NB.res])
        for dist in range(4):
            B.op("dve", (lambda e, h=h, dist=dist: e.scalar_tensor_tensor(B.NB[:, h, :], cn[:, dist, :], rb[:, dist * 4 + h:dist * 4 + h + 1],
                                                                         B.NB[:, h, :], OP.mult, OP.add)),
                 reads=[cn.res, rb.res, B.NB.res], writes=[B.NB.res])
    lb_ = B.sb("lastb", [128, 4, 32])
    B.dmaq(lb_[:, :, :], d["c_lastb"][:, :, :], writes=[lb_.res])
    tmp = B.sb("tmpl", [128, 4, 32])
    bl4 = B.sb("bl4", [128, 4, 4])
    for h in range(4):
        B.op("dve", (lambda e, h=h: e.tensor_tensor(tmp[:, :, :], lb_[:, :, :],
                                                    rb[:, :].rearrange("p (b h) -> p h b", h=4)[:, h, :].unsqueeze(1).broadcast_to([128, 4, 32]), OP.mult)),
             reads=[lb_.res, rb.res, tmp.res], writes=[tmp.res])
        B.op("dve", (lambda e, h=h: e.tensor_reduce(bl4[:, h, :], tmp[:, :, :], AX.X, OP.add)), reads=[tmp.res, bl4.res], writes=[bl4.res])
        B.op("dve", (lambda e, h=h: e.tensor_scalar(bl4[:, h, :], bl4[:, h, :], rb[:, 31 * 4 + h:31 * 4 + h + 1], None, OP.subtract)),
             reads=[bl4.res, rb.res], writes=[bl4.res])
        for m in range(2):
            B.op("dve", (lambda e, h=h, m=m: e.tensor_copy(B.BLg[:, 2 * h + m, :], bl4[:, h, :])), reads=[bl4.res, B.BLg.res], writes=[B.BLg.res])
    B.op("act", lambda e: e.activation(B.ec[:, :], rb[:, 124:128], AF.Exp), reads=[rb.res], writes=[B.ec.res])
    pt = B.psum[3]
    B.tr(pt[0:16, 0:128], bl4[:, :, :].rearrange("p h t -> p (h t)"), B.ident[:], reads=[bl4.res, B.ident.res], writes=[pt.res])
    B.op("act", lambda e: e.activation(B.BLT[:, :], pt[0:16, 0:128], AF.Copy, scale=float(32 ** 0.5)), reads=[pt.res], writes=[B.BLT.res])
    indf = B.sb("indf", [16, 512])
    B.dmaq(indf[:, :], d["c_ind"][:, :], writes=[indf.res])
    B.op("pool", lambda e: e.tensor_copy(B.Ind[:, :], indf[:, :]), reads=[indf.res], writes=[B.Ind.res])


def _sample_attn(B, l, lam_init):
    d = B.d
    TP, NLP = B.TP, B.NLP
    SC = 32 ** -0.5
    B.phase_begin()
    B.ev_ring = B.ring("ev", [128, 512], F32, 4)
    B.ybf = B.ring("ybf", [128, 256], BF16, 2)
    grp = lambda g: (slice(0, 32), g)
    Em = lambda E_, m: E_[:, :, :].rearrange("p (h m) q -> p m h q", m=2)[:, m]
    stg = B.sb("stg", [32, 8, 128])
    qTs = B.sb("qTs", [32, 8, 128], BF16)
    kTn = B.sb("kTn", [32, 8, 128], BF16)
    for (dst, r0) in ((qTs, R_DQ), (kTn, R_DK)):
        for g in range(8):
            rs, blk = grp(g)
            B.dmaq(stg[rs, blk, :], d["uT"][r0 + g * 32:r0 + (g + 1) * 32, TP:TP + 128], reads=[B.uT_res], writes=[stg.res])
        B.op("pool", (lambda e, dst=dst: e.tensor_copy(dst[:, :, :], stg[:, :, :])), reads=[stg.res], writes=[dst.res])
    vst = B.sb("vst", [128, 256])
    Vn = B.sb("Vn", [128, 4, 65], BF16)
    B.dmaq(vst[:, :], d["uM"][TP:TP + 128, C_DV:C_DV + 256], reads=[B.uM_res], writes=[vst.res])
    B.op("pool", lambda e: e.tensor_copy(Vn[:, :, 0:64], vst[:, :].rearrange("p (h d) -> p h d", h=4)), reads=[vst.res], writes=[Vn.res])
    B.op("pool", lambda e: e.memset(Vn[:, :, 64:65], 1.0), writes=[Vn.res])
    acc = B.sb("acc", [128, 8, 65])
    B.op("pool", lambda e: e.memset(acc[:, :, :], 0.0), writes=[acc.res])
    import os as _os
    _stop = int(_os.environ.get('SA_STOP', '9'))
    if _stop <= 1:
        B.phase_end()
        return
    kps = [B.sb("kp%d" % i, [128, 256]) for i in range(3)]
    vps = [B.sb("vp%d" % i, [128, 256]) for i in range(3)]
    kTs = [B.sb("kT%d" % i, [32, 8, 128], BF16) for i in range(2)]
    Vas = [B.sb("Va%d" % i, [128, 4, 65], BF16) for i in range(2)]
    Es = [B.sb("Ef%d" % i, [128, 8, 128], BF16) for i in range(2)]
    for Va in Vas:
        B.op("pool", (lambda e, Va=Va: e.memset(Va[:, :, 64:65], 1.0)), writes=[Va.res])
    Fps = [B.sb("Fp%d" % i, [16, 128], BF16) for i in range(2)]

    def kT_from(kp, kT, pt):
        for c in range(8):
            pp = pt[c // 4]
            B.tr(pp[0:32, (c % 4) * 128:(c % 4 + 1) * 128], kp[:, c * 32:(c + 1) * 32], B.ident[:], reads=[kp.res, B.ident.res], writes=[pp.res])
        for c2 in range(2):
            pp = pt[c2]
            B.op("act", (lambda e, pp=pp, c2=c2: e.copy(kT[:, c2 * 4:(c2 + 1) * 4, :], pp[0:32, 0:512].rearrange("p (c t) -> p c t", c=4))),
                 reads=[pp.res], writes=[kT.res])

    import os as _os
    bc_reg = [None]
    for p in range(4 * B.NPS):
        def page(p=p):
            kp, vp = kps[p % 3], vps[p % 3]
            for (dst, src) in ((kp, d["cache_k%d" % l]), (vp, d["cache_v%d" % l])):
                def gather(e, dst=dst, src=src):
                    if bc_reg[0] is None:
                        bc_reg[0] = e.to_reg(NLP * 128 - 1)
                    return e.indirect_dma_start(
                        out=dst[:, :], out_offset=None, in_=src[:, :],
                        in_offset=bass.IndirectOffsetOnAxis(ap=B.gidx[:, p:p + 1], axis=0),
                        bounds_check=bc_reg[0], oob_is_err=False)
                B.S.op("pool", gather, reads=[B.gidx.res], writes=[dst.res], dma=True)
            kT, Va, E = kTs[p % 2], Vas[p % 2], Es[p % 2]
            kT_from(kp, kT, (B.psum[6], B.psum[7]))
            B.op("pool", lambda e: e.tensor_copy(Va[:, :, 0:64], vp[:, :].rearrange("p (h d) -> p h d", h=4)), reads=[vp.res], writes=[Va.res])
            sM = (B.psum[2 * (p % 2)], B.psum[2 * (p % 2) + 1])
            Fp = Fps[p % 2]
            B.op("dve", lambda e: e.tensor_scalar(Fp[:, :], B.BLT[:, :], B.islast[:, p:p + 1], None, OP.mult),
                 reads=[B.BLT.res, B.islast.res], writes=[Fp.res])
            for m in range(2):
                B.mm(sM[m][:, :], Fp[:, :], B.Ind[:, :], True, False, reads=[Fp.res, B.Ind.res], writes=[sM[m].res])
            for g in range(8):
                rs, blk = grp(g)
                sp = sM[g % 2]
                B.mm(sp[:, (g // 2) * 128:(g // 2 + 1) * 128], kT[rs, blk, :], qTs[rs, blk, :], False, g >= 6,
                     reads=[kT.res, qTs.res], writes=[sp.res])
            for m in range(2):
                B.op("act", (lambda e, m=m: e.activation(Em(E, m), sM[m][:, :].rearrange("p (h q) -> p h q", h=4), AF.Exp, scale=SC)),
                     reads=[sM[m].res], writes=[E.res])
            oA, oB = B.psum[4], B.psum[5]
            for g in range(8):
                oo = oA if g < 4 else oB
                B.mm(oo[:, (g % 4) * 65:(g % 4 + 1) * 65], E[:, g, :], Va[:, g // 2, :], True, True, reads=[E.res, Va.res], writes=[oo.res])
            for (oo, gs) in ((oA, slice(0, 4)), (oB, slice(4, 8))):
                B.op("dve", (lambda e, oo=oo, gs=gs: e.scalar_tensor_tensor(
                    acc[:, gs, :], oo[:, 0:260].rearrange("p (g c) -> p g c", g=4), B.ownT[:, p:p + 1], acc[:, gs, :], OP.mult, OP.add)),
                    reads=[oo.res, B.ownT.res, acc.res], writes=[acc.res])
        page()
    tot = B.sb("tot", [128, 8, 65])
    if B.n_cores > 1:
        ag_res = Res()
        B.dmaq(d["agin"][:, :], acc[:, :, :].rearrange("p g c -> p (g c)"), reads=[acc.res], writes=[ag_res])
        B.S.op("pool", lambda e: e.collective_compute("AllGather", op=OP.bypass, replica_groups=[list(range(B.n_cores))],
                                                     ins=[d["agin"][:, :]], outs=[d["agout"][:, :]]),
               reads=[ag_res], writes=[ag_res], dma=True)
        gat = B.sb("gat", [128, B.n_cores, 520])
        B.dmaq(gat[:, :, :], d["agout"].t.rearrange("(r p) c -> p r c", p=128), reads=[ag_res], writes=[gat.res])
        totf = tot[:, :, :].rearrange("p g c -> p (g c)")
        B.op("dve", lambda e: e.tensor_tensor(totf, gat[:, 0, :], gat[:, 1, :], OP.add), reads=[gat.res], writes=[tot.res])
        for r in range(2, B.n_cores):
            B.op("dve", (lambda e, r=r: e.tensor_tensor(totf, totf, gat[:, r, :], OP.add)), reads=[gat.res, tot.res], writes=[tot.res])
    else:
        B.op("dve", lambda e: e.tensor_copy(tot[:, :, :], acc[:, :, :]), reads=[acc.res], writes=[tot.res])
    if _stop <= 2:
        B.phase_end()
        return
    En = Es[0]
    nA, nB = B.psum[0], B.psum[1]
    for g in range(8):
        rs, blk = grp(g)
        sp = (nA, nB)[g % 2]
        B.mm(sp[:, (g // 2) * 128:(g // 2 + 1) * 128], kTn[rs, blk, :], qTs[rs, blk, :], True, True, reads=[kTn.res, qTs.res], writes=[sp.res])
    if _os.environ.get('SA_NOEXP'):
        for sp in (nA, nB):
            tmp = B.ev_ring.next()
            B.op("dve", (lambda e, sp=sp, tmp=tmp: e.tensor_copy(tmp[:, :], sp[:, :])), reads=[sp.res], writes=[tmp.res])
            B.dmaq(d["facc"][0:128, 0:512], tmp[:, :], reads=[tmp.res])
        B.phase_end()
        return
    for g in range(8):
        sp = (nA, nB)[g % 2]
        tmp = B.ev_ring.next()
        B.op("dve", (lambda e, g=g, sp=sp, tmp=tmp: e.scalar_tensor_tensor(tmp[:, 0:128], sp[:, (g // 2) * 128:(g // 2 + 1) * 128], SC,
                                                                          B.NB[:, g // 2, :], OP.mult, OP.add)),
             reads=[sp.res, B.NB.res], writes=[tmp.res])
        B.op("act", (lambda e, g=g, tmp=tmp: e.activation(En[:, g, :], tmp[:, 0:128], AF.Exp)), reads=[tmp.res, En.res], writes=[En.res])
    if _stop <= 3:
        B.phase_end()
        return
    oA, oB = B.psum[2], B.psum[3]
    for g in range(8):
        oo = oA if g < 4 else oB
        B.mm(oo[:, (g % 4) * 65:(g % 4 + 1) * 65], En[:, g, :], Vn[:, g // 2, :], True, True, reads=[En.res, Vn.res], writes=[oo.res])
    for g in range(8):
        oo = oA if g < 4 else oB
        B.op("dve", (lambda e, g=g, oo=oo: e.scalar_tensor_tensor(tot[:, g, :], tot[:, g, :], B.ec[:, g // 2:g // 2 + 1],
                                                                 oo[:, (g % 4) * 65:(g % 4 + 1) * 65], OP.mult, OP.add)),
             reads=[tot.res, B.ec.res, oo.res], writes=[tot.res])
    if B.debug and l == 0:
        for nm, t_ in (("dbgp_tot", tot), ("dbgp_acc", acc)):
            dd = B.dram(nm, [128, 520])
            B.dmaq(dd[:, :], t_[:, :, :].rearrange("p g c -> p (g c)"), reads=[t_.res], output=True)
        dd = B.dram("dbgp_own", [128, NLP])
        B.dmaq(dd[:, :], B.ownT[:, :], reads=[B.ownT.res], output=True)
        dd = B.dram("dbgp_isl", [16, NLP])
        B.dmaq(dd[:, :], B.islast[:, :], reads=[B.islast.res], output=True)
    if _stop <= 4:
        B.phase_end()
        return
    for h in range(4):
        _diff_combine(B, l, lam_init, tot[:, 2 * h, :], tot[:, 2 * h + 1, :], 128, [tot.res], 768 + h * 64, TP, h)
    B.phase_end()
```

```python
import math
from contextlib import ExitStack
import numpy as np
import concourse.bass as bass
import concourse.mybir as mybir
from concourse.bass_utils import run_bass_kernel_spmd

F32 = mybir.dt.float32
BF16 = mybir.dt.bfloat16
I32 = mybir.dt.int32
AF = mybir.ActivationFunctionType
OP = mybir.AluOpType
AX = mybir.AxisListType

D_MODEL = 1024
GW = 256
D_IN = 3592
D_FF = 2816
D_FFE = 1408
N_EXP = 8
ALPHA = 4 ** 0.25
LN_EPS = 1e-5
RMS_EPS = 1e-6
O_AIN, O_AGB, O_AGC, O_BQKV, O_BA, O_BB, O_BZ = 0, 256, 512, 768, 1536, 1540, 1544
O_CQ, O_CF, O_CI, O_CG, O_DQ, O_DK, O_DV = 1800, 2056, 2312, 2568, 2824, 3080, 3336
FM_COLS = [(O_AIN, 768), (O_CQ, 512), (O_DQ, 512)]
FM_N = 1792
R_AIN, R_AGB, R_AGC, R_CQ, R_CF, R_DQ, R_DK = 0, 256, 512, 768, 1024, 1280, 1536
TM_COLS = [(O_BQKV, 1032), (O_CI, 512), (O_DK, 512)]
TM_N = 2056
C_BQKV, C_BA, C_BB, C_BZ, C_CI, C_CG, C_DK, C_DV = 0, 768, 772, 776, 1032, 1288, 1544, 1800


class Res:
    __slots__ = ("w", "r")

    def __init__(self):
        self.w = None
        self.r = []


class Sched:
    NDMA = 24

    def __init__(self, nc, es):
        self.nc = nc
        self.ops = []
        self.out_dmas = []
        self.engs = {"pe": nc.tensor, "act": nc.scalar, "pool": nc.gpsimd, "dve": nc.vector, "sp": nc.sync}
        self.esem = {e: es.enter_context(nc.semaphore("s_" + e)) for e in self.engs}
        self.dsem = [es.enter_context(nc.semaphore("d%d" % i)) for i in range(self.NDMA)]
        self.tok = []
        self.ecount = {e: 0 for e in self.engs}
        self.dval = [0] * self.NDMA
        self.clock = {e: {} for e in self.engs}
        self.dcount = 0
        self.last_on_sem = [None] * self.NDMA
        self.flushed = 0
        self.n_instr = {e: 0 for e in self.engs}

    def op(self, eng, fn, reads=(), writes=(), dma=False):
        idx = len(self.ops)
        deps = set()
        for r in reads:
            if r.w is not None:
                deps.add(r.w)
        for w in writes:
            if w.w is not None:
                deps.add(w.w)
            deps.update(w.r)
        for r in reads:
            r.r.append(idx)
        for w in writes:
            w.w = idx
            w.r = []
        self.ops.append([eng, fn, deps, dma])
        return idx

    def dma(self, eng, out, in_, reads=(), writes=(), output=False, **kw):
        idx = self.op(eng, lambda e: e.dma_start(out=out, in_=in_, **kw), reads, writes, dma=True)
        if output:
            self.out_dmas.append(idx)
        return idx

    def flush(self):
        nc = self.nc
        ops = self.ops
        start = self.flushed
        ops.append(["sp", None, set(i for i in range(start, len(ops)) if ops[i][3]), False])
        n = len(ops)
        self.tok.extend([None] * (n - len(self.tok)))
        tok = self.tok
        needed = {}
        dma_slot = {}
        for i in range(start, n):
            o = ops[i]
            if o[3]:
                k = self.dcount % self.NDMA
                self.dcount += 1
                if self.last_on_sem[k] is not None:
                    o[2].add(self.last_on_sem[k])
                self.last_on_sem[k] = i
                dma_slot[i] = k
        for i in range(start, n):
            o = ops[i]
            for d in o[2]:
                if d < start:
                    continue
                od = ops[d]
                if od[3] or o[3] or od[0] != o[0] or o[0] != "pe":
                    needed[d] = True
        streams = {e: [] for e in self.engs}
        for i in range(start, n):
            eng, fn, deps, is_dma = ops[i]
            waits = {}
            clk = self.clock[eng]
            for d in deps:
                if d < start:
                    continue
                od = ops[d]
                if not (od[3] or is_dma or od[0] != eng or eng != "pe"):
                    continue
                sem, val = tok[d]
                key = id(sem)
                if clk.get(key, 0) >= val:
                    continue
                if key not in waits or waits[key][1] < val:
                    waits[key] = (sem, val)
            for key, (sem, val) in waits.items():
                clk[key] = val
            wl = list(waits.values())
            if is_dma:
                k = dma_slot[i]
                self.dval[k] += 16
                tok[i] = (self.dsem[k], self.dval[k])
                inc = (self.dsem[k], 16)
            elif needed.get(i, False):
                self.ecount[eng] += 1
                tok[i] = (self.esem[eng], self.ecount[eng])
                inc = (self.esem[eng], 1)
            else:
                inc = None
            streams[eng].append((wl, fn, inc))
        for e, v in streams.items():
            self.n_instr[e] += len(v)
        self.flushed = n

        def run(e, lst):
            for wl, fn, inc in lst:
                for sem, val in wl:
                    e.wait_ge(sem, val)
                if fn is None:
                    continue
                ins = fn(e)
                if inc is not None:
                    ins.then_inc(inc[0], inc[1])

        with nc.Block() as block:
            @block.sync
            def _(e):
                run(e, streams["sp"])

            @block.scalar
            def _(e):
                run(e, streams["act"])

            @block.vector
            def _(e):
                run(e, streams["dve"])

            @block.gpsimd
            def _(e):
                run(e, streams["pool"])

            @block.tensor
            def _(e):
                run(e, streams["pe"])


class Buf:
    def __init__(self, t):
        self.t = t
        self.res = Res()

    def __getitem__(self, k):
        return self.t[k]


class View:
    def __init__(self, ap, res):
        self.t = ap
        self.res = res

    def __getitem__(self, k):
        return self.t[k]


class Ring:
    def __init__(self, bufs):
        self.bufs = bufs
        self.i = 0

    def next(self):
        b = self.bufs[self.i % len(self.bufs)]
        self.i += 1
        return b


class Builder:
    def __init__(self, NPT, n_cores, n_local_pages, n_pages_seq):
        self.NPT = NPT
        self.NT = (NPT + 1) * 128
        self.TP = NPT * 128
        self.n_cores = n_cores
        self.NLP = n_local_pages
        self.NPS = n_pages_seq
        self.nc = bass.Bass("TRN2", target_bir_lowering=False)
        self.es = ExitStack()
        self.S = Sched(self.nc, self.es)
        self.sb_bytes = 0
        self.ph = None
        self.ph_bytes = 0
        self.ph_max = 0
        self.uid = 0

    def sb(self, name, shape, dt=F32, glob=False):
        stack = self.es if (glob or self.ph is None) else self.ph
        self.uid += 1
        t = stack.enter_context(self.nc.sbuf_tensor("%s_%d" % (name, self.uid), list(shape), dt))
        nb = int(np.prod(shape[1:])) * (2 if dt == BF16 else 4)
        if stack is self.es:
            self.sb_bytes += nb
        else:
            self.ph_bytes += nb
            self.ph_max = max(self.ph_max, self.ph_bytes)
        return Buf(t)

    def ring(self, name, shape, dt=F32, n=2):
        return Ring([self.sb("%s%d" % (name, i), shape, dt) for i in range(n)])

    def phase_begin(self):
        assert self.ph is None
        self.ph = ExitStack()
        self.ph_bytes = 0

    def phase_end(self):
        self.S.flush()
        self.ph.close()
        self.ph = None

    def ps(self, name, shape, dt=F32):
        t = self.es.enter_context(self.nc.psum_tensor(name, list(shape), dt))
        return Buf(t)

    def dram(self, name, shape, dt=F32, kind="Internal"):
        if kind == "Internal" and getattr(self, "debug", False):
            kind = "ExternalOutput"
        t = self.nc.dram_tensor(name, list(shape), dt, kind=kind)
        b = Buf(t.ap())
        return b

    def dmaq(self, out, in_, reads=(), writes=(), eng="sp", output=False, **kw):
        return self.S.dma(eng, out, in_, reads, writes, output=output, **kw)

    def op(self, eng, fn, reads=(), writes=()):
        return self.S.op(eng, fn, reads, writes)

    def mm(self, out, lhsT, rhs, start, stop, reads, writes):
        return self.S.op("pe", lambda e: e.matmul(out, lhsT, rhs, start=start, stop=stop), reads, writes)

    def tr(self, out, in_, ident, reads, writes):
        return self.S.op("pe", lambda e: e.transpose(out, in_, ident), reads, writes)


def _setup(B):
    nc = B.nc
    NT, TP = B.NT, B.TP
    d = {}

    def inp(name, shape, dt=F32):
        d[name] = B.dram(name, shape, dt, kind="ExternalInput")

    def outp(name, shape):
        d[name] = B.dram(name, shape, F32, kind="ExternalOutput")

    inp("x_prompt", [TP, D_MODEL])
    inp("x_sample", [128, D_MODEL])
    for l_ in range(2):
        inp("cache_k%d" % l_, [B.NLP * 128, 256])
        inp("cache_v%d" % l_, [B.NLP * 128, 256])
    inp("state_conv_a", [2, 32, 2, 256])
    inp("state_conv_b", [2, 32, 3, 768])
    inp("state_gdn", [2, 128, 4096])
    inp("state_hgrn", [2, 128, 4096])
    inp("page_table", [32, B.NPS], I32)
    inp("w_in", [2, D_MODEL, D_IN])
    inp("conv_a", [2, 3, 256])
    inp("conv_b", [2, 4, 768])
    inp("gdn_a_log", [2, 4])
    inp("gdn_dt_bias", [2, 4])
    inp("norm_b", [2, 64])
    inp("lower_bounds", [2, 256])
    inp("norm_c", [2, 64])
    for nm in ("lambda_q1", "lambda_k1", "lambda_q2", "lambda_k2"):
        inp(nm, [2, 32])
    inp("norm_d", [2, 64])
    inp("rel_bias", [32, 4])
    inp("w_o", [2, D_MODEL, D_MODEL])
    inp("ln1_g", [2, D_MODEL])
    inp("ln1_b", [2, D_MODEL])
    inp("ffn_w_gate", [1, D_MODEL, D_FF])
    inp("ffn_w_up", [1, D_MODEL, D_FF])
    inp("ffn_w_down", [1, D_FF, D_MODEL])
    inp("router_w", [1, D_MODEL, N_EXP])
    inp("moe_w_gate", [1, N_EXP, D_MODEL, D_FFE])
    inp("moe_w_up", [1, N_EXP, D_MODEL, D_FFE])
    inp("moe_w_down", [1, N_EXP, D_FFE, D_MODEL])
    inp("ln2_g", [2, D_MODEL])
    inp("ln2_b", [2, D_MODEL])
    inp("c_ident", [128, 128])
    inp("c_misc", [128, 8])
    inp("c_bkt_diag", [128, 33, 128])
    inp("c_bkt_prev", [128, 32, 128])
    inp("c_masks", [128, 8, 128])
    inp("c_scan", [128, 1024])
    inp("c_new", [128, 5, 128])
    inp("c_rep", [32, 128])
    inp("c_pid", [32, B.NLP])
    inp("c_lastb", [128, 4, 32])
    inp("c_ind", [16, 512])
    inp("pt_own", [4 * B.NPS], I32)
    inp("c_ownL", [128, 4 * B.NPS])
    inp("c_isl", [16, 4 * B.NPS])

    outp("y_prompt", [TP, D_MODEL])
    outp("y_sample", [128, D_MODEL])
    outp("k_prompt", [2, TP, 256])
    outp("v_prompt", [2, TP, 256])
    outp("k_sample", [2, 128, 256])
    outp("v_sample", [2, 128, 256])
    outp("conv_a_prompt", [2, 2, 256])
    outp("conv_a_sample", [2, 32, 2, 256])
    outp("conv_b_prompt", [2, 3, 768])
    outp("conv_b_sample", [2, 32, 3, 768])
    outp("gdn_prompt", [2, 256, 64])
    outp("gdn_sample", [2, 128, 4096])
    outp("hgrn_prompt", [2, 256, 64])
    outp("hgrn_sample", [2, 128, 4096])

    d["uT"] = B.dram("uT", [FM_N, NT])
    d["uM"] = B.dram("uM", [NT, TM_N])
    d["yT"] = B.dram("yT", [D_MODEL, NT], BF16)
    d["x1"] = B.dram("x1", [NT, D_MODEL])
    d["x1T"] = B.dram("x1T", [D_MODEL, NT], BF16)
    d["facc"] = B.dram("facc", [NT, D_MODEL])
    d["xs"] = B.dram("xs", [NT, D_MODEL])
    d["gate"] = B.dram("gate", [NT, N_EXP])
    d["uMs"] = B.dram("uMs", [128, 512])
    d["agin"] = B.dram("agin", [128, 520])
    d["agout"] = B.dram("agout", [B.n_cores * 128, 520])
    d["sqd"] = B.dram("sqd", [128, 768])
    d["sod"] = B.dram("sod", [2, 128, 256])
    B.d = d

    B.ident = B.sb("ident", [128, 128])
    B.dmaq(B.ident[:], d["c_ident"][:], writes=[B.ident.res])
    B.masks = B.sb("masks", [128, 8, 128])
    B.dmaq(B.masks[:, :, :], d["c_masks"][:, :, :], writes=[B.masks.res])
    B.psum = [B.ps("ps%d" % i, [128, 512]) for i in range(8)]
    B.ps_i = 0


def _psum2(B):
    p = B.ps_i % 4
    B.ps_i += 1
    return B.psum[2 * p], B.psum[2 * p + 1]


def _psum1(B):
    p = B.ps_i2 % 8 if hasattr(B, "ps_i2") else 0
    B.ps_i2 = p + 1
    return B.psum[p]


def _load_w_bf16(B, dst, src_ap, nk, ncols, stage_ring, eng_cast="pool"):
    src = src_ap.rearrange("(k p) n -> p k n", p=128)
    for k in range(nk):
        st = stage_ring.next()
        B.dmaq(st[:, :ncols], src[:, k, :], writes=[st.res])
        B.op(eng_cast, (lambda e, st=st, k=k: e.tensor_copy(dst[:, k, :], st[:, :ncols])),
             reads=[st.res], writes=[dst.res])


def _phase_inproj(B, l, xsrc_prompt, xsrc_sample):
    d = B.d
    NT, TP = B.NT, B.TP
    B.phase_begin()
    w = B.sb("w_in", [128, 8, D_IN], BF16)
    B.stage = B.ring("stage", [128, D_IN], F32, 2)
    B.xT_ring = B.ring("xT", [128, 8, 512], BF16, 2)
    B.x_ring = B.ring("xt", [128, 1024], F32, 3)
    B.ev_ring = B.ring("ev", [128, 512], F32, 4)
    _load_w_bf16(B, w, d["w_in"][l], 8, D_IN, B.stage)
    ntiles = NT // 128
    groups = []
    t = 0
    while t < ntiles:
        g = min(4, ntiles - t)
        groups.append((t, g))
        t += g
    def grp(t0, g):
        ntok = g * 128
        xT = B.xT_ring.next()
        for j in range(g):
            ti = t0 + j
            xt = B.x_ring.next()
            if ti < B.NPT:
                B.dmaq(xt[:], xsrc_prompt[ti * 128:(ti + 1) * 128, :], writes=[xt.res],
                       reads=[B.xsrc_res])
            else:
                B.dmaq(xt[:], xsrc_sample[:, :], writes=[xt.res], reads=[B.xsrc_res])
            pa, pb = _psum2(B)
            for k in range(8):
                pp = pa if k < 4 else pb
                B.tr(pp[:, (k % 4) * 128:(k % 4 + 1) * 128], xt[:, k * 128:(k + 1) * 128], B.ident[:],
                     reads=[xt.res, B.ident.res], writes=[pp.res])
            B.op("act", (lambda e, xT=xT, pa=pa, j=j: e.copy(
                xT[:, 0:4, j * 128:(j + 1) * 128], pa[:, :].rearrange("p (k t) -> p k t", k=4))),
                reads=[pa.res], writes=[xT.res])
            B.op("dve", (lambda e, xT=xT, pb=pb, j=j: e.tensor_copy(
                xT[:, 4:8, j * 128:(j + 1) * 128], pb[:, :].rearrange("p (k t) -> p k t", k=4))),
                reads=[pb.res], writes=[xT.res])
        row = 0
        ci = 0
        for (c0, cn) in FM_COLS:
            for cc in range(cn // 128):
                col = c0 + cc * 128
                pp = _psum1(B)
                for k in range(8):
                    B.mm(pp[:, :ntok], w[:, k, col:col + 128], xT[:, k, :ntok], k == 0, k == 7,
                         reads=[w.res, xT.res], writes=[pp.res])
                ev = B.ev_ring.next()
                eng = "act" if ci % 2 == 0 else "dve"
                if eng == "act":
                    B.op("act", (lambda e, ev=ev, pp=pp: e.copy(ev[:, :ntok], pp[:, :ntok])),
                         reads=[pp.res], writes=[ev.res])
                else:
                    B.op("dve", (lambda e, ev=ev, pp=pp: e.tensor_copy(ev[:, :ntok], pp[:, :ntok])),
                         reads=[pp.res], writes=[ev.res])
                B.dmaq(d["uT"][row:row + 128, t0 * 128:t0 * 128 + ntok], ev[:, :ntok],
                       reads=[ev.res], writes=[B.uT_res], eng="pool")
                row += 128
                ci += 1
        for j in range(g):
            ti = t0 + j
            colo = 0
            for (c0, cn) in TM_COLS:
                off = 0
                while off < cn:
                    n = min(512, cn - off)
                    pp = _psum1(B)
                    for k in range(8):
                        B.mm(pp[:, :n], xT[:, k, j * 128:(j + 1) * 128], w[:, k, c0 + off:c0 + off + n],
                             k == 0, k == 7, reads=[w.res, xT.res], writes=[pp.res])
                    ev = B.ev_ring.next()
                    eng = "act" if ci % 2 == 0 else "dve"
                    ci += 1
                    if eng == "act":
                        B.op("act", (lambda e, ev=ev, pp=pp, n=n: e.copy(ev[:, :n], pp[:, :n])),
                             reads=[pp.res], writes=[ev.res])
                    else:
                        B.op("dve", (lambda e, ev=ev, pp=pp, n=n: e.tensor_copy(ev[:, :n], pp[:, :n])),
                             reads=[pp.res], writes=[ev.res])
                    B.dmaq(d["uM"][ti * 128:(ti + 1) * 128, colo + off:colo + off + n], ev[:, :n],
                           reads=[ev.res], writes=[B.uM_res], eng="pool")
                    if c0 == O_DK:
                        if ti < B.NPT:
                            ko, vo = d["k_prompt"][l, ti * 128:(ti + 1) * 128, :], d["v_prompt"][l, ti * 128:(ti + 1) * 128, :]
                        else:
                            ko, vo = d["k_sample"][l, :, :], d["v_sample"][l, :, :]
                        B.dmaq(ko, ev[:, 0:256], reads=[ev.res], eng="pool", output=True)
                        B.dmaq(vo, ev[:, 256:512], reads=[ev.res], eng="pool", output=True)
                    off += n
                colo += cn
            if ti >= B.NPT:
                pp = _psum1(B)
                for k in range(8):
                    B.mm(pp[:, :512], xT[:, k, j * 128:(j + 1) * 128], w[:, k, O_CQ:O_CQ + 512],
                         k == 0, k == 7, reads=[w.res, xT.res], writes=[pp.res])
                ev = B.ev_ring.next()
                B.op("act", (lambda e, ev=ev, pp=pp: e.copy(ev[:, :512], pp[:, :512])), reads=[pp.res], writes=[ev.res])
                B.dmaq(d["uMs"][:, :], ev[:, :512], reads=[ev.res], writes=[B.uM_res], eng="pool")

    for (t0, g) in groups:
        grp(t0, g)
    B.phase_end()


def build(NPT=32, n_cores=8, n_local_pages=640, n_pages_seq=128, debug=False, nlayers=2, mixers="abcdsp"):
    B = Builder(NPT, n_cores, n_local_pages, n_pages_seq)
    B.debug = debug
    _setup(B)
    d = B.d
    for nm in ("uT_res", "uM_res", "xsrc_res", "yT_res", "x1_res", "x1T_res", "gate_res", "facc_res"):
        setattr(B, nm, Res())
    B.sm_ring = Ring([B.sb("sm%d" % i, [128, 16], F32, glob=True) for i in range(12)])
    B.rbb = B.sb("rbb", [128, 128], glob=True)
    B.Bd = B.sb("Bd", [128, 4, 128], glob=True)
    B.Bp = B.sb("Bp", [128, 4, 128], glob=True)
    B.lam = B.sb("lam", [128, 4], glob=True)
    B.nrmd = B.sb("nrmd", [128, 128], glob=True)
    B.phase_begin()
    B.G = B.ring("G", [128, 1026], F32, 4)
    B.ybf = B.ring("ybf", [128, 1024], BF16, 1)
    zt = B.ybf.next()
    B.op("pool", lambda e: e.memset(zt[:, :], 0.0), writes=[zt.res])
    for r in range(8):
        for c0 in range(0, B.NT, 1024):
            n = min(1024, B.NT - c0)
            B.dmaq(d["yT"][r * 128:(r + 1) * 128, c0:c0 + n], zt[:, :n], reads=[zt.res], writes=[B.yT_res])
    B.dmaq(B.nrmd[:, :], d["norm_d"].t.rearrange("l d -> (l d)").partition_broadcast(128), writes=[B.nrmd.res])
    _attn_consts(B)
    lam_inits = [_lambda(B, l) for l in range(2)]
    B.phase_end()
    if "p" in mixers or "q" in mixers:
        _sample_attn_setup(B)
    xp, xsm = d["x_prompt"], d["x_sample"]
    for l in range(nlayers):
        lam_init = lam_inits[l]
        _phase_inproj(B, l, xp, xsm)
        if "a" in mixers:
            _mixer_a(B, l)
        if "d" in mixers:
            _mixer_d_prompt(B, l, lam_init)
        if "c" in mixers:
            _mixer_c_prompt(B, l)
        if "b" in mixers:
            _mixer_b_prompt(B, l)
        if "s" in mixers:
            _sample_bc(B, l)
        if "p" in mixers:
            _sample_attn(B, l, lam_init)
        _phase_outproj(B, l, xp, xsm)
        _phase_ffn(B, l)
        if l == 0:
            _phase_ln2(B, l, d["xs"][0:B.TP, :], d["xs"][B.TP:B.NT, :], False)
            xp, xsm = d["xs"][0:B.TP, :], d["xs"][B.TP:B.NT, :]
        else:
            _phase_ln2(B, l, d["y_prompt"], d["y_sample"], True)
    B.phase_begin()
    B.phase_end()
    B.es.close()
    return B


def _own_consts(core, nps):
    nl = 4 * nps
    qseq = np.arange(128)[:, None] // 4
    lseq = 4 * core + np.arange(nl)[None, :] // nps
    ownl = (qseq == lseq).astype(np.float32)
    isl = np.broadcast_to(((np.arange(nl) % nps) == nps - 1).astype(np.float32)[None, :], (16, nl)).copy()
    return ownl, isl


def _host_consts(core, n_local_pages):
    ident = np.eye(128, dtype=np.float32)
    misc = np.zeros((128, 8), np.float32)
    misc[:, 0] = core * n_local_pages
    def bucket(n):
        n = np.maximum(n, 0)
        nf = np.maximum(n, 16).astype(np.float32)
        large = 16 + (np.log(nf / 16) / np.float32(math.log(128 / 16)) * 16).astype(np.int32)
        large = np.minimum(large, 31)
        return np.where(n < 16, n, large)
    kk = np.arange(128)[:, None]
    qq = np.arange(128)[None, :]
    bd = bucket(qq - kk)
    diag = np.zeros((128, 33, 128), np.float32)
    prev = np.zeros((128, 32, 128), np.float32)
    for b in range(32):
        diag[:, b, :] = ((bd == b) & (qq >= kk))
    diag[:, 32, :] = (qq < kk)
    bp = bucket(qq + 128 - kk)
    for b in range(32):
        prev[:, b, :] = (bp == b)
    masks = np.zeros((128, 8, 128), np.float32)
    jj = np.arange(128)[:, None]
    ii = np.arange(128)[None, :]
    masks[:, 0, :] = (jj <= ii)
    masks[:, 1, :] = (jj <= ii) & (jj // 64 == ii // 64)
    masks[:, 2, :] = (jj < ii)
    masks[:, 3, :] = np.where(jj < ii, 0.0, -30000.0)
    masks[:, 4, :] = 1.0
    masks[:, 6, :] = (jj // 64 == ii // 64)
    scan = np.ones((128, 1024), np.float32)
    scan[:, ::64] = 0.0
    misc[:, 1] = np.arange(128)
    kb_, kt_ = np.arange(128)[:, None] // 4, np.arange(128)[:, None] % 4
    qb_, qt_ = np.arange(128)[None, :] // 4, np.arange(128)[None, :] % 4
    cnew = np.zeros((128, 5, 128), np.float32)
    for dist in range(4):
        cnew[:, dist, :] = (kb_ == qb_) & (qt_ - kt_ == dist)
    cnew[:, 4, :] = ~((kb_ == qb_) & (kt_ <= qt_))
    rep = (np.arange(32)[:, None] == (np.arange(128)[None, :] // 4)).astype(np.float32)
    pid = np.broadcast_to((core * n_local_pages + np.arange(n_local_pages, dtype=np.float32))[None, :], (32, n_local_pages)).copy()
    lastb = np.zeros((128, 4, 32), np.float32)
    rr = np.arange(128)[:, None]
    tt = np.arange(4)[None, :]
    bl = bucket(128 + tt - rr)
    for b in range(32):
        lastb[:, :, b] = (bl == b)
    ind = np.zeros((16, 4, 32, 4), np.float32)
    for h in range(4):
        for t in range(4):
            ind[h * 4 + t, h, :, t] = 1.0
    ind = ind.reshape(16, 512)
    return ident, misc, diag, prev, masks, scan, cnew, rep, pid, lastb, ind


def _bcast_row(B, dst, src_row_ap, n):
    B.dmaq(dst[:, :n], src_row_ap.partition_broadcast(128), writes=[dst.res])


def _layernorm(B, tt, g, b, out):
    st = B.sm_ring.next()
    B.op("dve", lambda e: e.bn_stats(st[:, 0:6], tt[:, 0:512]), reads=[tt.res], writes=[st.res])
    B.op("dve", lambda e: e.bn_stats(st[:, 6:12], tt[:, 512:1024]), reads=[tt.res], writes=[st.res])
    B.op("dve", lambda e: e.bn_aggr(st[:, 12:14], st[:, 0:12]), reads=[st.res], writes=[st.res])
    B.op("dve", lambda e: e.tensor_scalar(st[:, 14:15], st[:, 13:14], LN_EPS, None, OP.add), reads=[st.res], writes=[st.res])
    B.op("act", lambda e: e.activation(st[:, 14:15], st[:, 14:15], AF.Ln), reads=[st.res], writes=[st.res])
    B.op("act", lambda e: e.activation(st[:, 14:15], st[:, 14:15], AF.Exp, scale=-0.5), reads=[st.res], writes=[st.res])
    B.op("dve", lambda e: e.scalar_tensor_tensor(st[:, 15:16], st[:, 12:13], -1.0, st[:, 14:15], OP.mult, OP.mult),
         reads=[st.res], writes=[st.res])
    B.op("act", lambda e: e.activation(tt[:, :], tt[:, :], AF.Identity, bias=st[:, 15:16], scale=st[:, 14:15]),
         reads=[st.res, tt.res], writes=[tt.res])
    B.op("pool", lambda e: e.tensor_tensor(out[:, :], tt[:, :], g[:, :], OP.mult),
         reads=[tt.res, g.res], writes=[out.res])
    B.op("pool", lambda e: e.tensor_tensor(out[:, :], out[:, :], b[:, :], OP.add),
         reads=[out.res, b.res], writes=[out.res])


def _phase_outproj(B, l, xsrc_prompt, xsrc_sample):
    d = B.d
    B.phase_begin()
    wo = B.sb("wo", [128, 8, D_MODEL], BF16)
    B.stage = B.ring("stage", [128, D_MODEL], F32, 2)
    B.lng = B.sb("lng", [128, D_MODEL])
    B.lnb = B.sb("lnb", [128, D_MODEL])
    B.t_ring = B.ring("tt", [128, 1024], F32, 6)
    B.x_ring = B.t_ring
    B.yT_ring = B.ring("yTt", [128, 8, 128], BF16, 4)
    B.xTf = B.sb("xTf", [128, 8, 128])
    B.wr = B.sb("wr", [128, 8, 8])
    _load_w_bf16(B, wo, d["w_o"][l], 8, D_MODEL, B.stage)
    _bcast_row(B, B.lng, d["ln1_g"][l], D_MODEL)
    _bcast_row(B, B.lnb, d["ln1_b"][l], D_MODEL)
    moe = (l % 2 == 1)
    if moe:
        B.dmaq(B.wr[:, :, :], d["router_w"][0].rearrange("(k p) n -> p k n", p=128), writes=[B.wr.res])
    yTv = d["yT"].t.rearrange("(k p) t -> p k t", p=128)
    x1Tv = d["x1T"].t.rearrange("(k p) t -> p k t", p=128)
    def tile(ti):
        tok = slice(ti * 128, (ti + 1) * 128)
        xt = B.x_ring.next()
        if ti < B.NPT:
            B.dmaq(xt[:], xsrc_prompt[tok, :], writes=[xt.res], reads=[B.xsrc_res])
        else:
            B.dmaq(xt[:], xsrc_sample[:, :], writes=[xt.res], reads=[B.xsrc_res])
        yt = B.yT_ring.next()
        B.dmaq(yt[:, :, :], yTv[:, :, tok], writes=[yt.res], reads=[B.yT_res])
        pa, pb = _psum2(B)
        for h, pp in enumerate((pa, pb)):
            for k in range(8):
                B.mm(pp[:, :], yt[:, k, :], wo[:, k, h * 512:(h + 1) * 512], k == 0, k == 7,
                     reads=[yt.res, wo.res], writes=[pp.res])
        tt = B.t_ring.next()
        for h, pp in enumerate((pa, pb)):
            B.op("dve", (lambda e, pp=pp, h=h: e.scalar_tensor_tensor(
                tt[:, h * 512:(h + 1) * 512], xt[:, h * 512:(h + 1) * 512], ALPHA, pp[:, :], OP.mult, OP.add)),
                reads=[xt.res, pp.res], writes=[tt.res])
        x1 = B.t_ring.next()
        _layernorm(B, tt, B.lng, B.lnb, x1)
        B.dmaq(d["x1"][tok, :], x1[:, :], reads=[x1.res], writes=[B.x1_res], eng="pool")
        pa, pb = _psum2(B)
        for k in range(8):
            pp = pa if k < 4 else pb
            B.tr(pp[:, (k % 4) * 128:(k % 4 + 1) * 128], x1[:, k * 128:(k + 1) * 128], B.ident[:],
                 reads=[x1.res, B.ident.res], writes=[pp.res])
        xT = B.yT_ring.next()
        B.op("act", lambda e: e.copy(xT[:, 0:4, :], pa[:, :].rearrange("p (k t) -> p k t", k=4)),
             reads=[pa.res], writes=[xT.res])
        B.op("dve", lambda e: e.tensor_copy(xT[:, 4:8, :], pb[:, :].rearrange("p (k t) -> p k t", k=4)),
             reads=[pb.res], writes=[xT.res])
        B.dmaq(x1Tv[:, :, tok], xT[:, :, :], reads=[xT.res], writes=[B.x1T_res], eng="pool")
        gt = B.sm_ring.next()
        if moe:
            xTf = B.xTf
            B.op("act", lambda e: e.copy(xTf[:, 0:4, :], pa[:, :].rearrange("p (k t) -> p k t", k=4)),
                 reads=[pa.res], writes=[xTf.res])
            B.op("dve", lambda e: e.tensor_copy(xTf[:, 4:8, :], pb[:, :].rearrange("p (k t) -> p k t", k=4)),
                 reads=[pb.res], writes=[xTf.res])
            pr = _psum1(B)
            for k in range(8):
                B.mm(pr[:, 0:8], xTf[:, k, :], B.wr[:, k, :], k == 0, k == 7,
                     reads=[xTf.res, B.wr.res], writes=[pr.res])
            lg = B.sm_ring.next()
            B.op("dve", lambda e: e.tensor_copy(lg[:, 0:8], pr[:, 0:8]), reads=[pr.res], writes=[lg.res])
            B.op("dve", lambda e: e.tensor_reduce(lg[:, 8:9], lg[:, 0:8], AX.X, OP.max), reads=[lg.res], writes=[lg.res])
            mk = B.sm_ring.next()
            B.op("dve", lambda e: e.tensor_scalar(mk[:, 0:8], lg[:, 0:8], lg[:, 8:9], None, OP.is_equal),
                 reads=[lg.res], writes=[mk.res])
            B.op("dve", lambda e: e.scalar_tensor_tensor(mk[:, 8:16], mk[:, 0:8], -1e30, lg[:, 0:8], OP.mult, OP.add),
                 reads=[lg.res, mk.res], writes=[mk.res])
            B.op("dve", lambda e: e.tensor_reduce(lg[:, 9:10], mk[:, 8:16], AX.X, OP.max), reads=[mk.res], writes=[lg.res])
            B.op("dve", lambda e: e.tensor_scalar(mk[:, 8:16], mk[:, 8:16], lg[:, 9:10], None, OP.is_equal),
                 reads=[lg.res, mk.res], writes=[mk.res])
            B.op("dve", lambda e: e.tensor_tensor(lg[:, 10:11], lg[:, 8:9], lg[:, 9:10], OP.subtract),
                 reads=[lg.res], writes=[lg.res])
            B.op("act", lambda e: e.activation(lg[:, 10:11], lg[:, 10:11], AF.Sigmoid), reads=[lg.res], writes=[lg.res])
            B.op("dve", lambda e: e.tensor_scalar(lg[:, 11:12], lg[:, 10:11], -1.0, 1.0, OP.mult, OP.add),
                 reads=[lg.res], writes=[lg.res])
            B.op("dve", lambda e: e.tensor_scalar(gt[:, 0:8], mk[:, 0:8], lg[:, 10:11], None, OP.mult),
                 reads=[lg.res, mk.res], writes=[gt.res])
            B.op("dve", lambda e: e.scalar_tensor_tensor(gt[:, 0:8], mk[:, 8:16], lg[:, 11:12], gt[:, 0:8], OP.mult, OP.add),
                 reads=[lg.res, mk.res, gt.res], writes=[gt.res])
        else:
            B.op("pool", lambda e: e.memset(gt[:, 0:8], 1.0), writes=[gt.res])
        B.dmaq(d["gate"][tok, :], gt[:, 0:8], reads=[gt.res], writes=[B.gate_res], eng="pool")

    for ti in range(B.NT // 128):
        tile(ti)
    B.phase_end()


def _phase_ffn(B, l):
    d = B.d
    moe = (l % 2 == 1)
    B.phase_begin()
    Ws = [B.sb("arena%d" % i, [128, 33792], BF16) for i in range(2)]
    B.stage = B.ring("stage", [128, D_FFE], F32, 2)
    B.xT_ring = B.ring("xT", [128, 8, 512], BF16, 1)
    B.hT_ring = B.ring("hT", [128, 11, 512], BF16, 1)
    B.ev_ring = B.ring("ev", [128, 512], F32, 3)
    B.t_ring = B.ring("tt", [128, 1024], F32, 2)
    B.sm4_ring = B.ring("sm4", [128, 4, 8], F32, 2)
    x1Tv = d["x1T"].t.rearrange("(k p) t -> p k t", p=128)
    n_exp = N_EXP if moe else 2
    ntiles = B.NT // 128
    groups = []
    t = 0
    while t < ntiles:
        g = min(4, ntiles - t)
        groups.append((t, g))
        t += g
    for ex in range(n_exp):
        W = Ws[ex % 2]
        wflat = W.t
        wg = wflat[:, 0:8 * D_FFE].rearrange("p (k n) -> p k n", k=8)
        wu = wflat[:, 8 * D_FFE:16 * D_FFE].rearrange("p (k n) -> p k n", k=8)
        wd = wflat[:, 16 * D_FFE:16 * D_FFE + 11 * D_MODEL].rearrange("p (k n) -> p k n", k=11)
        if moe:
            sg, su, sd = d["moe_w_gate"][0, ex], d["moe_w_up"][0, ex], d["moe_w_down"][0, ex]
        else:
            sg = d["ffn_w_gate"][0][:, ex * D_FFE:(ex + 1) * D_FFE]
            su = d["ffn_w_up"][0][:, ex * D_FFE:(ex + 1) * D_FFE]
            sd = d["ffn_w_down"][0][ex * D_FFE:(ex + 1) * D_FFE, :]
        for (dstv, src, nk, ncol) in ((wg, sg, 8, D_FFE), (wu, su, 8, D_FFE), (wd, sd, 11, D_MODEL)):
            srcv = src.rearrange("(k p) n -> p k n", p=128)
            for k in range(nk):
                st = B.stage.next()
                B.dmaq(st[:, :ncol], srcv[:, k, :], writes=[st.res])
                B.op("pool", (lambda e, st=st, k=k, dstv=dstv, ncol=ncol: e.tensor_copy(dstv[:, k, :], st[:, :ncol])),
                     reads=[st.res], writes=[W.res])
        def group(t0, g, ex=ex, W=W, wg=wg, wu=wu, wd=wd):
            ntok = g * 128
            xT = B.xT_ring.next()
            B.dmaq(xT[:, :, :ntok], x1Tv[:, :, t0 * 128:t0 * 128 + ntok], writes=[xT.res], reads=[B.x1T_res])
            gsb = B.sm4_ring.next()
            B.dmaq(gsb[:, :g, :], d["gate"][t0 * 128:t0 * 128 + ntok, :].rearrange("(j p) n -> p j n", p=128),
                   writes=[gsb.res], reads=[B.gate_res])
            hT = B.hT_ring.next()
            for fc in range(11):
                pg, pu = _psum2(B)
                for k in range(8):
                    B.mm(pg[:, :ntok], wg[:, k, fc * 128:(fc + 1) * 128], xT[:, k, :ntok], k == 0, k == 7,
                         reads=[W.res, xT.res], writes=[pg.res])
                for k in range(8):
                    B.mm(pu[:, :ntok], wu[:, k, fc * 128:(fc + 1) * 128], xT[:, k, :ntok], k == 0, k == 7,
                         reads=[W.res, xT.res], writes=[pu.res])
                sg_ = B.ev_ring.next()
                B.op("act", (lambda e, sg_=sg_, pg=pg: e.activation(sg_[:, :ntok], pg[:, :ntok], AF.Silu)),
                     reads=[pg.res], writes=[sg_.res])
                B.op("dve", (lambda e, sg_=sg_, pu=pu, fc=fc, hT=hT: e.tensor_tensor(hT[:, fc, :ntok], sg_[:, :ntok], pu[:, :ntok], OP.mult)),
                     reads=[sg_.res, pu.res], writes=[hT.res])
            for j in range(g):
                ti = t0 + j
                pa, pb = _psum2(B)
                for h, pp in enumerate((pa, pb)):
                    for k in range(11):
                        B.mm(pp[:, :], hT[:, k, j * 128:(j + 1) * 128], wd[:, k, h * 512:(h + 1) * 512], k == 0, k == 10,
                             reads=[hT.res, W.res], writes=[pp.res])
                fo = B.t_ring.next()
                ge = ex if moe else 0
                B.op("act", (lambda e, fo=fo, pa=pa, gsb=gsb, j=j, ge=ge: e.activation(
                    fo[:, 0:512], pa[:, :], AF.Copy, scale=gsb[:, j, ge:ge + 1])),
                    reads=[pa.res, gsb.res], writes=[fo.res])
                B.op("dve", (lambda e, fo=fo, pb=pb, gsb=gsb, j=j, ge=ge: e.tensor_scalar(
                    fo[:, 512:1024], pb[:, :], gsb[:, j, ge:ge + 1], None, OP.mult)),
                    reads=[pb.res, gsb.res], writes=[fo.res])
                tok = slice(ti * 128, (ti + 1) * 128)
                if ex == 0:
                    B.dmaq(d["facc"][tok, :], fo[:, :], reads=[fo.res], writes=[B.facc_res], eng="pool")
                else:
                    B.dmaq(d["facc"][tok, :], fo[:, :], reads=[fo.res], writes=[B.facc_res], eng="pool",
                           accum_op=OP.add)

        for (t0, g) in groups:
            group(t0, g)
    B.phase_end()


def _phase_ln2(B, l, dst_prompt, dst_sample, final):
    d = B.d
    B.phase_begin()
    B.lng = B.sb("lng", [128, D_MODEL])
    B.lnb = B.sb("lnb", [128, D_MODEL])
    B.t_ring = B.ring("tt", [128, 1024], F32, 8)
    B.x_ring = B.t_ring
    _bcast_row(B, B.lng, d["ln2_g"][l], D_MODEL)
    _bcast_row(B, B.lnb, d["ln2_b"][l], D_MODEL)
    for ti in range(B.NT // 128):
        tok = slice(ti * 128, (ti + 1) * 128)
        xt = B.x_ring.next()
        B.dmaq(xt[:], d["x1"][tok, :], writes=[xt.res], reads=[B.x1_res])
        ft = B.x_ring.next()
        B.dmaq(ft[:], d["facc"][tok, :], writes=[ft.res], reads=[B.facc_res])
        tt = B.t_ring.next()
        B.op("dve", (lambda e, tt=tt, xt=xt, ft=ft: e.scalar_tensor_tensor(tt[:, :], xt[:, :], ALPHA, ft[:, :], OP.mult, OP.add)),
             reads=[xt.res, ft.res], writes=[tt.res])
        x2 = B.t_ring.next()
        _layernorm(B, tt, B.lng, B.lnb, x2)
        if ti < B.NPT:
            B.dmaq(dst_prompt[tok, :], x2[:, :], reads=[x2.res], writes=[B.xsrc_res], eng="pool", output=final)
        else:
            B.dmaq(dst_sample[:, :], x2[:, :], reads=[x2.res], writes=[B.xsrc_res], eng="pool", output=final)
    B.phase_end()


def _mixer_a(B, l):
    d = B.d
    TP = B.TP
    SEG = min(1024, TP)
    B.phase_begin()
    B.G = B.ring("G", [128, 1026], F32, 8)
    B.ybf = B.ring("ybf", [128, 1024], BF16, 2)
    B.zprev = B.sb("zprev", [128, 2])
    cw = B.sb("cwa", [128, 2, 3])
    for c in range(2):
        B.dmaq(cw[:, c, :], d["conv_a"][l].rearrange("j (c p) -> p c j", p=128)[:, c, :], writes=[cw.res],
               allow_slow_non_contiguous=True)
    for c in range(2):
        rows = lambda r0: slice(r0 + c * 128, r0 + (c + 1) * 128)

        def seg_fn(s0, n, first, last, c=c, rows=rows):
            ain, agc, agb = B.G.next(), B.G.next(), B.G.next()
            B.dmaq(ain[:, :n], d["uT"][rows(R_AIN), s0:s0 + n], writes=[ain.res], reads=[B.uT_res])
            B.dmaq(agc[:, :n], d["uT"][rows(R_AGC), s0:s0 + n], writes=[agc.res], reads=[B.uT_res])
            B.dmaq(agb[:, :n], d["uT"][rows(R_AGB), s0:s0 + n], writes=[agb.res], reads=[B.uT_res])
            z = B.G.next()
            zp = B.zprev
            if first:
                B.op("pool", lambda e: e.memset(z[:, 0:2], 0.0), writes=[z.res])
            else:
                B.op("pool", lambda e: e.tensor_copy(z[:, 0:2], zp[:, 0:2]), reads=[zp.res], writes=[z.res])
            B.op("pool", lambda e: e.tensor_tensor(z[:, 2:2 + n], agc[:, :n], ain[:, :n], OP.mult),
                 reads=[agc.res, ain.res], writes=[z.res])
            B.op("pool", lambda e: e.tensor_copy(zp[:, 0:2], z[:, n:n + 2]), reads=[z.res], writes=[zp.res])
            y = agc
            B.op("dve", lambda e: e.tensor_scalar(y[:, :n], z[:, 0:n], cw[:, c, 0:1], None, OP.mult),
                 reads=[z.res, cw.res], writes=[y.res])
            B.op("dve", lambda e: e.scalar_tensor_tensor(y[:, :n], z[:, 1:n + 1], cw[:, c, 1:2], y[:, :n], OP.mult, OP.add),
                 reads=[z.res, cw.res, y.res], writes=[y.res])
            B.op("dve", lambda e: e.scalar_tensor_tensor(y[:, :n], z[:, 2:n + 2], cw[:, c, 2:3], y[:, :n], OP.mult, OP.add),
                 reads=[z.res, cw.res, y.res], writes=[y.res])
            yb = B.ybf.next()
            B.op("dve", lambda e: e.tensor_tensor(yb[:, :n], y[:, :n], agb[:, :n], OP.mult),
                 reads=[y.res, agb.res], writes=[yb.res])
            B.dmaq(d["yT"][c * 128:(c + 1) * 128, s0:s0 + n], yb[:, :n], reads=[yb.res], writes=[B.yT_res], eng="pool")
            if last:
                B.dmaq(d["conv_a_prompt"][l].rearrange("j (c p) -> p c j", p=128)[:, c, :], zp[:, 0:2],
                       reads=[zp.res], eng="pool", output=True, allow_slow_non_contiguous=True)

        nseg = TP // SEG
        for s in range(nseg):
            seg_fn(s * SEG, SEG, s == 0, s == nseg - 1)

        def samp(c=c, rows=rows):
            ain, agc, agb, z = B.G.next(), B.G.next(), B.G.next(), B.G.next()
            B.dmaq(ain[:, :128], d["uT"][rows(R_AIN), TP:TP + 128], writes=[ain.res], reads=[B.uT_res])
            B.dmaq(agc[:, :128], d["uT"][rows(R_AGC), TP:TP + 128], writes=[agc.res], reads=[B.uT_res])
            B.dmaq(agb[:, :128], d["uT"][rows(R_AGB), TP:TP + 128], writes=[agb.res], reads=[B.uT_res])
            zv = z[:, 0:192].rearrange("p (b j) -> p b j", j=6)
            for jj in range(2):
                B.dmaq(zv[:, :, jj],
                       d["state_conv_a"][l].rearrange("b j (c p) -> p c j b", p=128)[:, c, jj, :],
                       writes=[z.res], allow_slow_non_contiguous=True)
            v3 = lambda t, n=4: t[:, 0:128].rearrange("p (b j) -> p b j", j=4)
            B.op("pool", lambda e: e.tensor_tensor(zv[:, :, 2:6], v3(agc), v3(ain), OP.mult),
                 reads=[agc.res, ain.res, z.res], writes=[z.res])
            y = agc
            B.op("dve", lambda e: e.tensor_scalar(v3(y), zv[:, :, 0:4], cw[:, c, 0:1], None, OP.mult),
                 reads=[z.res, cw.res], writes=[y.res])
            B.op("dve", lambda e: e.scalar_tensor_tensor(v3(y), zv[:, :, 1:5], cw[:, c, 1:2], v3(y), OP.mult, OP.add),
                 reads=[z.res, cw.res, y.res], writes=[y.res])
            B.op("dve", lambda e: e.scalar_tensor_tensor(v3(y), zv[:, :, 2:6], cw[:, c, 2:3], v3(y), OP.mult, OP.add),
                 reads=[z.res, cw.res, y.res], writes=[y.res])
            yb = B.ybf.next()
            B.op("dve", lambda e: e.tensor_tensor(yb[:, :128], y[:, :128], agb[:, :128], OP.mult),
                 reads=[y.res, agb.res], writes=[yb.res])
            B.dmaq(d["yT"][c * 128:(c + 1) * 128, TP:TP + 128], yb[:, :128], reads=[yb.res], writes=[B.yT_res], eng="pool")
            for jj in range(2):
                B.dmaq(d["conv_a_sample"][l].rearrange("b j (c p) -> p c j b", p=128)[:, c, jj, :],
                       zv[:, :, 4 + jj], reads=[z.res], eng="pool", output=True,
                       allow_slow_non_contiguous=True)
        samp()
    B.phase_end()


def _attn_consts(B):
    d = B.d
    rb = B.rbb
    B.dmaq(rb[:, :], d["rel_bias"].t.rearrange("b h -> (b h)").partition_broadcast(128), writes=[rb.res])
    Bd, Bp = B.Bd, B.Bp
    for (dst, src, nb) in ((Bd, d["c_bkt_diag"], 33), (Bp, d["c_bkt_prev"], 32)):
        def one(bk, dst=dst, src=src):
            oh = B.G.next()
            B.dmaq(oh[:, 0:128], src[:, bk, :], writes=[oh.res])
            for h in range(4):
                if bk == 32:
                    B.op("dve", (lambda e, h=h: e.scalar_tensor_tensor(dst[:, h, :], oh[:, 0:128], -30000.0, dst[:, h, :], OP.mult, OP.add)),
                         reads=[oh.res, dst.res], writes=[dst.res])
                elif bk == 0:
                    B.op("dve", (lambda e, h=h: e.tensor_scalar(dst[:, h, :], oh[:, 0:128], rb[:, bk * 4 + h:bk * 4 + h + 1], None, OP.mult)),
                         reads=[oh.res, rb.res], writes=[dst.res])
                else:
                    B.op("dve", (lambda e, h=h: e.scalar_tensor_tensor(dst[:, h, :], oh[:, 0:128], rb[:, bk * 4 + h:bk * 4 + h + 1], dst[:, h, :], OP.mult, OP.add)),
                         reads=[oh.res, rb.res, dst.res], writes=[dst.res])
        for bk in range(nb):
            one(bk)


def _lambda(B, l):
    d = B.d
    lam_init = 0.8 - 0.6 * math.exp(-0.3 * l)
    t = B.sm_ring.next()
    lv = B.G.next()
    for i, nm in enumerate(("lambda_q1", "lambda_k1", "lambda_q2", "lambda_k2")):
        B.dmaq(lv[:, i * 32:(i + 1) * 32], d[nm][l].partition_broadcast(128), writes=[lv.res])
    B.op("dve", lambda e: e.tensor_tensor(lv[:, 128:192].rearrange("p (a c) -> p a c", a=2), lv[:, 0:128].rearrange("p (a b c) -> p a b c", a=2, b=2)[:, :, 0, :],
                                          lv[:, 0:128].rearrange("p (a b c) -> p a b c", a=2, b=2)[:, :, 1, :], OP.mult),
         reads=[lv.res], writes=[lv.res])
    B.op("dve", lambda e: e.tensor_reduce(t[:, 0:2], lv[:, 128:192].rearrange("p (a c) -> p a c", a=2), AX.X, OP.add),
         reads=[lv.res], writes=[t.res])
    B.op("act", lambda e: e.activation(t[:, 0:2], t[:, 0:2], AF.Exp), reads=[t.res], writes=[t.res])
    lam = B.lam
    B.op("dve", lambda e: e.tensor_tensor(lam[:, l:l + 1], t[:, 0:1], t[:, 1:2], OP.subtract), reads=[t.res], writes=[lam.res])
    B.op("dve", lambda e: e.tensor_scalar(lam[:, l:l + 1], lam[:, l:l + 1], lam_init, None, OP.add), reads=[lam.res], writes=[lam.res])
    B.op("dve", lambda e: e.tensor_scalar(lam[:, 2 + l:3 + l], lam[:, l:l + 1], -1.0, None, OP.mult), reads=[lam.res], writes=[lam.res])
    return lam_init


def _diff_combine(B, l, lam_init, O1, O2, ncols_tok, nw, yrow0, tok0, h):
    d = B.d
    w = B.sm_ring.next()
    o = B.ev_ring.next()
    lam = B.lam
    B.op("dve", lambda e: e.reciprocal(w[:, 0:1], O1[:, 64:65]), reads=nw, writes=[w.res])
    B.op("dve", lambda e: e.reciprocal(w[:, 1:2], O2[:, 64:65]), reads=nw, writes=[w.res])
    B.op("dve", lambda e: e.tensor_tensor(w[:, 1:2], w[:, 1:2], lam[:, 2 + l:3 + l], OP.mult), reads=[w.res, lam.res], writes=[w.res])
    B.op("dve", lambda e: e.tensor_scalar(o[:, 0:64], O1[:, 0:64], w[:, 0:1], None, OP.mult), reads=nw + [w.res], writes=[o.res])
    B.op("dve", lambda e: e.scalar_tensor_tensor(o[:, 0:64], O2[:, 0:64], w[:, 1:2], o[:, 0:64], OP.mult, OP.add),
         reads=nw + [w.res, o.res], writes=[o.res])
    B.op("dve", lambda e: e.tensor_tensor(o[:, 64:128], o[:, 0:64], o[:, 0:64], OP.mult), reads=[o.res], writes=[o.res])
    B.op("dve", lambda e: e.tensor_reduce(w[:, 2:3], o[:, 64:128], AX.X, OP.add), reads=[o.res], writes=[w.res])
    B.op("dve", lambda e: e.tensor_scalar(w[:, 4:5], w[:, 2:3], 64 * RMS_EPS, None, OP.add), reads=[w.res], writes=[w.res])
    B.op("act", lambda e: e.activation(w[:, 4:5], w[:, 4:5], AF.Ln), reads=[w.res], writes=[w.res])
    B.op("act", lambda e: e.activation(w[:, 4:5], w[:, 4:5], AF.Exp, scale=-0.5), reads=[w.res], writes=[w.res])
    B.op("dve", lambda e: e.tensor_scalar(w[:, 3:4], w[:, 4:5], 8.0 * (1.0 - lam_init), None, OP.mult), reads=[w.res], writes=[w.res])
    B.op("dve", lambda e: e.scalar_tensor_tensor(o[:, 128:192], o[:, 0:64], w[:, 3:4], B.nrmd[:, l * 64:(l + 1) * 64], OP.mult, OP.mult),
         reads=[o.res, w.res, B.nrmd.res], writes=[o.res])
    pt = B.psum[7]
    B.tr(pt[0:64, 0:128], o[:, 128:192], B.ident[:], reads=[o.res, B.ident.res], writes=[pt.res])
    yb = B.ybf.next()
    B.op("act", lambda e: e.copy(yb[0:64, 0:128], pt[0:64, 0:128]), reads=[pt.res], writes=[yb.res])
    B.dmaq(d["yT"][yrow0:yrow0 + 64, tok0:tok0 + 128], yb[0:64, 0:128], reads=[yb.res], writes=[B.yT_res], eng="pool")


def _mixer_d_prompt(B, l, lam_init):
    d = B.d
    TP, NPT = B.TP, B.NPT
    SC = 32 ** -0.5
    B.phase_begin()
    qTb = B.sb("qTb", [64, TP], BF16)
    kTb = B.sb("kTb", [64, TP], BF16)
    Vaug = B.sb("Vaug", [128, NPT, 65], BF16)
    B.E_ring = B.ring("E", [128, 512], BF16, 4)
    zl = B.sb("zl", [128, 128], BF16)
    zr = B.sb("zr", [128, 260], BF16)
    B.op("pool", lambda e: e.memset(zl[:, :], 0.0), writes=[zl.res])
    B.op("pool", lambda e: e.memset(zr[:, :], 0.0), writes=[zr.res])
    B.ev_ring = B.ring("ev", [128, 512], F32, 4)
    B.G = B.ring("G", [128, 1026], F32, 4)
    B.ybf = B.ring("ybf", [128, 1024], BF16, 2)
    rb = B.rbb
    for h in range(4):
        def load_head(h=h):
            for (dst, r0) in ((qTb, R_DQ), (kTb, R_DK)):
                for m in range(2):
                    row = r0 + h * 64 + m * 32
                    for s0 in range(0, TP, 1024):
                        n = min(1024, TP - s0)
                        st = B.G.next()
                        B.dmaq(st[m * 32:(m + 1) * 32, :n], d["uT"][row:row + 32, s0:s0 + n], writes=[st.res], reads=[B.uT_res])
                        B.op("pool", (lambda e, st=st, dst=dst, m=m, s0=s0, n=n: e.tensor_copy(dst[m * 32:(m + 1) * 32, s0:s0 + n], st[m * 32:(m + 1) * 32, :n])),
                             reads=[st.res], writes=[dst.res])
            vs = B.G.next()
            for j0 in range(0, NPT, 16):
                nj = min(16, NPT - j0)
                B.dmaq(vs[:, :nj * 64].rearrange("p (j d) -> p j d", d=64),
                       d["uM"][j0 * 128:(j0 + nj) * 128, C_DV + h * 64:C_DV + (h + 1) * 64].rearrange("(j p) d -> p j d", p=128),
                       writes=[vs.res], reads=[B.uM_res])
                B.op("pool", (lambda e, j0=j0, nj=nj: e.tensor_copy(Vaug[:, j0:j0 + nj, 0:64], vs[:, :nj * 64].rearrange("p (j d) -> p j d", d=64))),
                     reads=[vs.res], writes=[Vaug.res])
            B.op("pool", lambda e: e.memset(Vaug[:, :, 64:65], 1.0), writes=[Vaug.res])
        load_head()
        nqt = (NPT + 3) // 4
        for qt in range(nqt):
            def qtile(qt=qt, h=h):
                ns = min(4, NPT - qt * 4)
                nq = ns * 128
                OA, OB = B.psum[2 * (qt % 2)], B.psum[2 * (qt % 2) + 1]
                jmax = qt * 4 + ns - 1
                for OO in (OA, OB):
                    B.mm(OO[:, 0:ns * 65], zl[:, :], zr[:, 0:ns * 65], True, False, reads=[zl.res, zr.res], writes=[OO.res])
                for j in range(jmax + 1):
                    for m, OO in ((0, OA), (1, OB)):
                        sp = _psum1s(B)
                        B.mm(sp[:, :nq], kTb[m * 32:(m + 1) * 32, j * 128:(j + 1) * 128], qTb[m * 32:(m + 1) * 32, qt * 512:qt * 512 + nq], True, True,
                             reads=[kTb.res, qTb.res], writes=[sp.res])
                        E = B.E_ring.next()
                        far_all = (j <= qt * 4 - 2)
                        if far_all:
                            B.op("act", (lambda e, sp=sp, E=E: e.activation(E[:, :nq], sp[:, :nq], AF.Exp,
                                                                           bias=rb[:, 31 * 4 + h:31 * 4 + h + 1], scale=SC)),
                                 reads=[sp.res, rb.res], writes=[E.res])
                            subs = list(range(ns))
                        else:
                            subs = []
                            for s in range(ns):
                                i = qt * 4 + s
                                cs = slice(s * 128, (s + 1) * 128)
                                if j > i:
                                    continue
                                subs.append(s)
                                if j <= i - 2:
                                    B.op("act", (lambda e, sp=sp, E=E, cs=cs: e.activation(E[:, cs], sp[:, cs], AF.Exp,
                                                                                          bias=rb[:, 31 * 4 + h:31 * 4 + h + 1], scale=SC)),
                                         reads=[sp.res, rb.res], writes=[E.res])
                                else:
                                    Bm = B.Bd if j == i else B.Bp
                                    tmp = B.ev_ring.next()
                                    B.op("dve", (lambda e, sp=sp, tmp=tmp, cs=cs, Bm=Bm: e.scalar_tensor_tensor(
                                        tmp[:, 0:128], sp[:, cs], SC, Bm[:, h, :], OP.mult, OP.add)),
                                        reads=[sp.res, Bm.res], writes=[tmp.res])
                                    B.op("act", (lambda e, tmp=tmp, E=E, cs=cs: e.activation(E[:, cs], tmp[:, 0:128], AF.Exp)),
                                         reads=[tmp.res], writes=[E.res])
                        for s in subs:
                            i = qt * 4 + s
                            B.mm(OO[:, s * 65:(s + 1) * 65], E[:, s * 128:(s + 1) * 128], Vaug[:, j, :], False, (j == jmax and s == ns - 1),
                                 reads=[E.res, Vaug.res], writes=[OO.res])
                for s in range(ns):
                    i = qt * 4 + s
                    _diff_combine(B, l, lam_init, OA[:, s * 65:(s + 1) * 65], OB[:, s * 65:(s + 1) * 65], 128,
                                  [OA.res, OB.res], 768 + h * 64, i * 128, h)
            qtile()
    B.phase_end()


def _psum1s(B):
    p = getattr(B, "ps_s", 0)
    B.ps_s = p + 1
    return B.psum[4 + (p % 3)]


_CACHE = {}


def kernel(**inputs):
    NC = 8
    NLP = 5120
    if "B" not in _CACHE:
        _CACHE["B"] = build(NPT=32, n_cores=1, n_local_pages=NLP, n_pages_seq=128)
    B = _CACHE["B"]
    f = lambda a: np.ascontiguousarray(np.asarray(a))
    shared = {}
    for nm in ("w_in", "conv_a", "conv_b", "gdn_a_log", "gdn_dt_bias", "norm_b", "lower_bounds", "norm_c",
               "lambda_q1", "lambda_k1", "lambda_q2", "lambda_k2", "norm_d", "rel_bias", "w_o", "ln1_g", "ln1_b",
               "ffn_w_gate", "ffn_w_up", "ffn_w_down", "router_w", "moe_w_gate", "moe_w_up", "moe_w_down",
               "ln2_g", "ln2_b", "state_conv_a", "state_conv_b", "page_table"):
        shared[nm] = f(inputs[nm])
    shared["x_sample"] = f(inputs["x_sample"]).reshape(128, D_MODEL)
    shared["state_gdn"] = f(inputs["state_gdn"]).reshape(2, 128, 4096)
    shared["state_hgrn"] = f(inputs["state_hgrn"]).reshape(2, 128, 4096)
    ck = np.asarray(inputs["cache_k"]).reshape(2, NLP * 128, 256)
    cv = np.asarray(inputs["cache_v"]).reshape(2, NLP * 128, 256)
    for l_ in range(2):
        shared["cache_k%d" % l_] = f(ck[l_])
        shared["cache_v%d" % l_] = f(cv[l_])
    consts = _host_consts(0, NLP)
    xp = np.asarray(inputs["x_prompt"])
    in_maps = []
    for c in range(NC):
        m = dict(shared)
        m["x_prompt"] = f(xp[c])
        ident, misc, diag, prev, masks, scan, cnew, rep, pid, lastb, ind = consts
        m["pt_own"] = f(np.asarray(inputs["page_table"])[4 * c:4 * c + 4].reshape(-1).astype(np.int32))
        m["c_ownL"], m["c_isl"] = _own_consts(c, 128)
        m["c_ind"] = ind
        m["c_ident"], m["c_misc"], m["c_bkt_diag"], m["c_bkt_prev"] = ident, misc, diag, prev
        m["c_masks"], m["c_scan"] = masks, scan
        m["c_new"], m["c_rep"], m["c_pid"], m["c_lastb"] = cnew, rep, pid, lastb
        in_maps.append(m)
    res = run_bass_kernel_spmd(B.nc, in_maps, core_ids=list(range(NC))).results
    st = lambda nm: np.stack([res[c][nm] for c in range(NC)], 0)
    y_prompt = st("y_prompt")
    def samp(nm, lead):
        outs_ = []
        for c in range(NC):
            a = res[c][nm].reshape(lead + (32, -1))
            outs_.append(a[..., 4 * c:4 * c + 4, :])
        return np.concatenate(outs_, axis=len(lead))
    y_sample = samp("y_sample", ()).reshape(32, 4, D_MODEL)
    k_p = st("k_prompt").transpose(1, 0, 2, 3).reshape(2, NC, 4096, 4, 64)
    v_p = st("v_prompt").transpose(1, 0, 2, 3).reshape(2, NC, 4096, 4, 64)
    k_s = samp("k_sample", (2,)).reshape(2, 32, 4, 4, 64)
    v_s = samp("v_sample", (2,)).reshape(2, 32, 4, 4, 64)
    ca_p = st("conv_a_prompt").transpose(1, 0, 2, 3)
    ca_s = samp("conv_a_sample", (2,)).reshape(2, 32, 2, 256)
    cb_p = st("conv_b_prompt").transpose(1, 0, 2, 3)
    cb_s = samp("conv_b_sample", (2,)).reshape(2, 32, 3, 768)
    g_p = st("gdn_prompt").transpose(1, 0, 2, 3).reshape(2, NC, 4, 64, 64)
    g_s = samp("gdn_sample", (2,)).reshape(2, 32, 4, 64, 64)
    h_p = st("hgrn_prompt").transpose(1, 0, 2, 3).reshape(2, NC, 4, 64, 64)
    h_s = samp("hgrn_sample", (2,)).reshape(2, 32, 4, 64, 64)
    outs = (y_prompt, y_sample, k_p, v_p, k_s, v_s, ca_p, ca_s, cb_p, cb_s, g_p, g_s, h_p, h_s)
    return tuple(np.ascontiguousarray(o, dtype=np.float32) for o in outs)


def _norm_gate_out(B, o_all, z, nw, yrow0, tok0):
    d = B.d
    sq = B.W256.next()
    st = B.sm_ring.next()
    B.op("pool", lambda e: e.tensor_tensor(sq[:, :], o_all[:, :], o_all[:, :], OP.mult), reads=[o_all.res], writes=[sq.res])
    B.op("dve", lambda e: e.tensor_reduce(st[:, 0:4], sq[:, :].rearrange("p (h d) -> p h d", h=4), AX.X, OP.add),
         reads=[sq.res], writes=[st.res])
    B.op("dve", lambda e: e.tensor_scalar(st[:, 4:8], st[:, 0:4], 1.0 / 64, RMS_EPS, OP.mult, OP.add), reads=[st.res], writes=[st.res])
    B.op("act", lambda e: e.activation(st[:, 4:8], st[:, 4:8], AF.Ln), reads=[st.res], writes=[st.res])
    B.op("act", lambda e: e.activation(st[:, 4:8], st[:, 4:8], AF.Exp, scale=-0.5), reads=[st.res], writes=[st.res])
    sz = B.W256.next()
    B.op("act", lambda e: e.activation(sz[:, :], z, AF.Silu), reads=[B.zres], writes=[sz.res])
    on = sq
    B.op("dve", lambda e: e.tensor_tensor(on[:, :].rearrange("p (h d) -> p h d", h=4), o_all[:, :].rearrange("p (h d) -> p h d", h=4),
                                          st[:, 4:8].unsqueeze(2).broadcast_to([128, 4, 64]), OP.mult),
         reads=[o_all.res, st.res], writes=[on.res])
    B.op("pool", lambda e: e.tensor_tensor(on[:, :], on[:, :], nw[:, :], OP.mult), reads=[on.res, nw.res], writes=[on.res])
    B.op("pool", lambda e: e.tensor_tensor(on[:, :], on[:, :], sz[:, :], OP.mult), reads=[on.res, sz.res], writes=[on.res])
    pt = _psr(B)
    for c in range(2):
        B.tr(pt[:, c * 128:(c + 1) * 128], on[:, c * 128:(c + 1) * 128], B.ident[:], reads=[on.res, B.ident.res], writes=[pt.res])
    yb = B.ybf.next()
    B.op("act", lambda e: e.copy(yb[:, 0:256], pt[:, 0:256]), reads=[pt.res], writes=[yb.res])
    for c in range(2):
        B.dmaq(d["yT"][yrow0 + c * 128:yrow0 + (c + 1) * 128, tok0:tok0 + 128], yb[:, c * 128:(c + 1) * 128],
               reads=[yb.res], writes=[B.yT_res], eng="pool")


def _psr(B):
    p = getattr(B, "ps_r", 0)
    B.ps_r = p + 1
    return B.psum[p % 6]


def _mixer_c_prompt(B, l):
    d = B.d
    TP, NPT = B.TP, B.NPT
    SEG = min(1024, TP)
    M = B.masks
    B.phase_begin()
    B.G = B.ring("G", [128, 1024], F32, 14)
    B.W128 = B.ring("W128", [128, 128], F32, 12)
    B.W256 = B.ring("W256", [128, 256], F32, 8)
    B.ybf = B.ring("ybf", [128, 256], BF16, 3)
    scanm = B.sb("scanm", [128, 1024])
    B.dmaq(scanm[:, :], d["c_scan"][:, :], writes=[scanm.res])
    nw = B.sb("nwc", [128, 256])
    for h in range(4):
        B.dmaq(nw[:, h * 64:(h + 1) * 64], d["norm_c"][l].partition_broadcast(128), writes=[nw.res])
    lbc = B.sb("lbc", [128, 2, 4])
    for pc in range(2):
        if l == 0:
            B.op("pool", (lambda e, pc=pc: e.memset(lbc[:, pc, 0:1], 0.0)), writes=[lbc.res])
        else:
            lbt = B.sm_ring.next()
            B.dmaq(lbt[:, 0:2], d["lower_bounds"].t.rearrange("l (c p) -> p c l", p=128)[:, pc, :], writes=[lbt.res],
                   allow_slow_non_contiguous=True)
            B.op("dve", (lambda e, pc=pc, lbt=lbt: e.tensor_tensor(lbc[:, pc, 3:4], lbt[:, 1:2], lbt[:, 0:1], OP.subtract)),
                 reads=[lbt.res], writes=[lbc.res])
            B.op("act", (lambda e, pc=pc: e.activation(lbc[:, pc, 0:1], lbc[:, pc, 3:4], AF.Sigmoid)), reads=[lbc.res], writes=[lbc.res])
        B.op("dve", (lambda e, pc=pc: e.tensor_scalar(lbc[:, pc, 1:2], lbc[:, pc, 0:1], -1.0, 1.0, OP.mult, OP.add)), reads=[lbc.res], writes=[lbc.res])
        B.op("dve", (lambda e, pc=pc: e.tensor_scalar(lbc[:, pc, 2:3], lbc[:, pc, 1:2], -1.0, None, OP.mult)), reads=[lbc.res], writes=[lbc.res])
    kz = [[B.sb("kz%d_%d" % (c, i), [128, 128]) for i in range(3)] for c in range(2)]
    qz = [[B.sb("qz%d_%d" % (c, i), [128, 128]) for i in range(3)] for c in range(2)]
    for c in range(2):
        for i in range(3):
            for t_ in (kz[c][i], qz[c][i]):
                B.op("pool", (lambda e, t_=t_: e.memset(t_[:, :], 0.0)), writes=[t_.res])
    Sst = [[B.sb("S%d_%d" % (pc, i), [128, 64]) for i in range(3)] for pc in range(2)]
    for pc in range(2):
        B.op("pool", (lambda e, pc=pc: e.memset(Sst[pc][0][:, :], 0.0)), writes=[Sst[pc][0].res])
    sidx = [0, 0]
    kzi = [0]
    scb = [[B.sb("scb%d_%d" % (pc, i), [128, 64]) for i in range(2)] for pc in range(2)]
    nseg = TP // SEG
    for sg in range(nseg):
        def seg(sg=sg):
            s0 = sg * SEG
            nch = SEG // 64
            feat = []
            for pc in range(2):
                cq, cf = B.G.next(), B.G.next()
                B.dmaq(cq[:, :SEG], d["uT"][R_CQ + pc * 128:R_CQ + (pc + 1) * 128, s0:s0 + SEG], writes=[cq.res], reads=[B.uT_res])
                B.dmaq(cf[:, :SEG], d["uT"][R_CF + pc * 128:R_CF + (pc + 1) * 128, s0:s0 + SEG], writes=[cf.res], reads=[B.uT_res])
                sig, lf, kc, bb = B.G.next(), B.G.next(), B.G.next(), B.G.next()
                B.op("act", (lambda e, sig=sig, cf=cf: e.activation(sig[:, :SEG], cf[:, :SEG], AF.Sigmoid)), reads=[cf.res], writes=[sig.res])
                B.op("dve", (lambda e, lf=lf, sig=sig, pc=pc: e.tensor_scalar(lf[:, :SEG], sig[:, :SEG], lbc[:, pc, 1:2], lbc[:, pc, 0:1], OP.mult, OP.add)),
                     reads=[sig.res, lbc.res], writes=[lf.res])
                B.op("act", (lambda e, lf=lf: e.activation(lf[:, :SEG], lf[:, :SEG], AF.Ln)), reads=[lf.res], writes=[lf.res])
                B.op("dve", (lambda e, kc=kc, sig=sig, pc=pc: e.tensor_scalar(kc[:, :SEG], sig[:, :SEG], lbc[:, pc, 2:3], lbc[:, pc, 1:2], OP.mult, OP.add)),
                     reads=[sig.res, lbc.res], writes=[kc.res])
                B.op("dve", (lambda e, bb=bb, lf=lf: e.tensor_tensor_scan(bb[:, :SEG], scanm[:, :SEG], lf[:, :SEG], 0.0, OP.mult, OP.add)),
                     reads=[lf.res, scanm.res], writes=[bb.res])
                sq_ = cf
                B.op("act", (lambda e, sq_=sq_, cq=cq: e.activation(sq_[:, :SEG], cq[:, :SEG], AF.Silu)), reads=[cq.res], writes=[sq_.res])
                sc = scb[pc][sg % 2]
                b3 = lambda t_: t_[:, :SEG].rearrange("p (c j) -> p c j", j=64)
                B.op("pool", (lambda e, sc=sc, bb=bb: e.tensor_copy(sc[:, 0:nch], b3(bb)[:, :, 31])), reads=[bb.res], writes=[sc.res])
                B.op("pool", (lambda e, sc=sc, bb=bb: e.tensor_copy(sc[:, 16:16 + nch], b3(bb)[:, :, 63])), reads=[bb.res], writes=[sc.res])
                B.op("dve", (lambda e, sc=sc: e.tensor_tensor(sc[:, 32:32 + nch], sc[:, 16:16 + nch], sc[:, 0:nch], OP.subtract)), reads=[sc.res], writes=[sc.res])
                B.op("act", (lambda e, sc=sc: e.activation(sc[:, 32:32 + nch], sc[:, 32:32 + nch], AF.Exp)), reads=[sc.res], writes=[sc.res])
                B.op("act", (lambda e, sc=sc: e.activation(sc[:, 48:48 + nch], sc[:, 16:16 + nch], AF.Exp)), reads=[sc.res], writes=[sc.res])
                dl, qt_, kt_, qe = lf, B.G.next(), sig, cq
                B.op("dve", (lambda e, dl=dl, bb=bb, sc=sc: e.tensor_tensor(b3(dl), b3(bb), sc[:, 0:nch].unsqueeze(2).broadcast_to([128, nch, 64]), OP.subtract)),
                     reads=[bb.res, sc.res], writes=[dl.res])
                B.op("act", (lambda e, qt_=qt_, dl=dl: e.activation(qt_[:, :SEG], dl[:, :SEG], AF.Exp)), reads=[dl.res], writes=[qt_.res])
                B.op("act", (lambda e, kt_=kt_, dl=dl: e.activation(kt_[:, :SEG], dl[:, :SEG], AF.Exp, scale=-1.0)), reads=[dl.res], writes=[kt_.res])
                B.op("act", (lambda e, qe=qe, bb=bb: e.activation(qe[:, :SEG], bb[:, :SEG], AF.Exp)), reads=[bb.res], writes=[qe.res])
                B.op("pool", (lambda e, qt_=qt_, sq_=sq_: e.tensor_tensor(qt_[:, :SEG], qt_[:, :SEG], sq_[:, :SEG], OP.mult)), reads=[qt_.res, sq_.res], writes=[qt_.res])
                B.op("pool", (lambda e, kt_=kt_, kc=kc: e.tensor_tensor(kt_[:, :SEG], kt_[:, :SEG], kc[:, :SEG], OP.mult)), reads=[kt_.res, kc.res], writes=[kt_.res])
                B.op("pool", (lambda e, qe=qe, sq_=sq_: e.tensor_tensor(qe[:, :SEG], qe[:, :SEG], sq_[:, :SEG], OP.mult)), reads=[qe.res, sq_.res], writes=[qe.res])
                feat.append((qt_, kt_, qe, sc))
                if B.debug and l == 0 and sg == 0 and pc == 0:
                    for nm, t_ in (("dbgc_b", bb), ("dbgc_q", qt_), ("dbgc_k", kt_), ("dbgc_qe", qe), ("dbgc_lf", kc)):
                        dd = B.dram(nm, [128, SEG])
                        B.dmaq(dd[:, :], t_[:, :SEG], reads=[t_.res], output=True)
                    dd = B.dram("dbgc_sc", [128, 64])
                    B.dmaq(dd[:, :], sc[:, :], reads=[sc.res], output=True)
            for tl in range(SEG // 128):
                def tile(tl=tl):
                    ti = s0 // 128 + tl
                    tok0 = ti * 128
                    c0 = tl * 128
                    vg = B.W256.next()
                    zg = B.W256.next()
                    B.dmaq(vg[:, :], d["uM"][tok0:tok0 + 128, C_CI:C_CI + 256], writes=[vg.res], reads=[B.uM_res])
                    B.dmaq(zg[:, :], d["uM"][tok0:tok0 + 128, C_CG:C_CG + 256], writes=[zg.res], reads=[B.uM_res])
                    po = B.psum[6 + (ti % 2)]
                    for pc in range(2):
                        qt_, kt_, qe, sc = feat[pc]
                        ki = kzi[0] % 3
                        kzi[0] += 1
                        kz0, kz1, qz0, qz1 = kz[0][ki], kz[1][ki], qz[0][ki], qz[1][ki]
                        B.op("pool", (lambda e, kz0=kz0, kt_=kt_: e.tensor_copy(kz0[:, 0:64], kt_[:, c0:c0 + 64])), reads=[kt_.res], writes=[kz0.res])
                        B.op("pool", (lambda e, kz1=kz1, kt_=kt_: e.tensor_copy(kz1[:, 64:128], kt_[:, c0 + 64:c0 + 128])), reads=[kt_.res], writes=[kz1.res])
                        B.op("pool", (lambda e, qz0=qz0, qe=qe: e.tensor_copy(qz0[:, 0:64], qe[:, c0:c0 + 64])), reads=[qe.res], writes=[qz0.res])
                        B.op("pool", (lambda e, qz1=qz1, qe=qe: e.tensor_copy(qz1[:, 64:128], qe[:, c0 + 64:c0 + 128])), reads=[qe.res], writes=[qz1.res])
                        ptk = _psr(B)
                        B.tr(ptk[:, 0:128], kz0[:, :], B.ident[:], reads=[kz0.res, B.ident.res], writes=[ptk.res])
                        B.tr(ptk[:, 128:256], kz1[:, :], B.ident[:], reads=[kz1.res, B.ident.res], writes=[ptk.res])
                        kzT = B.W256.next()
                        B.op("act", (lambda e, kzT=kzT, ptk=ptk: e.copy(kzT[:, :], ptk[:, 0:256])), reads=[ptk.res], writes=[kzT.res])
                        S0 = Sst[pc][sidx[pc] % 3]
                        S1 = Sst[pc][(sidx[pc] + 1) % 3]
                        S2 = Sst[pc][(sidx[pc] + 2) % 3]
                        sidx[pc] += 2
                        ch = (c0 // 64)
                        for (cc, Sa, Sb_) in ((0, S0, S1), (1, S1, S2)):
                            pS = _psr(B)
                            B.mm(pS[:, 0:128], kzT[:, cc * 128:(cc + 1) * 128], vg[:, pc * 128:(pc + 1) * 128], True, True,
                                 reads=[kzT.res, vg.res], writes=[pS.res])
                            for hh in range(2):
                                rows = slice(hh * 64, (hh + 1) * 64)
                                tmp = B.W128.next()
                                B.op("dve", (lambda e, tmp=tmp, pS=pS, rows=rows, hh=hh, sc=sc, cc=cc: e.tensor_scalar(
                                    tmp[rows, 0:64], pS[rows, hh * 64:(hh + 1) * 64], sc[rows, 32 + ch + cc:33 + ch + cc], None, OP.mult)),
                                    reads=[pS.res, sc.res], writes=[tmp.res])
                                B.op("dve", (lambda e, tmp=tmp, Sa=Sa, Sb_=Sb_, rows=rows, sc=sc, cc=cc: e.scalar_tensor_tensor(
                                    Sb_[rows, :], Sa[rows, :], sc[rows, 48 + ch + cc:49 + ch + cc], tmp[rows, 0:64], OP.mult, OP.add)),
                                    reads=[Sa.res, tmp.res, sc.res], writes=[Sb_.res])
                        for hh in range(2):
                            h = pc * 2 + hh
                            rows = slice(hh * 64, (hh + 1) * 64)
                            pa = _psr(B)
                            B.mm(pa[:, 0:64], kz0[rows, :], qt_[rows, c0:c0 + 64], True, True, reads=[kz0.res, qt_.res], writes=[pa.res])
                            B.mm(pa[:, 64:128], kz1[rows, :], qt_[rows, c0 + 64:c0 + 128], True, True, reads=[kz1.res, qt_.res], writes=[pa.res])
                            aT = B.W128.next()
                            B.op("dve", (lambda e, aT=aT, pa=pa: e.tensor_tensor(aT[:, :], pa[:, 0:128], M[:, 1, :], OP.mult)),
                                 reads=[pa.res, M.res], writes=[aT.res])
                            oc = slice(h * 64, (h + 1) * 64)
                            B.mm(po[:, oc], aT[:, :], vg[:, oc], True, False, reads=[aT.res, vg.res], writes=[po.res])
                            B.mm(po[:, oc], qz0[rows, :], S0[rows, :], False, False, reads=[qz0.res, S0.res], writes=[po.res])
                            B.mm(po[:, oc], qz1[rows, :], S1[rows, :], False, True, reads=[qz1.res, S1.res], writes=[po.res])
                    o_all = B.W256.next()
                    B.op("act", (lambda e, o_all=o_all, po=po: e.copy(o_all[:, :], po[:, 0:256])), reads=[po.res], writes=[o_all.res])
                    if B.debug and l == 0 and ti == 0:
                        dd = B.dram("dbgc_o", [128, 256])
                        B.dmaq(dd[:, :], o_all[:, :], reads=[o_all.res], output=True)
                    B.zres = zg.res
                    _norm_gate_out(B, o_all, zg[:, :], nw, 512, tok0)
                tile()
        seg()
    if B.debug and l == 0:
        for i in range(3):
            dd = B.dram("dbgc_S%d" % i, [128, 64])
            B.dmaq(dd[:, :], Sst[0][i][:, :], reads=[Sst[0][i].res], output=True)
    for pc in range(2):
        Sf = Sst[pc][sidx[pc] % 3]
        B.dmaq(d["hgrn_prompt"][l, pc * 128:(pc + 1) * 128, :], Sf[:, :], reads=[Sf.res], output=True)
    B.phase_end()


def _mixer_b_prompt(B, l):
    d = B.d
    TP, NPT = B.TP, B.NPT
    M = B.masks
    ident = B.ident
    B.phase_begin()
    B.W256 = B.ring("W256", [128, 256], F32, 6)
    B.ybf = B.ring("ybf", [128, 256], BF16, 3)
    cw = B.sb("cwb", [128, 4, 768])
    B.dmaq(cw[:, :, :].rearrange("p j c -> p (j c)"), d["conv_b"][l].rearrange("j c -> (j c)").partition_broadcast(128), writes=[cw.res])
    nAb = B.sb("nAb", [128, 8])
    B.dmaq(nAb[:, 0:4], d["gdn_a_log"][l].partition_broadcast(128), writes=[nAb.res])
    B.dmaq(nAb[:, 4:8], d["gdn_dt_bias"][l].partition_broadcast(128), writes=[nAb.res])
    B.op("act", lambda e: e.activation(nAb[:, 0:4], nAb[:, 0:4], AF.Exp), reads=[nAb.res], writes=[nAb.res])
    B.op("dve", lambda e: e.tensor_scalar(nAb[:, 0:4], nAb[:, 0:4], -1.0, None, OP.mult), reads=[nAb.res], writes=[nAb.res])
    nwb = B.sb("nwb", [128, 256])
    for h in range(4):
        B.dmaq(nwb[:, h * 64:(h + 1) * 64], d["norm_b"][l].partition_broadcast(128), writes=[nwb.res])
    Xs = [B.sb("X%d" % j, [128, 768]) for j in range(4)]
    acc = B.sb("acc", [128, 768])
    tmpc = B.sb("tmpc", [128, 768])
    qkv = B.sb("qkv", [128, 768])
    sqb = B.sb("sqb", [128, 512])
    smx = B.sb("smx", [128, 64])
    kn, qn, kb, vb, qg = [B.sb(nm, [128, 256]) for nm in ("kn", "qn", "kb", "vb", "qg")]
    bab = B.sb("bab", [128, 8])
    zt = B.sb("ztb", [128, 256])
    kbgz = B.sb("kbgz", [128, 4, 128])
    kdecz = B.sb("kdecz", [128, 4, 128])
    B.op("pool", lambda e: e.memset(kbgz[:, :, :], 0.0), writes=[kbgz.res])
    B.op("pool", lambda e: e.memset(kdecz[:, :, :], 0.0), writes=[kdecz.res])
    knT, kbT, qnT, qgT = [B.sb(nm, [128, 2, 128]) for nm in ("knT", "kbT", "qnT", "qgT")]
    o_all = B.sb("o_allb", [128, 256])
    H = []
    for h in range(4):
        hb = {}
        for nm in ("diag", "arg", "Dst", "PT", "Xa", "Xb", "Ya", "Yb", "TT", "wT"):
            hb[nm] = B.sb("%s%d" % (nm, h), [128, 128])
        for nm in ("u", "vnew"):
            hb[nm] = B.sb("%s%d" % (nm, h), [128, 64])
        H.append(hb)
    Sp = [[B.sb("Sb%d_%d" % (blk, i), [128, 64]) for i in range(2)] for blk in range(2)]
    for blk in range(2):
        B.op("pool", (lambda e, blk=blk: e.memset(Sp[blk][0][:, :], 0.0)), writes=[Sp[blk][0].res])
    bc3 = lambda col: col.unsqueeze(2).broadcast_to([128, 4, 64])
    v3 = lambda ap: ap.rearrange("p (h d) -> p h d", h=4)

    for ti in range(NPT):
        def tile(ti=ti):
            tok0 = ti * 128
            Sin = [Sp[blk][ti % 2] for blk in range(2)]
            Sout = [Sp[blk][(ti + 1) % 2] for blk in range(2)]
            for j in range(4):
                sh = 3 - j
                if ti == 0 and sh > 0:
                    B.op("pool", (lambda e, j=j, sh=sh: e.memset(Xs[j][0:sh, :], 0.0)), writes=[Xs[j].res])
                    B.dmaq(Xs[j][sh:128, :], d["uM"][0:128 - sh, C_BQKV:C_BQKV + 768], writes=[Xs[j].res], reads=[B.uM_res])
                else:
                    B.dmaq(Xs[j][:, :], d["uM"][tok0 - sh:tok0 - sh + 128, C_BQKV:C_BQKV + 768], writes=[Xs[j].res], reads=[B.uM_res])
            B.dmaq(bab[:, :], d["uM"][tok0:tok0 + 128, C_BA:C_BA + 8], writes=[bab.res], reads=[B.uM_res])
            B.dmaq(zt[:, :], d["uM"][tok0:tok0 + 128, C_BZ:C_BZ + 256], writes=[zt.res], reads=[B.uM_res])
            B.op("dve", lambda e: e.tensor_tensor(acc[:, :], Xs[0][:, :], cw[:, 0, :], OP.mult), reads=[Xs[0].res, cw.res], writes=[acc.res])
            for j in range(1, 4):
                B.op("pool", (lambda e, j=j: e.tensor_tensor(tmpc[:, :], Xs[j][:, :], cw[:, j, :], OP.mult)), reads=[Xs[j].res, cw.res], writes=[tmpc.res])
                B.op("dve", lambda e: e.tensor_tensor(acc[:, :], acc[:, :], tmpc[:, :], OP.add), reads=[acc.res, tmpc.res], writes=[acc.res])
            B.op("act", lambda e: e.activation(qkv[:, :], acc[:, :], AF.Silu), reads=[acc.res], writes=[qkv.res])
            B.op("pool", lambda e: e.tensor_tensor(sqb[:, :], qkv[:, 0:512], qkv[:, 0:512], OP.mult), reads=[qkv.res], writes=[sqb.res])
            B.op("dve", lambda e: e.tensor_reduce(smx[:, 28:36], sqb[:, :].rearrange("p (h d) -> p h d", h=8), AX.X, OP.add), reads=[sqb.res], writes=[smx.res])
            B.op("dve", lambda e: e.tensor_scalar(smx[:, 28:36], smx[:, 28:36], RMS_EPS, None, OP.add), reads=[smx.res], writes=[smx.res])
            B.op("act", lambda e: e.activation(smx[:, 28:36], smx[:, 28:36], AF.Ln), reads=[smx.res], writes=[smx.res])
            B.op("act", lambda e: e.activation(smx[:, 28:36], smx[:, 28:36], AF.Exp, scale=-0.5), reads=[smx.res], writes=[smx.res])
            B.op("dve", lambda e: e.tensor_scalar(smx[:, 28:32], smx[:, 28:32], 0.125, None, OP.mult), reads=[smx.res], writes=[smx.res])
            B.op("dve", lambda e: e.tensor_tensor(smx[:, 0:4], bab[:, 0:4], nAb[:, 4:8], OP.add), reads=[bab.res, nAb.res], writes=[smx.res])
            B.op("act", lambda e: e.activation(smx[:, 0:4], smx[:, 0:4], AF.Exp), reads=[smx.res], writes=[smx.res])
            B.op("act", lambda e: e.activation(smx[:, 4:8], bab[:, 4:8], AF.Exp, scale=-1.0), reads=[bab.res, smx.res], writes=[smx.res])
            B.op("dve", lambda e: e.tensor_scalar(smx[:, 0:8], smx[:, 0:8], 1.0, None, OP.add), reads=[smx.res], writes=[smx.res])
            B.op("act", lambda e: e.activation(smx[:, 0:4], smx[:, 0:4], AF.Ln), reads=[smx.res], writes=[smx.res])
            B.op("dve", lambda e: e.reciprocal(smx[:, 4:8], smx[:, 4:8]), reads=[smx.res], writes=[smx.res])
            B.op("dve", lambda e: e.tensor_tensor(smx[:, 0:4], smx[:, 0:4], nAb[:, 0:4], OP.mult), reads=[smx.res, nAb.res], writes=[smx.res])
            pg = _psr(B)
            B.mm(pg[:, 0:4], M[:, 0, :], smx[:, 0:4], True, True, reads=[M.res, smx.res], writes=[pg.res])
            B.mm(pg[:, 4:8], M[:, 4, :], smx[:, 0:4], True, True, reads=[M.res, smx.res], writes=[pg.res])
            B.op("dve", lambda e: e.tensor_copy(smx[:, 8:16], pg[:, 0:8]), reads=[pg.res, smx.res], writes=[smx.res])
            B.op("act", lambda e: e.activation(smx[:, 16:20], smx[:, 8:12], AF.Exp), reads=[smx.res], writes=[smx.res])
            B.op("dve", lambda e: e.tensor_tensor(smx[:, 20:24], smx[:, 12:16], smx[:, 8:12], OP.subtract), reads=[smx.res], writes=[smx.res])
            B.op("act", lambda e: e.activation(smx[:, 20:24], smx[:, 20:24], AF.Exp), reads=[smx.res], writes=[smx.res])
            B.op("act", lambda e: e.activation(smx[:, 24:28], smx[:, 12:16], AF.Exp), reads=[smx.res], writes=[smx.res])
            B.op("dve", lambda e: e.tensor_tensor(smx[:, 36:40], smx[:, 4:8], smx[:, 16:20], OP.mult), reads=[smx.res], writes=[smx.res])
            qv, kv_, vv = qkv[:, 0:256], qkv[:, 256:512], qkv[:, 512:768]
            B.op("dve", lambda e: e.tensor_tensor(v3(kn[:, :]), v3(kv_), bc3(smx[:, 32:36]), OP.mult), reads=[qkv.res, smx.res], writes=[kn.res])
            B.op("pool", lambda e: e.tensor_tensor(v3(qn[:, :]), v3(qv), bc3(smx[:, 28:32]), OP.mult), reads=[qkv.res, smx.res], writes=[qn.res])
            B.op("pool", lambda e: e.tensor_tensor(v3(vb[:, :]), v3(vv), bc3(smx[:, 4:8]), OP.mult), reads=[qkv.res, smx.res], writes=[vb.res])
            B.op("dve", lambda e: e.tensor_tensor(v3(kb[:, :]), v3(kn[:, :]), bc3(smx[:, 4:8]), OP.mult), reads=[kn.res, smx.res], writes=[kb.res])
            B.op("pool", lambda e: e.tensor_tensor(v3(qg[:, :]), v3(qn[:, :]), bc3(smx[:, 16:20]), OP.mult), reads=[qn.res, smx.res], writes=[qg.res])
            for h in range(4):
                cs = slice((h % 2) * 64, (h % 2) * 64 + 64)
                hs = slice(h * 64, (h + 1) * 64)
                B.op("dve", (lambda e, h=h, cs=cs, hs=hs: e.tensor_scalar(kbgz[:, h, cs], kn[:, hs], smx[:, 36 + h:37 + h], None, OP.mult)),
                     reads=[kn.res, smx.res], writes=[kbgz.res])
                B.op("pool", (lambda e, h=h, cs=cs, hs=hs: e.tensor_scalar(kdecz[:, h, cs], kn[:, hs], smx[:, 20 + h:21 + h], None, OP.mult)),
                     reads=[kn.res, smx.res], writes=[kdecz.res])
            for (src, dst) in ((kn, knT), (kb, kbT), (qn, qnT), (qg, qgT)):
                pt = _psr(B)
                for c in range(2):
                    B.tr(pt[:, c * 128:(c + 1) * 128], src[:, c * 128:(c + 1) * 128], ident[:], reads=[src.res, ident.res], writes=[pt.res])
                B.op("act", (lambda e, dst=dst, pt=pt: e.copy(dst[:, :, :].rearrange("p c t -> p (c t)"), pt[:, 0:256])),
                     reads=[pt.res], writes=[dst.res])
            for h in range(4):
                hb = H[h]
                rows = slice((h % 2) * 64, (h % 2) * 64 + 64)
                blk = h // 2
                B.op("pool", (lambda e, hb=hb, h=h: e.tensor_scalar(hb["diag"][:, :], ident[:, :], smx[:, 8 + h:9 + h], None, OP.mult)),
                     reads=[ident.res, smx.res], writes=[hb["diag"].res])
                pA = _psr(B)
                B.mm(pA[:, 0:128], M[:, 4, :], hb["diag"][:, :], True, True, reads=[M.res, hb["diag"].res], writes=[pA.res])
                B.op("dve", (lambda e, hb=hb, h=h, pA=pA: e.scalar_tensor_tensor(hb["arg"][:, :], pA[:, 0:128], smx[:, 8 + h:9 + h], M[:, 3, :], OP.subtract, OP.add)),
                     reads=[pA.res, smx.res, M.res], writes=[hb["arg"].res])
                B.op("act", (lambda e, hb=hb: e.activation(hb["Dst"][:, :], hb["arg"][:, :], AF.Exp)), reads=[hb["arg"].res], writes=[hb["Dst"].res])
                pB = _psr(B)
                B.mm(pB[:, 0:128], knT[rows, blk, :], kbT[rows, blk, :], True, True, reads=[knT.res, kbT.res], writes=[pB.res])
                B.op("dve", (lambda e, hb=hb, pB=pB: e.scalar_tensor_tensor(hb["Xa"][:, :], pB[:, 0:128], -1.0, hb["Dst"][:, :], OP.mult, OP.mult)),
                     reads=[pB.res, hb["Dst"].res], writes=[hb["Xa"].res])
                pC = _psr(B)
                B.mm(pC[:, 0:128], knT[rows, blk, :], qnT[rows, blk, :], True, True, reads=[knT.res, qnT.res], writes=[pC.res])
                B.op("pool", (lambda e, hb=hb: e.tensor_tensor(hb["arg"][:, :], hb["Dst"][:, :], ident[:, :], OP.add)),
                     reads=[hb["Dst"].res, ident.res, hb["arg"].res], writes=[hb["arg"].res])
                B.op("dve", (lambda e, hb=hb, pC=pC: e.tensor_tensor(hb["PT"][:, :], pC[:, 0:128], hb["arg"][:, :], OP.mult)),
                     reads=[pC.res, hb["arg"].res], writes=[hb["PT"].res])
                pD = _psr(B)
                B.tr(pD[:, 0:128], hb["Xa"][:, :], ident[:], reads=[hb["Xa"].res, ident.res], writes=[pD.res])
                B.op("act", (lambda e, hb=hb, pD=pD: e.copy(hb["Ya"][:, :], pD[:, 0:128])), reads=[pD.res], writes=[hb["Ya"].res])
                B.op("pool", (lambda e, hb=hb: e.tensor_tensor(hb["TT"][:, :], hb["Xa"][:, :], ident[:, :], OP.add)),
                     reads=[hb["Xa"].res, ident.res], writes=[hb["TT"].res])
            for k in range(6):
                for h in range(4):
                    hb = H[h]
                    X, Y = (hb["Xa"], hb["Ya"]) if k % 2 == 0 else (hb["Xb"], hb["Yb"])
                    Xn, Yn = (hb["Xb"], hb["Yb"]) if k % 2 == 0 else (hb["Xa"], hb["Ya"])
                    pY = _psr(B)
                    B.mm(pY[:, 0:128], X[:, :], Y[:, :], True, True, reads=[X.res, Y.res], writes=[pY.res])
                    if k < 5:
                        pX = _psr(B)
                        B.mm(pX[:, 0:128], Y[:, :], X[:, :], True, True, reads=[X.res, Y.res], writes=[pX.res])
                    B.op("act", (lambda e, Yn=Yn, pY=pY: e.copy(Yn[:, :], pY[:, 0:128])), reads=[pY.res], writes=[Yn.res])
                    if k < 5:
                        B.op("dve", (lambda e, Xn=Xn, pX=pX: e.tensor_copy(Xn[:, :], pX[:, 0:128])), reads=[pX.res], writes=[Xn.res])
                    pT = _psr(B)
                    B.mm(pT[:, 0:128], Yn[:, :], hb["TT"][:, :], True, True, reads=[Yn.res, hb["TT"].res], writes=[pT.res])
                    B.op("dve", (lambda e, hb=hb, pT=pT: e.tensor_tensor(hb["TT"][:, :], hb["TT"][:, :], pT[:, 0:128], OP.add)),
                         reads=[pT.res, hb["TT"].res], writes=[hb["TT"].res])
            for h in range(4):
                hb = H[h]
                hs = slice(h * 64, (h + 1) * 64)
                pU = _psr(B)
                B.mm(pU[:, 0:64], hb["TT"][:, :], vb[:, hs], True, True, reads=[hb["TT"].res, vb.res], writes=[pU.res])
                B.op("act", (lambda e, hb=hb, pU=pU: e.copy(hb["u"][:, :], pU[:, 0:64])), reads=[pU.res], writes=[hb["u"].res])
                pW = _psr(B)
                B.mm(pW[:, 0:128], kbgz[:, h, :], hb["TT"][:, :], True, True, reads=[kbgz.res, hb["TT"].res], writes=[pW.res])
                B.op("act", (lambda e, hb=hb, pW=pW: e.copy(hb["wT"][:, :], pW[:, 0:128])), reads=[pW.res], writes=[hb["wT"].res])
            po = B.psum[6 + (ti % 2)]
            for h in range(4):
                hb = H[h]
                rows = slice((h % 2) * 64, (h % 2) * 64 + 64)
                blk = h // 2
                hs = slice(h * 64, (h + 1) * 64)
                S0, S1 = Sin[blk], Sout[blk]
                pV = _psr(B)
                B.mm(pV[:, 0:64], hb["wT"][rows, :], S0[rows, :], True, True, reads=[hb["wT"].res, S0.res], writes=[pV.res])
                B.op("dve", (lambda e, hb=hb, pV=pV: e.tensor_tensor(hb["vnew"][:, :], hb["u"][:, :], pV[:, 0:64], OP.subtract)),
                     reads=[hb["u"].res, pV.res], writes=[hb["vnew"].res])
                B.mm(po[:, hs], qgT[rows, blk, :], S0[rows, :], True, False, reads=[qgT.res, S0.res], writes=[po.res])
                B.mm(po[:, hs], hb["PT"][:, :], hb["vnew"][:, :], False, True, reads=[hb["PT"].res, hb["vnew"].res], writes=[po.res])
                pS = _psr(B)
                B.mm(pS[:, 0:64], kdecz[:, h, :], hb["vnew"][:, :], True, True, reads=[kdecz.res, hb["vnew"].res], writes=[pS.res])
                B.op("dve", (lambda e, S0=S0, S1=S1, rows=rows, h=h, pS=pS: e.scalar_tensor_tensor(
                    S1[rows, :], S0[rows, :], smx[rows, 24 + h:25 + h], pS[rows, 0:64], OP.mult, OP.add)),
                    reads=[S0.res, smx.res, pS.res], writes=[S1.res])
            B.op("act", lambda e: e.copy(o_all[:, :], po[:, 0:256]), reads=[po.res], writes=[o_all.res])
            B.zres = zt.res
            _norm_gate_out(B, o_all, zt[:, :], nwb, 256, tok0)
        tile()
    for blk in range(2):
        Sf = Sp[blk][NPT % 2]
        B.dmaq(d["gdn_prompt"][l, blk * 128:(blk + 1) * 128, :], Sf[:, :], reads=[Sf.res], output=True)
    B.dmaq(d["conv_b_prompt"][l], d["uM"][TP - 3:TP, C_BQKV:C_BQKV + 768], reads=[B.uM_res], output=True)
    B.phase_end()


def _sample_bc(B, l):
    d = B.d
    TP = B.TP
    B.phase_begin()
    B.W256 = B.ring("W256", [128, 256], F32, 6)
    B.ybf = B.ring("ybf", [128, 256], BF16, 2)
    S = B.sb("S_s", [128, 4096])
    T1 = B.sb("T1_s", [128, 4096])
    T2 = B.sb("T2_s", [128, 4096])
    S3 = lambda t_: t_[:, :].rearrange("p (k v) -> p k v", k=64)
    S3T = lambda t_: t_[:, :].rearrange("p (k v) -> p v k", k=64)
    rows_bt = lambda ap: ap.rearrange("(b t) c -> b t c", t=4)
    bk = lambda col: col.unsqueeze(2).broadcast_to([128, 64, 64])
    bv = lambda col: col.unsqueeze(1).broadcast_to([128, 64, 64])
    b4 = lambda col: col.unsqueeze(2).broadcast_to([128, 4, 64])
    xp = B.sb("xp", [32, 7, 768])
    cw = B.sb("cw32", [32, 4, 768])
    acc = B.sb("acc_s", [32, 4, 768])
    tmpc = B.sb("tmpc_s", [32, 4, 768])
    B.dmaq(cw[:, :, :].rearrange("p j c -> p (j c)"), d["conv_b"][l].rearrange("j c -> (j c)").partition_broadcast(32), writes=[cw.res])
    B.dmaq(xp[:, 0:3, :], d["state_conv_b"][l], writes=[xp.res])
    B.dmaq(xp[:, 3:7, :], rows_bt(d["uM"][TP:TP + 128, C_BQKV:C_BQKV + 768]), writes=[xp.res], reads=[B.uM_res])
    B.dmaq(d["conv_b_sample"][l], xp[:, 4:7, :], reads=[xp.res], output=True)
    cwj = lambda j: cw[:, j, :].unsqueeze(1).broadcast_to([32, 4, 768])
    B.op("dve", lambda e: e.tensor_tensor(acc[:, :, :], xp[:, 0:4, :], cwj(0), OP.mult), reads=[xp.res, cw.res], writes=[acc.res])
    for j in range(1, 4):
        B.op("pool", (lambda e, j=j: e.tensor_tensor(tmpc[:, :, :], xp[:, j:j + 4, :], cwj(j), OP.mult)), reads=[xp.res, cw.res], writes=[tmpc.res])
        B.op("dve", lambda e: e.tensor_tensor(acc[:, :, :], acc[:, :, :], tmpc[:, :, :], OP.add), reads=[acc.res, tmpc.res], writes=[acc.res])
    B.op("act", lambda e: e.activation(acc[:, :, :], acc[:, :, :], AF.Silu), reads=[acc.res], writes=[acc.res])
    sqd_res = Res()
    B.dmaq(rows_bt(d["sqd"][:, :]), acc[:, :, :], reads=[acc.res], writes=[sqd_res])
    q, k, v = [B.sb(nm, [128, 4, 64]) for nm in ("q_s", "k_s", "v_s")]
    gab = B.sb("gab_s", [128, 4, 2])
    cst = B.sb("cst_s", [128, 4])
    for h in range(4):
        ps_ = slice(h * 32, (h + 1) * 32)
        for (dst, c0) in ((q, 0), (k, 256), (v, 512)):
            B.dmaq(dst[ps_, :, :], rows_bt(d["sqd"][:, c0 + h * 64:c0 + (h + 1) * 64]), reads=[sqd_res], writes=[dst.res])
        for i_, c0 in enumerate((C_BA, C_BB)):
            B.dmaq(gab[ps_, :, i_], rows_bt(d["uM"][TP:TP + 128, c0 + h:c0 + h + 1])[:, :, 0], reads=[B.uM_res], writes=[gab.res],
                   allow_slow_non_contiguous=True)
        B.dmaq(cst[ps_, 0:1], d["gdn_a_log"][l, h:h + 1].partition_broadcast(32), writes=[cst.res])
        B.dmaq(cst[ps_, 1:2], d["gdn_dt_bias"][l, h:h + 1].partition_broadcast(32), writes=[cst.res])
        B.dmaq(S[ps_, :], d["state_gdn"][l].rearrange("(b h) x -> h b x", h=4)[h], writes=[S.res])
    B.op("act", lambda e: e.activation(cst[:, 0:1], cst[:, 0:1], AF.Exp), reads=[cst.res], writes=[cst.res])
    B.op("dve", lambda e: e.tensor_scalar(cst[:, 0:1], cst[:, 0:1], -1.0, None, OP.mult), reads=[cst.res], writes=[cst.res])
    sm = B.sb("sm_s", [128, 32])
    sqt = B.sb("sqt_s", [128, 4, 64])
    for (src, c0) in ((q, 0), (k, 4)):
        B.op("pool", (lambda e, src=src: e.tensor_tensor(sqt[:, :, :], src[:, :, :], src[:, :, :], OP.mult)), reads=[src.res, sqt.res], writes=[sqt.res])
        B.op("dve", (lambda e, c0=c0: e.tensor_reduce(sm[:, c0:c0 + 4], sqt[:, :, :], AX.X, OP.add)), reads=[sqt.res, sm.res], writes=[sm.res])
    B.op("dve", lambda e: e.tensor_scalar(sm[:, 0:8], sm[:, 0:8], RMS_EPS, None, OP.add), reads=[sm.res], writes=[sm.res])
    B.op("act", lambda e: e.activation(sm[:, 0:8], sm[:, 0:8], AF.Ln), reads=[sm.res], writes=[sm.res])
    B.op("act", lambda e: e.activation(sm[:, 0:8], sm[:, 0:8], AF.Exp, scale=-0.5), reads=[sm.res], writes=[sm.res])
    B.op("dve", lambda e: e.tensor_scalar(sm[:, 0:4], sm[:, 0:4], 0.125, None, OP.mult), reads=[sm.res], writes=[sm.res])
    B.op("dve", lambda e: e.tensor_tensor(q[:, :, :], q[:, :, :], b4(sm[:, 0:4]), OP.mult), reads=[q.res, sm.res], writes=[q.res])
    B.op("dve", lambda e: e.tensor_tensor(k[:, :, :], k[:, :, :], b4(sm[:, 4:8]), OP.mult), reads=[k.res, sm.res], writes=[k.res])
    B.op("act", lambda e: e.activation(sm[:, 8:12], gab[:, :, 1], AF.Exp, scale=-1.0), reads=[gab.res, sm.res], writes=[sm.res])
    B.op("dve", lambda e: e.tensor_scalar(sm[:, 8:12], sm[:, 8:12], 1.0, None, OP.add), reads=[sm.res], writes=[sm.res])
    B.op("dve", lambda e: e.reciprocal(sm[:, 8:12], sm[:, 8:12]), reads=[sm.res], writes=[sm.res])
    B.op("dve", lambda e: e.tensor_scalar(sm[:, 12:16], gab[:, :, 0], cst[:, 1:2], None, OP.add), reads=[gab.res, cst.res, sm.res], writes=[sm.res])
    B.op("act", lambda e: e.activation(sm[:, 12:16], sm[:, 12:16], AF.Exp), reads=[sm.res], writes=[sm.res])
    B.op("dve", lambda e: e.tensor_scalar(sm[:, 12:16], sm[:, 12:16], 1.0, None, OP.add), reads=[sm.res], writes=[sm.res])
    B.op("act", lambda e: e.activation(sm[:, 12:16], sm[:, 12:16], AF.Ln), reads=[sm.res], writes=[sm.res])
    B.op("dve", lambda e: e.tensor_scalar(sm[:, 12:16], sm[:, 12:16], cst[:, 0:1], None, OP.mult), reads=[sm.res, cst.res], writes=[sm.res])
    B.op("act", lambda e: e.activation(sm[:, 16:20], sm[:, 12:16], AF.Exp), reads=[sm.res], writes=[sm.res])
    B.op("dve", lambda e: e.tensor_scalar(sm[:, 20:24], sm[:, 16:20], -1.0, None, OP.mult), reads=[sm.res], writes=[sm.res])
    o = B.sb("o_s", [128, 4, 64])
    kS = B.sb("kS_s", [128, 64])
    dlt = B.sb("dlt_s", [128, 64])
    for t in range(4):
        def step(t=t):
            B.op("dve", lambda e: e.tensor_tensor(S3(T1), S3(S), bk(k[:, t, :]), OP.mult), reads=[S.res, k.res], writes=[T1.res])
            B.op("dve", lambda e: e.tensor_reduce(kS[:, :], S3T(T1), AX.X, OP.add), reads=[T1.res], writes=[kS.res])
            B.op("dve", lambda e: e.scalar_tensor_tensor(dlt[:, :], kS[:, :], sm[:, 20 + t:21 + t], v[:, t, :], OP.mult, OP.add),
                 reads=[kS.res, sm.res, v.res], writes=[dlt.res])
            B.op("dve", lambda e: e.tensor_scalar(dlt[:, :], dlt[:, :], sm[:, 8 + t:9 + t], None, OP.mult), reads=[dlt.res, sm.res], writes=[dlt.res])
            B.op("pool", lambda e: e.tensor_tensor(S3(T2), bk(k[:, t, :]), bv(dlt[:, :]), OP.mult), reads=[k.res, dlt.res], writes=[T2.res])
            B.op("dve", lambda e: e.scalar_tensor_tensor(S[:, :], S[:, :], sm[:, 16 + t:17 + t], T2[:, :], OP.mult, OP.add),
                 reads=[S.res, sm.res, T2.res], writes=[S.res])
            B.op("pool", lambda e: e.tensor_tensor(S3(T1), S3(S), bk(q[:, t, :]), OP.mult), reads=[S.res, q.res], writes=[T1.res])
            B.op("dve", lambda e: e.tensor_reduce(o[:, t, :], S3T(T1), AX.X, OP.add), reads=[T1.res], writes=[o.res])
        step()
    sod_res = Res()
    for h in range(4):
        ps_ = slice(h * 32, (h + 1) * 32)
        B.dmaq(rows_bt(d["sod"][0][:, h * 64:(h + 1) * 64]), o[ps_, :, :], reads=[o.res], writes=[sod_res])
        B.dmaq(d["gdn_sample"][l].rearrange("(b h) x -> h b x", h=4)[h], S[ps_, :], reads=[S.res], output=True)
    nwb = B.sb("nwb_s", [128, 256])
    nwc = B.sb("nwc_s", [128, 256])
    for h in range(4):
        B.dmaq(nwb[:, h * 64:(h + 1) * 64], d["norm_b"][l].partition_broadcast(128), writes=[nwb.res])
        B.dmaq(nwc[:, h * 64:(h + 1) * 64], d["norm_c"][l].partition_broadcast(128), writes=[nwc.res])
    otm = B.sb("otm_s", [128, 256])
    zt = B.sb("zt_s", [128, 256])
    B.dmaq(otm[:, :], d["sod"][0], reads=[sod_res], writes=[otm.res])
    B.dmaq(zt[:, :], d["uM"][TP:TP + 128, C_BZ:C_BZ + 256], reads=[B.uM_res], writes=[zt.res])
    B.zres = zt.res
    _norm_gate_out(B, otm, zt[:, :], nwb, 256, TP)
    cq, cf, ci = q, k, v
    lbt = B.sb("lbt_s", [128, 3, 64])
    for h in range(4):
        ps_ = slice(h * 32, (h + 1) * 32)
        B.dmaq(cq[ps_, :, :], rows_bt(d["uMs"][:, h * 64:(h + 1) * 64]), reads=[B.uM_res, o.res], writes=[cq.res])
        B.dmaq(cf[ps_, :, :], rows_bt(d["uMs"][:, 256 + h * 64:256 + (h + 1) * 64]), reads=[B.uM_res, o.res], writes=[cf.res])
        B.dmaq(ci[ps_, :, :], rows_bt(d["uM"][TP:TP + 128, C_CI + h * 64:C_CI + (h + 1) * 64]), reads=[B.uM_res, o.res], writes=[ci.res])
        B.dmaq(S[ps_, :], d["state_hgrn"][l].rearrange("(b h) x -> h b x", h=4)[h], writes=[S.res])
        if l > 0:
            B.dmaq(lbt[ps_, 0, :], d["lower_bounds"][1, h * 64:(h + 1) * 64].partition_broadcast(32), writes=[lbt.res])
            B.dmaq(lbt[ps_, 2, :], d["lower_bounds"][0, h * 64:(h + 1) * 64].partition_broadcast(32), writes=[lbt.res])
    if l == 0:
        B.op("pool", lambda e: e.memset(lbt[:, 0, :], 0.0), writes=[lbt.res])
    else:
        B.op("dve", lambda e: e.tensor_tensor(lbt[:, 0, :], lbt[:, 0, :], lbt[:, 2, :], OP.subtract), reads=[lbt.res], writes=[lbt.res])
        B.op("act", lambda e: e.activation(lbt[:, 0, :], lbt[:, 0, :], AF.Sigmoid), reads=[lbt.res], writes=[lbt.res])
    B.op("dve", lambda e: e.tensor_scalar(lbt[:, 1, :], lbt[:, 0, :], -1.0, 1.0, OP.mult, OP.add), reads=[lbt.res], writes=[lbt.res])
    bt = lambda ap: ap.unsqueeze(1).broadcast_to([128, 4, 64])
    f_, kc = cf, sqt
    B.op("act", lambda e: e.activation(cf[:, :, :], cf[:, :, :], AF.Sigmoid), reads=[cf.res], writes=[cf.res])
    B.op("dve", lambda e: e.tensor_scalar(kc[:, :, :], cf[:, :, :], -1.0, 1.0, OP.mult, OP.add), reads=[cf.res, sqt.res], writes=[kc.res])
    B.op("dve", lambda e: e.tensor_tensor(kc[:, :, :], kc[:, :, :], bt(lbt[:, 1, :]), OP.mult), reads=[kc.res, lbt.res], writes=[kc.res])
    B.op("dve", lambda e: e.tensor_tensor(f_[:, :, :], cf[:, :, :], bt(lbt[:, 1, :]), OP.mult), reads=[cf.res, lbt.res], writes=[f_.res])
    B.op("dve", lambda e: e.tensor_tensor(f_[:, :, :], f_[:, :, :], bt(lbt[:, 0, :]), OP.add), reads=[f_.res, lbt.res], writes=[f_.res])
    B.op("act", lambda e: e.activation(cq[:, :, :], cq[:, :, :], AF.Silu), reads=[cq.res], writes=[cq.res])
    o2 = B.sb("o2_s", [128, 4, 64])
    for t in range(4):
        def step2(t=t):
            B.op("dve", lambda e: e.tensor_tensor(S3(S), S3(S), bk(f_[:, t, :]), OP.mult), reads=[S.res, f_.res], writes=[S.res])
            B.op("pool", lambda e: e.tensor_tensor(S3(T2), bk(kc[:, t, :]), bv(ci[:, t, :]), OP.mult), reads=[kc.res, ci.res], writes=[T2.res])
            B.op("dve", lambda e: e.tensor_tensor(S[:, :], S[:, :], T2[:, :], OP.add), reads=[S.res, T2.res], writes=[S.res])
            B.op("pool", lambda e: e.tensor_tensor(S3(T1), S3(S), bk(cq[:, t, :]), OP.mult), reads=[S.res, cq.res], writes=[T1.res])
            B.op("dve", lambda e: e.tensor_reduce(o2[:, t, :], S3T(T1), AX.X, OP.add), reads=[T1.res], writes=[o2.res])
        step2()
    sod2_res = Res()
    for h in range(4):
        ps_ = slice(h * 32, (h + 1) * 32)
        B.dmaq(rows_bt(d["sod"][1][:, h * 64:(h + 1) * 64]), o2[ps_, :, :], reads=[o2.res], writes=[sod2_res])
        B.dmaq(d["hgrn_sample"][l].rearrange("(b h) x -> h b x", h=4)[h], S[ps_, :], reads=[S.res], output=True)
    otm2 = B.sb("otm2_s", [128, 256])
    zt2 = B.sb("zt2_s", [128, 256])
    B.dmaq(otm2[:, :], d["sod"][1], reads=[sod2_res], writes=[otm2.res])
    B.dmaq(zt2[:, :], d["uM"][TP:TP + 128, C_CG:C_CG + 256], reads=[B.uM_res], writes=[zt2.res])
    B.zres = zt2.res
    _norm_gate_out(B, otm2, zt2[:, :], nwc, 512, TP)
    B.phase_end()


def _sample_attn_setup(B):
    d = B.d
    NLP, NPS = B.NLP, B.NPS
    B.ownT = B.sb("ownT", [128, 4 * NPS], glob=True)
    B.gidx = B.sb("gidx", [128, 4 * NPS], I32, glob=True)
    B.lastidx = B.sb("lastidx", [128, 32], I32, glob=True)
    B.locr = B.sb("locr", [128, 32], glob=True)
    B.NB = B.sb("NBn", [128, 4, 128], glob=True)
    B.BLg = B.sb("BLg", [128, 8, 4], glob=True)
    B.ec = B.sb("ec", [128, 4], glob=True)
    B.islast = B.sb("islast", [16, 4 * NPS], glob=True)
    B.BLT = B.sb("BLT", [16, 128], glob=True)
    B.Ind = B.sb("Ind", [16, 512], BF16, glob=True)
    B.phase_begin()
    rb = B.rbb
    misc = B.sb("misc", [128, 8])
    B.dmaq(misc[:, :], d["c_misc"][:, :], writes=[misc.res])
    import os as _os
    parts = _os.environ.get('SETUP_PARTS', '123')
    if '1' in parts:
      _setup_own(B, d, NLP, NPS)
    if '3' in parts:
      _setup_bias(B, d, rb)
    B.phase_end()


def _setup_own(B, d, NLP, NPS):
    NL = 4 * NPS
    B.dmaq(B.ownT[:, :], d["c_ownL"][:, :], writes=[B.ownT.res])
    B.dmaq(B.islast[:, :], d["c_isl"][:, :], writes=[B.islast.res])
    misc = B.sb("misc2", [128, 8])
    B.dmaq(misc[:, :], d["c_misc"][:, :], writes=[misc.res])
    pi = B.sb("pi", [128, NL], I32)
    pf = B.sb("pf", [128, NL])
    B.dmaq(pi[:, :], d["pt_own"].t.partition_broadcast(128), writes=[pi.res])
    B.op("dve", lambda e: e.tensor_copy(pf[:, :], pi[:, :]), reads=[pi.res], writes=[pf.res])
    B.op("dve", lambda e: e.tensor_scalar(pf[:, :], pf[:, :], 128.0, misc[:, 1:2], OP.mult, OP.add), reads=[pf.res, misc.res], writes=[pf.res])
    B.op("dve", lambda e: e.tensor_copy(B.gidx[:, :], pf[:, :]), reads=[pf.res], writes=[B.gidx.res])


def _setup_last(B, d, NLP, NPS, misc):
    li = B.sb("li", [128, 32 * NPS], I32)
    lf = B.sb("lf", [128, 32])
    t1 = B.sb("t1", [128, 32])
    B.dmaq(li[:, :], d["page_table"].t.rearrange("b j -> (b j)").partition_broadcast(128), writes=[li.res])
    B.op("dve", lambda e: e.tensor_copy(lf[:, :], li[:, :].rearrange("p (b j) -> p b j", j=NPS)[:, :, NPS - 1]), reads=[li.res], writes=[lf.res])
    B.op("dve", lambda e: e.tensor_scalar(lf[:, :], lf[:, :], misc[:, 0:1], None, OP.subtract), reads=[lf.res, misc.res], writes=[lf.res])
    B.op("dve", lambda e: e.tensor_scalar(B.locr[:, :], lf[:, :], 0.0, None, OP.is_ge), reads=[lf.res], writes=[B.locr.res])
    B.op("dve", lambda e: e.tensor_scalar(t1[:, :], lf[:, :], float(NLP - 1), None, OP.is_le), reads=[lf.res], writes=[t1.res])
    B.op("dve", lambda e: e.tensor_tensor(B.locr[:, :], B.locr[:, :], t1[:, :], OP.mult), reads=[B.locr.res, t1.res], writes=[B.locr.res])
    BIG = float(NLP * 128 + 4096)
    B.op("dve", lambda e: e.tensor_scalar(lf[:, :], lf[:, :], 128.0, misc[:, 1:2], OP.mult, OP.add), reads=[lf.res, misc.res], writes=[lf.res])
    B.op("dve", lambda e: e.tensor_scalar(lf[:, :], lf[:, :], -BIG, None, OP.add), reads=[lf.res], writes=[lf.res])
    B.op("dve", lambda e: e.tensor_tensor(lf[:, :], lf[:, :], B.locr[:, :], OP.mult), reads=[lf.res, B.locr.res], writes=[lf.res])
    B.op("dve", lambda e: e.tensor_scalar(lf[:, :], lf[:, :], BIG, None, OP.add), reads=[lf.res], writes=[lf.res])
    B.op("dve", lambda e: e.tensor_copy(B.lastidx[:, :], lf[:, :]), reads=[lf.res], writes=[B.lastidx.res])


def _setup_bias(B, d, rb):
    cn = B.sb("cn", [128, 5, 128])
    B.dmaq(cn[:, :, :], d["c_new"][:, :, :], writes=[cn.res])
    for h in range(4):
        B.op("dve", (lambda e, h=h: e.tensor_scalar(B.NB[:, h, :], cn[:, 4, :], -30000.0, None, OP.mult)), reads=[cn.res], writes=[B.NB.res])
        for dist in range(4):
            B.op("dve", (lambda e, h=h, dist=dist: e.scalar_tensor_tensor(B.NB[:, h, :], cn[:, dist, :], rb[:, dist * 4 + h:dist * 4 + h + 1],
                                                                         B.NB[:, h, :], OP.mult, OP.add)),
                 reads=[cn.res, rb.res, B.NB.res], writes=[B.NB.res])
    lb_ = B.sb("lastb", [128, 4, 32])
    B.dmaq(lb_[:, :, :], d["c_lastb"][:, :, :], writes=[lb_.res])
    tmp = B.sb("tmpl", [128, 4, 32])
    bl4 = B.sb("bl4", [128, 4, 4])
    for h in range(4):
        B.op("dve", (lambda e, h=h: e.tensor_tensor(tmp[:, :, :], lb_[:, :, :],
                                                    rb[:, :].rearrange("p (b h) -> p h b", h=4)[:, h, :].unsqueeze(1).broadcast_to([128, 4, 32]), OP.mult)),
             reads=[lb_.res, rb.res, tmp.res], writes=[tmp.res])
        B.op("dve", (lambda e, h=h: e.tensor_reduce(bl4[:, h, :], tmp[:, :, :], AX.X, OP.add)), reads=[tmp.res, bl4.res], writes=[bl4.res])
        B.op("dve", (lambda e, h=h: e.tensor_scalar(bl4[:, h, :], bl4[:, h, :], rb[:, 31 * 4 + h:31 * 4 + h + 1], None, OP.subtract)),
             reads=[bl4.res, rb.res], writes=[bl4.res])
        for m in range(2):
            B.op("dve", (lambda e, h=h, m=m: e.tensor_copy(B.BLg[:, 2 * h + m, :], bl4[:, h, :])), reads=[bl4.res, B.BLg.res], writes=[B.BLg.res])
    B.op("act", lambda e: e.activation(B.ec[:, :], rb[:, 124:128], AF.Exp), reads=[rb.res], writes=[B.ec.res])
    pt = B.psum[3]
    B.tr(pt[0:16, 0:128], bl4[:, :, :].rearrange("p h t -> p (h t)"), B.ident[:], reads=[bl4.res, B.ident.res], writes=[pt.res])
    B.op("act", lambda e: e.activation(B.BLT[:, :], pt[0:16, 0:128], AF.Copy, scale=float(32 ** 0.5)), reads=[pt.res], writes=[B.BLT.res])
    indf = B.sb("indf", [16, 512])
    B.dmaq(indf[:, :], d["c_ind"][:, :], writes=[indf.res])
    B.op("pool", lambda e: e.tensor_copy(B.Ind[:, :], indf[:, :]), reads=[indf.res], writes=[B.Ind.res])


def _sample_attn(B, l, lam_init):
    d = B.d
    TP, NLP = B.TP, B.NLP
    SC = 32 ** -0.5
    B.phase_begin()
    B.ev_ring = B.ring("ev", [128, 512], F32, 4)
    B.ybf = B.ring("ybf", [128, 256], BF16, 2)
    grp = lambda g: (slice(0, 32), g)
    Em = lambda E_, m: E_[:, :, :].rearrange("p (h m) q -> p m h q", m=2)[:, m]
    stg = B.sb("stg", [32, 8, 128])
    qTs = B.sb("qTs", [32, 8, 128], BF16)
    kTn = B.sb("kTn", [32, 8, 128], BF16)
    for (dst, r0) in ((qTs, R_DQ), (kTn, R_DK)):
        for g in range(8):
            rs, blk = grp(g)
            B.dmaq(stg[rs, blk, :], d["uT"][r0 + g * 32:r0 + (g + 1) * 32, TP:TP + 128], reads=[B.uT_res], writes=[stg.res])
        B.op("pool", (lambda e, dst=dst: e.tensor_copy(dst[:, :, :], stg[:, :, :])), reads=[stg.res], writes=[dst.res])
    vst = B.sb("vst", [128, 256])
    Vn = B.sb("Vn", [128, 4, 65], BF16)
    B.dmaq(vst[:, :], d["uM"][TP:TP + 128, C_DV:C_DV + 256], reads=[B.uM_res], writes=[vst.res])
    B.op("pool", lambda e: e.tensor_copy(Vn[:, :, 0:64], vst[:, :].rearrange("p (h d) -> p h d", h=4)), reads=[vst.res], writes=[Vn.res])
    B.op("pool", lambda e: e.memset(Vn[:, :, 64:65], 1.0), writes=[Vn.res])
    acc = B.sb("acc", [128, 8, 65])
    B.op("pool", lambda e: e.memset(acc[:, :, :], 0.0), writes=[acc.res])
    import os as _os
    _stop = int(_os.environ.get('SA_STOP', '9'))
    if _stop <= 1:
        B.phase_end()
        return
    kps = [B.sb("kp%d" % i, [128, 256]) for i in range(3)]
    vps = [B.sb("vp%d" % i, [128, 256]) for i in range(3)]
    kTs = [B.sb("kT%d" % i, [32, 8, 128], BF16) for i in range(2)]
    Vas = [B.sb("Va%d" % i, [128, 4, 65], BF16) for i in range(2)]
    Es = [B.sb("Ef%d" % i, [128, 8, 128], BF16) for i in range(2)]
    for Va in Vas:
        B.op("pool", (lambda e, Va=Va: e.memset(Va[:, :, 64:65], 1.0)), writes=[Va.res])
    Fps = [B.sb("Fp%d" % i, [16, 128], BF16) for i in range(2)]

    def kT_from(kp, kT, pt):
        for c in range(8):
            pp = pt[c // 4]
            B.tr(pp[0:32, (c % 4) * 128:(c % 4 + 1) * 128], kp[:, c * 32:(c + 1) * 32], B.ident[:], reads=[kp.res, B.ident.res], writes=[pp.res])
        for c2 in range(2):
            pp = pt[c2]
            B.op("act", (lambda e, pp=pp, c2=c2: e.copy(kT[:, c2 * 4:(c2 + 1) * 4, :], pp[0:32, 0:512].rearrange("p (c t) -> p c t", c=4))),
                 reads=[pp.res], writes=[kT.res])

    import os as _os
    bc_reg = [None]
    for p in range(4 * B.NPS):
        def page(p=p):
            kp, vp = kps[p % 3], vps[p % 3]
            for (dst, src) in ((kp, d["cache_k%d" % l]), (vp, d["cache_v%d" % l])):
                def gather(e, dst=dst, src=src):
                    if bc_reg[0] is None:
                        bc_reg[0] = e.to_reg(NLP * 128 - 1)
                    return e.indirect_dma_start(
                        out=dst[:, :], out_offset=None, in_=src[:, :],
                        in_offset=bass.IndirectOffsetOnAxis(ap=B.gidx[:, p:p + 1], axis=0),
                        bounds_check=bc_reg[0], oob_is_err=False)
                B.S.op("pool", gather, reads=[B.gidx.res], writes=[dst.res], dma=True)
            kT, Va, E = kTs[p % 2], Vas[p % 2], Es[p % 2]
            kT_from(kp, kT, (B.psum[6], B.psum[7]))
            B.op("dve", lambda e: e.tensor_copy(Va[:, :, 0:64], vp[:, :].rearrange("p (h d) -> p h d", h=4)), reads=[vp.res], writes=[Va.res])
            sM = (B.psum[2 * (p % 2)], B.psum[2 * (p % 2) + 1])
            Fp = Fps[p % 2]
            B.op("dve", lambda e: e.tensor_scalar(Fp[:, :], B.BLT[:, :], B.islast[:, p:p + 1], None, OP.mult),
                 reads=[B.BLT.res, B.islast.res], writes=[Fp.res])
            for m in range(2):
                B.mm(sM[m][:, :], Fp[:, :], B.Ind[:, :], True, False, reads=[Fp.res, B.Ind.res], writes=[sM[m].res])
            for g in range(8):
                rs, blk = grp(g)
                sp = sM[g % 2]
                B.mm(sp[:, (g // 2) * 128:(g // 2 + 1) * 128], kT[rs, blk, :], qTs[rs, blk, :], False, g >= 6,
                     reads=[kT.res, qTs.res], writes=[sp.res])
            for m in range(2):
                B.op("act", (lambda e, m=m: e.activation(Em(E, m), sM[m][:, :].rearrange("p (h q) -> p h q", h=4), AF.Exp, scale=SC)),
                     reads=[sM[m].res], writes=[E.res])
            oA, oB = B.psum[4], B.psum[5]
            for g in range(8):
                oo = oA if g < 4 else oB
                B.mm(oo[:, (g % 4) * 65:(g % 4 + 1) * 65], E[:, g, :], Va[:, g // 2, :], True, True, reads=[E.res, Va.res], writes=[oo.res])
            for (oo, gs) in ((oA, slice(0, 4)), (oB, slice(4, 8))):
                B.op("dve", (lambda e, oo=oo, gs=gs: e.scalar_tensor_tensor(
                    acc[:, gs, :], oo[:, 0:260].rearrange("p (g c) -> p g c", g=4), B.ownT[:, p:p + 1], acc[:, gs, :], OP.mult, OP.add)),
                    reads=[oo.res, B.ownT.res, acc.res], writes=[acc.res])
        page()
    tot = B.sb("tot", [128, 8, 65])
    if B.n_cores > 1:
        ag_res = Res()
        B.dmaq(d["agin"][:, :], acc[:, :, :].rearrange("p g c -> p (g c)"), reads=[acc.res], writes=[ag_res])
        B.S.op("pool", lambda e: e.collective_compute("AllGather", op=OP.bypass, replica_groups=[list(range(B.n_cores))],
                                                     ins=[d["agin"][:, :]], outs=[d["agout"][:, :]]),
               reads=[ag_res], writes=[ag_res], dma=True)
        gat = B.sb("gat", [128, B.n_cores, 520])
        B.dmaq(gat[:, :, :], d["agout"].t.rearrange("(r p) c -> p r c", p=128), reads=[ag_res], writes=[gat.res])
        totf = tot[:, :, :].rearrange("p g c -> p (g c)")
        B.op("dve", lambda e: e.tensor_tensor(totf, gat[:, 0, :], gat[:, 1, :], OP.add), reads=[gat.res], writes=[tot.res])
        for r in range(2, B.n_cores):
            B.op("dve", (lambda e, r=r: e.tensor_tensor(totf, totf, gat[:, r, :], OP.add)), reads=[gat.res, tot.res], writes=[tot.res])
    else:
        B.op("dve", lambda e: e.tensor_copy(tot[:, :, :], acc[:, :, :]), reads=[acc.res], writes=[tot.res])
    if _stop <= 2:
        B.phase_end()
        return
    En = Es[0]
    nA, nB = B.psum[0], B.psum[1]
    for g in range(8):
        rs, blk = grp(g)
        sp = (nA, nB)[g % 2]
        B.mm(sp[:, (g // 2) * 128:(g // 2 + 1) * 128], kTn[rs, blk, :], qTs[rs, blk, :], True, True, reads=[kTn.res, qTs.res], writes=[sp.res])
    if _os.environ.get('SA_NOEXP'):
        for sp in (nA, nB):
            tmp = B.ev_ring.next()
            B.op("dve", (lambda e, sp=sp, tmp=tmp: e.tensor_copy(tmp[:, :], sp[:, :])), reads=[sp.res], writes=[tmp.res])
            B.dmaq(d["facc"][0:128, 0:512], tmp[:, :], reads=[tmp.res])
        B.phase_end()
        return
    for g in range(8):
        sp = (nA, nB)[g % 2]
        tmp = B.ev_ring.next()
        B.op("dve", (lambda e, g=g, sp=sp, tmp=tmp: e.scalar_tensor_tensor(tmp[:, 0:128], sp[:, (g // 2) * 128:(g // 2 + 1) * 128], SC,
                                                                          B.NB[:, g // 2, :], OP.mult, OP.add)),
             reads=[sp.res, B.NB.res], writes=[tmp.res])
        B.op("act", (lambda e, g=g, tmp=tmp: e.activation(En[:, g, :], tmp[:, 0:128], AF.Exp)), reads=[tmp.res, En.res], writes=[En.res])
    if _stop <= 3:
        B.phase_end()
        return
    oA, oB = B.psum[2], B.psum[3]
    for g in range(8):
        oo = oA if g < 4 else oB
        B.mm(oo[:, (g % 4) * 65:(g % 4 + 1) * 65], En[:, g, :], Vn[:, g // 2, :], True, True, reads=[En.res, Vn.res], writes=[oo.res])
    for g in range(8):
        oo = oA if g < 4 else oB
        B.op("dve", (lambda e, g=g, oo=oo: e.scalar_tensor_tensor(tot[:, g, :], tot[:, g, :], B.ec[:, g // 2:g // 2 + 1],
                                                                 oo[:, (g % 4) * 65:(g % 4 + 1) * 65], OP.mult, OP.add)),
             reads=[tot.res, B.ec.res, oo.res], writes=[tot.res])
    if B.debug and l == 0:
        for nm, t_ in (("dbgp_tot", tot), ("dbgp_acc", acc)):
            dd = B.dram(nm, [128, 520])
            B.dmaq(dd[:, :], t_[:, :, :].rearrange("p g c -> p (g c)"), reads=[t_.res], output=True)
        dd = B.dram("dbgp_own", [128, NLP])
        B.dmaq(dd[:, :], B.ownT[:, :], reads=[B.ownT.res], output=True)
        dd = B.dram("dbgp_isl", [16, NLP])
        B.dmaq(dd[:, :], B.islast[:, :], reads=[B.islast.res], output=True)
    if _stop <= 4:
        B.phase_end()
        return
    for h in range(4):
        _diff_combine(B, l, lam_init, tot[:, 2 * h, :], tot[:, 2 * h + 1, :], 128, [tot.res], 768 + h * 64, TP, h)
    B.phase_end()
```
